# Optimizing a Trainium2 kernel written in Bass

```python
import jax
import jax.numpy as jnp
from jax import lax
import numpy as np

D_MODEL = 1024
BATCH = 8
SEQ = 8192
DEPTH = 4

D_MIX = D_MODEL
HEAD_DIM = 64
ATTN_WIDTH = D_MIX // 2
ATTN_HEADS = ATTN_WIDTH // HEAD_DIM
DILATED_BRANCHES = ((128, 1), (512, 4), (2048, 16))
BAND = 128
ROPE_THETA = 10000.0
SGU_WIDTH = D_MIX // 4
SGU_GROUPS = 4
SGU_GROUP_DIM = SGU_WIDTH // SGU_GROUPS
SGU_CHUNK = 128
CONV_WIDTH = D_MIX - ATTN_WIDTH - SGU_WIDTH
CONV_TAPS = 3
PROJ_SIZES = (ATTN_WIDTH, ATTN_WIDTH, ATTN_WIDTH, SGU_WIDTH, SGU_WIDTH, CONV_WIDTH, CONV_WIDTH, CONV_WIDTH)
PROJ_WIDTH = 3 * ATTN_WIDTH + 2 * SGU_WIDTH + 3 * CONV_WIDTH
MOE_GROUPS = 4
EXPERTS_PER_GROUP = 8
N_EXPERTS = MOE_GROUPS * EXPERTS_PER_GROUP
TOP_K_INNER = 2
D_EXPERT = D_MODEL // 2
MOE_BLOCK = 256
ALPHA = (2.0 * DEPTH) ** 0.25
BETA = (8.0 * DEPTH) ** -0.25
EPS = 1e-5

kernel_name = 'hybrid_dilated_sgu_conv_hmoe'


def layer_norm(t, g, b):
    t32 = t.astype(jnp.float32)
    mu = jnp.mean(t32, -1, keepdims=True)
    var = jnp.mean(jnp.square(t32 - mu), -1, keepdims=True)
    return ((t32 - mu) * lax.rsqrt(var + EPS) * g + b).astype(t.dtype)


def rms_norm(t, g):
    t32 = t.astype(jnp.float32)
    return (t32 * lax.rsqrt(jnp.mean(jnp.square(t32), -1, keepdims=True) + EPS) * g).astype(t.dtype)


def rope(t):
    seq, hd = t.shape[1], t.shape[-1]
    half = hd // 2
    inv_freq = ROPE_THETA ** (-jnp.arange(half, dtype=jnp.float32) / half)
    ang = jnp.arange(seq, dtype=jnp.float32)[:, None] * inv_freq[None, :]
    cos = jnp.cos(ang)[None, :, None, :]
    sin = jnp.sin(ang)[None, :, None, :]
    t32 = t.astype(jnp.float32)
    t1, t2 = t32[..., :half], t32[..., half:]
    return jnp.concatenate([t1 * cos - t2 * sin, t2 * cos + t1 * sin], -1).astype(t.dtype)


def _to_strided(t, d, nb):
    b, s, h, hd = t.shape
    m = s // d
    t = t.reshape(b, m, d, h, hd).transpose(0, 2, 3, 1, 4)
    t = jnp.pad(t, ((0, 0), (0, 0), (0, 0), (0, nb * BAND - m), (0, 0)))
    return t.reshape(b, d, h, nb, BAND, hd)


def _with_prev(t):
    prev = jnp.pad(t[:, :, :, :-1], ((0, 0), (0, 0), (0, 0), (1, 0), (0, 0), (0, 0)))
    return jnp.concatenate([prev, t], axis=4)


def dilated_branch(q, k, v, d, n_back):
    b, s, h, hd = q.shape
    m = s // d
    nb = -(-m // BAND)
    qs = _to_strided(q, d, nb)
    ks = _with_prev(_to_strided(k, d, nb))
    vs = _with_prev(_to_strided(v, d, nb))
    sc = jnp.einsum('bdhnqc,bdhnkc->bdhnqk', qs, ks).astype(jnp.float32)
    qi = jnp.arange(BAND)[:, None] + BAND
    ki = jnp.arange(2 * BAND)[None, :]
    dist = qi - ki
    in_band = (dist >= 0) & (dist <= n_back)
    has_prev = (jnp.arange(nb)[:, None, None] > 0) | (ki[None] >= BAND)
    mask = in_band[None] & has_prev
    sc = jnp.where(mask, sc, -jnp.inf)
    mx = jnp.max(sc, -1, keepdims=True)
    p = jnp.exp(sc - mx)
    den = jnp.sum(p, -1, keepdims=True)
    o = jnp.einsum('bdhnqk,bdhnkc->bdhnqc', p, vs.astype(jnp.float32)) / den
    lse = (mx + jnp.log(den))[..., 0]
    o = o.reshape(b, d, h, nb * BAND, hd)[:, :, :, :m].transpose(0, 3, 1, 2, 4).reshape(b, s, h, hd)
    lse = lse.reshape(b, d, h, nb * BAND)[:, :, :, :m].transpose(0, 3, 1, 2).reshape(b, s, h)
    return o, lse


def dilated_attention(q, k, v):
    b, s, _ = q.shape
    q = rope(q.reshape(b, s, ATTN_HEADS, HEAD_DIM)) * (HEAD_DIM ** -0.5)
    k = rope(k.reshape(b, s, ATTN_HEADS, HEAD_DIM))
    v = v.reshape(b, s, ATTN_HEADS, HEAD_DIM)
    outs, lses = [], []
    for window, dil in DILATED_BRANCHES:
        o, l = dilated_branch(q, k, v, dil, window // dil)
        outs.append(o)
        lses.append(l)
    wts = jax.nn.softmax(jnp.stack(lses), axis=0)
    out = jnp.sum(wts[..., None] * jnp.stack(outs), axis=0)
    return out.reshape(b, s, ATTN_WIDTH).astype(q.dtype)


def spatial_gating(u, z, gain, w_s, b_s):
    b, s, _ = u.shape
    nc = s // SGU_CHUNK
    u = jax.nn.gelu(u)
    z = jax.nn.gelu(z).reshape(b, s, SGU_GROUPS, SGU_GROUP_DIM).astype(jnp.float32)
    mu = jnp.mean(z, -1, keepdims=True)
    var = jnp.mean(jnp.square(z - mu), -1, keepdims=True)
    z = (z - mu) * lax.rsqrt(var + EPS) * gain.reshape(SGU_GROUPS, SGU_GROUP_DIM)
    z = z.reshape(b, nc, SGU_CHUNK, SGU_GROUPS, SGU_GROUP_DIM)
    causal = jnp.tril(jnp.ones((SGU_CHUNK, SGU_CHUNK), dtype=bool))
    w = jnp.where(causal[None], w_s, 0)
    sp = jnp.einsum('gts,bnsgc->bntgc', w, z) + b_s.T[None, None, :, :, None]
    return (u * sp.reshape(b, s, SGU_WIDTH)).astype(u.dtype)


def short_conv(gb, gc, h, conv_w):
    z = gc * h
    y = lax.conv_general_dilated(z, conv_w[:, None, :], window_strides=(1,), padding=((CONV_TAPS - 1, 0),), dimension_numbers=('NWC', 'WIO', 'NWC'), feature_group_count=CONV_WIDTH)
    return gb * y


def hierarchical_moe(x, wg, bg, we, be, w_gate, w_up, w_down):
    b, s, dm = x.shape
    n = b * s
    xt = x.reshape(n, dm)
    g_logit = (xt @ wg).astype(jnp.float32) + bg.astype(jnp.float32)
    g_prob = jax.nn.softmax(g_logit, -1)
    _, g_idx = lax.top_k(g_logit, 1)
    g_p = jnp.take_along_axis(g_prob, g_idx, axis=1)
    e_logit = ((xt @ we).astype(jnp.float32) + be.astype(jnp.float32)).reshape(n, MOE_GROUPS, EXPERTS_PER_GROUP)
    e_logit = jnp.take_along_axis(e_logit, jnp.broadcast_to(g_idx[:, :, None], (n, 1, EXPERTS_PER_GROUP)), axis=1)[:, 0]
    top_logit, top_local = lax.top_k(e_logit, TOP_K_INNER)
    gate = g_p * jax.nn.softmax(top_logit, -1)
    expert = g_idx * EXPERTS_PER_GROUP + top_local
    n_assign = n * TOP_K_INNER
    e_flat = expert.reshape(n_assign)
    tok_flat = jnp.repeat(jnp.arange(n, dtype=jnp.int32), TOP_K_INNER)
    order = jnp.argsort(e_flat, stable=True)
    e_sorted = e_flat[order]
    tok_sorted = tok_flat[order]
    gate_sorted = gate.reshape(n_assign)[order]
    counts = jnp.bincount(e_flat, length=N_EXPERTS)
    padded = (counts + MOE_BLOCK - 1) // MOE_BLOCK * MOE_BLOCK
    pad_end = jnp.cumsum(padded)
    pad_start = pad_end - padded
    cnt_start = jnp.cumsum(counts) - counts
    dest = pad_start[e_sorted] + jnp.arange(n_assign, dtype=jnp.int32) - cnt_start[e_sorted]
    n_blocks = (n_assign + N_EXPERTS * (MOE_BLOCK - 1) + MOE_BLOCK - 1) // MOE_BLOCK
    row_tok = jnp.full((n_blocks * MOE_BLOCK,), n, jnp.int32).at[dest].set(tok_sorted)
    block_expert = jnp.minimum(jnp.searchsorted(pad_end, jnp.arange(n_blocks, dtype=jnp.int32) * MOE_BLOCK, side='right'), N_EXPERTS - 1)
    x_pad = jnp.concatenate([xt, jnp.zeros((1, dm), xt.dtype)], 0)

    def run_block(args):
        rows, e = args
        xb = x_pad[rows]
        hdn = jax.nn.silu(xb @ w_gate[e]) * (xb @ w_up[e])
        return hdn @ w_down[e]

    y_rows = lax.map(run_block, (row_tok.reshape(n_blocks, MOE_BLOCK), block_expert)).reshape(n_blocks * MOE_BLOCK, dm)
    y_assign = y_rows[dest] * gate_sorted[:, None].astype(y_rows.dtype)
    out = jax.ops.segment_sum(y_assign, tok_sorted, num_segments=n)
    return out.reshape(b, s, dm)


def setup_inputs(seed: int = 0) -> dict:
    key = jax.random.key(seed)
    ks = jax.random.split(key, 17)

    def nrm(k, shape, scale):
        return jax.random.normal(k, shape, jnp.float32) * scale

    return {
        'x': nrm(ks[0], (BATCH, SEQ, D_MODEL), 1.0),
        'w_in': nrm(ks[1], (DEPTH, D_MODEL, PROJ_WIDTH), D_MODEL ** -0.5),
        'w_out': nrm(ks[2], (DEPTH, D_MIX, D_MODEL), BETA * D_MIX ** -0.5),
        'branch_gain': 1.0 + nrm(ks[3], (DEPTH, D_MIX), 0.02),
        'sgu_gain': 1.0 + nrm(ks[4], (DEPTH, SGU_WIDTH), 0.02),
        'sgu_w': nrm(ks[5], (DEPTH, SGU_GROUPS, SGU_CHUNK, SGU_CHUNK), SGU_CHUNK ** -0.5),
        'sgu_b': 1.0 + nrm(ks[6], (DEPTH, SGU_GROUPS, SGU_CHUNK), 0.1),
        'conv_w': nrm(ks[7], (DEPTH, CONV_TAPS, CONV_WIDTH), CONV_TAPS ** -0.5),
        'ln_gain': 1.0 + nrm(ks[8], (DEPTH, 2, D_MODEL), 0.02),
        'ln_bias': nrm(ks[9], (DEPTH, 2, D_MODEL), 0.02),
        'router_group_w': nrm(ks[10], (DEPTH, D_MODEL, MOE_GROUPS), D_MODEL ** -0.5),
        'router_group_b': nrm(ks[11], (DEPTH, MOE_GROUPS), 0.01),
        'router_expert_w': nrm(ks[12], (DEPTH, D_MODEL, N_EXPERTS), D_MODEL ** -0.5),
        'router_expert_b': nrm(ks[13], (DEPTH, N_EXPERTS), 0.01),
        'expert_w_gate': nrm(ks[14], (DEPTH, N_EXPERTS, D_MODEL, D_EXPERT), D_MODEL ** -0.5),
        'expert_w_up': nrm(ks[15], (DEPTH, N_EXPERTS, D_MODEL, D_EXPERT), D_MODEL ** -0.5),
        'expert_w_down': nrm(ks[16], (DEPTH, N_EXPERTS, D_EXPERT, D_MODEL), BETA * D_EXPERT ** -0.5),
    }


def reference(x, w_in, w_out, branch_gain, sgu_gain, sgu_w, sgu_b, conv_w, ln_gain, ln_bias, router_group_w, router_group_b, router_expert_w, router_expert_b, expert_w_gate, expert_w_up, expert_w_down):
    a_end = ATTN_WIDTH
    b_end = ATTN_WIDTH + SGU_WIDTH
    split_at = []
    acc = 0
    for size in PROJ_SIZES[:-1]:
        acc += size
        split_at.append(acc)
    for l in range(DEPTH):
        proj = x @ w_in[l]
        q, k, v, u, z, gb, gc, h = jnp.split(proj, split_at, axis=-1)
        y_a = dilated_attention(q, k, v)
        y_b = spatial_gating(u, z, sgu_gain[l], sgu_w[l], sgu_b[l])
        y_c = short_conv(gb, gc, h, conv_w[l])
        g = branch_gain[l]
        mixed = jnp.concatenate([rms_norm(y_a, g[:a_end]), rms_norm(y_b, g[a_end:b_end]), rms_norm(y_c, g[b_end:])], axis=-1)
        x = layer_norm(ALPHA * x + mixed @ w_out[l], ln_gain[l, 0], ln_bias[l, 0])
        ffn = hierarchical_moe(x, router_group_w[l], router_group_b[l], router_expert_w[l], router_expert_b[l], expert_w_gate[l], expert_w_up[l], expert_w_down[l])
        x = layer_norm(ALPHA * x + ffn, ln_gain[l, 1], ln_bias[l, 1])
    return x
```

```python
import os
import numpy as np
from contextlib import ExitStack
import concourse.bass as bass
import concourse.mybir as mybir
from concourse.bass_utils import run_bass_kernel_spmd

F32 = mybir.dt.float32
BF16 = mybir.dt.bfloat16
I32 = mybir.dt.int32
AF = mybir.ActivationFunctionType
ALU = mybir.AluOpType
AX = mybir.AxisListType

NCORES = 8
S_LEN = 8192
DM = 1024
DEPTH = 4
PW = 2816
NE = 32
DE = 512
NT = S_LEN // 128
NG = S_LEN // 512
BLK = 512
NBLK = 63
NSLOT = NBLK * BLK
ALPHA = (2.0 * DEPTH) ** 0.25
EPS = 1e-5
BRANCH_D = (1, 4, 16)
NEG = -30000.0

C_ID, C_PERM, C_MASK, C_LS, C_UCAT, C_THR, C_BV, C_IE, C_IP, C_SEL, C_INVW, C_END = (
    0, 128, 256, 512, 640, 704, 737, 801, 833, 834, 898, 904)

ENGS = ["tensor", "vector", "scalar", "gpsimd", "sync"]


class Sched:
    def __init__(self, nc, stack):
        self.nc = nc
        self.stack = stack
        self.q = {e: [] for e in ENGS}
        self.sems = {}
        self.cnt = {}
        self.seen = {e: {} for e in ENGS}
        self.last_w = {}
        self.readers = {}
        self.epoch = 0
        for e in ENGS:
            self._sem("E_%s_0" % e)
        self.n_ops = 0
        self.breg = None
        self.breg_val = None

    def new_epoch(self):
        self.epoch += 1
        self._sem("E_tensor_%d" % self.epoch)

    def _sem(self, name):
        if name not in self.sems:
            self.sems[name] = self.stack.enter_context(self.nc.semaphore(name))
            self.cnt[name] = 0
        return self.sems[name]

    def op(self, eng, fn, reads=(), writes=(), dma=None):
        pr = [k for k in reads if k.startswith(("ps", "e_ps", "e_wg", "e_wu"))]
        if pr:
            writes = list(writes) + pr
        deps = {}

        def add(ev):
            if ev is not None and deps.get(ev[0], 0) < ev[1]:
                deps[ev[0]] = ev[1]

        for k in reads:
            add(self.last_w.get(k))
        for k in writes:
            add(self.last_w.get(k))
            for ev in self.readers.get(k, ()):
                add(ev)
        waits = []
        seen = self.seen[eng]
        for s, v in deps.items():
            if eng == "tensor" and s.startswith("E_tensor"):
                continue
            if seen.get(s, 0) < v:
                seen[s] = v
                waits.append((self.sems[s], v))
        if dma is None:
            sname, inc = "E_%s_%d" % (eng, self.epoch if eng == "tensor" else 0), 1
        else:
            sname, inc = "D_" + dma, 16
            self._sem(sname)
        self.cnt[sname] += inc
        ev = (sname, self.cnt[sname])
        self.q[eng].append((waits, fn, self.sems[sname], inc))
        for k in reads:
            self.readers.setdefault(k, []).append(ev)
        for k in writes:
            self.last_w[k] = ev
            self.readers[k] = []
        self.n_ops += 1
        return ev

    def barrier(self):
        for e in ENGS:
            waits = []
            for s, c in self.cnt.items():
                if c > 0 and self.seen[e].get(s, 0) < c:
                    self.seen[e][s] = c
                    waits.append((self.sems[s], c))
            if waits:
                self.q[e].append((waits, None, None, 0))
        self.last_w = {}
        self.readers = {}

    def emit(self, want_breg=False):
        with self.nc.Block() as block:
            for e in ENGS:
                items = self.q[e]

                def body(engine, items=items, e=e):
                    if e == "gpsimd" and want_breg:
                        self.breg = engine.to_reg(self.breg_val)
                    for waits, fn, sem, inc in items:
                        for s, v in waits:
                            engine.wait_ge(s, v)
                        if fn is not None:
                            fn(engine).then_inc(sem, inc)

                getattr(block, e)(body)
        self.q = {e: [] for e in ENGS}


def build_program(depth=DEPTH, dbg=None):
    nc = bass.Bass("TRN2", target_bir_lowering=False)
    dt_in = lambda name, shape, dt=F32: nc.dram_tensor(name, shape, dt, kind="ExternalInput").ap()
    dt_sc = lambda name, shape, dt: nc.dram_tensor(name, shape, dt, kind=("ExternalOutput" if dbg else "Internal")).ap()
    L = depth
    x_in = dt_in("x", [S_LEN, DM])
    w_in = dt_in("w_in", [L, DM, PW])
    w_out = dt_in("w_out", [L, DM, DM])
    branch_gain = dt_in("branch_gain", [L, DM])
    sgu_gain = dt_in("sgu_gain", [L, 256])
    sgu_w = dt_in("sgu_w", [L, 4, 128, 128])
    sgu_b = dt_in("sgu_b", [L, 4, 128])
    conv_w = dt_in("conv_w", [L, 3, 256])
    ln_gain = dt_in("ln_gain", [L, 2, DM])
    ln_bias = dt_in("ln_bias", [L, 2, DM])
    rgw = dt_in("router_group_w", [L, DM, 4])
    rgb = dt_in("router_group_b", [L, 4])
    rew = dt_in("router_expert_w", [L, DM, NE])
    reb = dt_in("router_expert_b", [L, NE])
    ewg = dt_in("expert_w_gate", [L, NE, DM, DE])
    ewu = dt_in("expert_w_up", [L, NE, DM, DE])
    ewd = dt_in("expert_w_down", [L, NE, DE, DM])
    consts = dt_in("consts", [128, C_END])
    ropecos = dt_in("ropecos", [128, S_LEN])
    ropesin = dt_in("ropesin", [128, S_LEN])
    out = nc.dram_tensor("out", [S_LEN, DM], F32, kind="ExternalOutput").ap()

    xT = dt_sc("xT", [8, 128, S_LEN], BF16)
    QT = dt_sc("QT", [4, 128, S_LEN], BF16)
    KT = dt_sc("KT", [4, 128, S_LEN], BF16)
    Vd = dt_sc("Vd", [S_LEN, 512], BF16)
    mixT = dt_sc("mixT", [8, 128, S_LEN], BF16)
    X1 = dt_sc("X1", [S_LEN, DM], F32)
    X1b = dt_sc("X1b", [S_LEN, DM], BF16)
    X2 = dt_sc("X2", [S_LEN, DM], F32)
    Xs = dt_sc("Xs", [NSLOT, DM], BF16)
    Ys = dt_sc("Ys", [NSLOT, DM], BF16)

    with ExitStack() as top:
        S = Sched(nc, top)
        S.breg_val = L * NE * 128 * 2 - 1

        uniq = [0]

        def SB(st, name, shape, dt):
            uniq[0] += 1
            return st.enter_context(nc.sbuf_tensor("%s_%d" % (name, uniq[0]), shape, dt))

        def PS(st, name, shape, dt=F32):
            uniq[0] += 1
            return st.enter_context(nc.psum_tensor("%s_%d" % (name, uniq[0]), shape, dt))

        cst = SB(top, "cst", [128, C_END], F32)
        ident_bf = SB(top, "ident_bf", [128, 128], BF16)
        perm_bf = SB(top, "perm_bf", [128, 128], BF16)
        mask_bf = SB(top, "mask_bf", [128, 256], BF16)
        ls_bf = SB(top, "ls_bf", [128, 128], BF16)
        ones_bf = SB(top, "ones_bf", [128, 128], BF16)
        zeroc = SB(top, "zeroc", [128, 1], F32)
        epsc = SB(top, "epsc", [128, 1], F32)
        epsA2 = SB(top, "epsA2", [128, 1], F32)
        epsLN = SB(top, "epsLN", [128, 1], F32)
        M1all = SB(top, "M1all", [128, NT, NE], F32)
        M2all = SB(top, "M2all", [128, NT, NE], F32)
        RK = SB(top, "RK", [128, NT, 2], F32)
        W01 = SB(top, "W01", [128, NT, 2], F32)
        DSTi = SB(top, "DSTi", [128, NT, 2], I32)
        GIDX = SB(top, "GIDX", [128, NBLK], I32)
        GIDX2 = SB(top, "GIDX2", [128, NBLK, 2], I32)

        S.op("sync", lambda e: e.dma_start(out=cst[:], in_=consts), writes=["cst"], dma="cst")
        S.op("vector", lambda e: e.tensor_copy(ident_bf[:], cst[:, C_ID:C_ID + 128]), reads=["cst"], writes=["ident_bf"])
        S.op("vector", lambda e: e.tensor_copy(perm_bf[:], cst[:, C_PERM:C_PERM + 128]), reads=["cst"], writes=["perm_bf"])
        S.op("vector", lambda e: e.tensor_copy(mask_bf[:], cst[:, C_MASK:C_MASK + 256]), reads=["cst"], writes=["mask_bf"])
        S.op("vector", lambda e: e.tensor_copy(ls_bf[:], cst[:, C_LS:C_LS + 128]), reads=["cst"], writes=["ls_bf"])
        S.op("vector", lambda e: e.memset(ones_bf[:], 1.0), writes=["ones_bf"])
        S.op("vector", lambda e: e.memset(zeroc[:], 0.0), writes=["zeroc"])
        S.op("vector", lambda e: e.memset(epsc[:], EPS), writes=["epsc"])
        S.op("vector", lambda e: e.memset(epsA2[:], EPS * ALPHA * ALPHA), writes=["epsA2"])
        S.op("vector", lambda e: e.memset(epsLN[:], EPS / (ALPHA * ALPHA)), writes=["epsLN"])
        S.barrier()
        S.emit()

        def emit_xT_group(st_tiles, src_key_fn, src_ap_fn, g):
            ps_t, xTg = st_tiles
            for sub in range(4):
                pk = "ps_xt%d" % (sub % 2)
                for j in range(8):
                    S.op("tensor", lambda e, sub=sub, j=j: e.transpose(
                        ps_t[sub % 2][:, j, :], src_ap_fn(sub)[:, j * 128:(j + 1) * 128], ident_bf[:]),
                        reads=[src_key_fn(sub), "ident_bf"], writes=[pk])
                S.op("scalar", lambda e, sub=sub: e.copy(xTg[:, :, sub * 128:(sub + 1) * 128], ps_t[sub % 2][:]),
                     reads=[pk], writes=["xTg"])
            S.op("sync", lambda e: e.dma_start(
                out=xT.rearrange("c p t -> p c t")[:, :, g * 512:(g + 1) * 512], in_=xTg[:]),
                reads=["xTg"], writes=["xT_dram"], dma="xTg")

        with ExitStack() as ph:
            xin = [SB(ph, "p_xin%d" % i, [128, DM], F32) for i in range(2)]
            xb = [SB(ph, "p_xb%d" % i, [128, DM], BF16) for i in range(4)]
            ps_t = [PS(ph, "p_pst%d" % i, [128, 8, 128], BF16) for i in range(2)]
            xTg = SB(ph, "p_xTg", [128, 8, 512], BF16)
            for g in range(NG):
                for sub in range(4):
                    tt = g * 4 + sub
                    k = "xin%d" % (tt % 2)
                    S.op("sync", lambda e, tt=tt: e.dma_start(out=xin[tt % 2][:], in_=x_in[tt * 128:(tt + 1) * 128, :]),
                         writes=[k], dma=k)
                    S.op("vector", lambda e, tt=tt, sub=sub: e.tensor_copy(xb[sub][:], xin[tt % 2][:]),
                         reads=[k], writes=["xb%d" % sub])
                emit_xT_group((ps_t, xTg), lambda sub: "xb%d" % sub, lambda sub: xb[sub], g)
            S.barrier()
            S.emit()

        for l in range(L):
            if l > 0:
                S.new_epoch()
            Xin = x_in if l == 0 else X2
            Xout = out if l == L - 1 else X2
            with ExitStack() as ph:
                win = SB(ph, "a_win", [128, 8, PW], BF16)
                xt = [SB(ph, "a_xt%d" % i, [128, 8, 512], BF16) for i in range(2)]
                cs = [SB(ph, "a_cos%d" % i, [128, 512], F32) for i in range(2)]
                sn = [SB(ph, "a_sin%d" % i, [128, 512], F32) for i in range(2)]
                qbf = [SB(ph, "a_qbf%d" % i, [128, 512], BF16) for i in range(2)]
                t1 = [SB(ph, "a_t1%d" % i, [128, 512], F32) for i in range(2)]
                t2 = [SB(ph, "a_t2%d" % i, [128, 512], F32) for i in range(2)]
                qr = [SB(ph, "a_qr%d" % i, [128, 512], BF16) for i in range(2)]
                vsb = [SB(ph, "a_v%d" % i, [128, 4, 512], BF16) for i in range(2)]
                ga = [SB(ph, "a_ga%d" % i, [128, 512], F32) for i in range(2)]
                gt = [SB(ph, "a_gt%d" % i, [128, 512], F32) for i in range(2)]
                ug = [SB(ph, "a_ug%d" % i, [64, 512], F32) for i in range(4)]
                zg = [SB(ph, "a_zg%d" % i, [128, 256], F32) for i in range(2)]
                zsq = SB(ph, "a_zsq", [128, 256], F32)
                zst = SB(ph, "a_zst", [128, 16], F32)
                zn = [SB(ph, "a_zn%d" % i, [128, 4, 64], BF16) for i in range(4)]
                spv = [SB(ph, "a_spv%d" % i, [64, 512], F32) for i in range(2)]
                yb = [SB(ph, "a_yb%d" % i, [64, 512], BF16) for i in range(2)]
                wsf = SB(ph, "a_wsf", [128, 4, 128], F32)
                wsT = SB(ph, "a_wsT", [128, 4, 128], BF16)
                bsb = SB(ph, "a_bsb", [64, 4, 128], F32)
                sgain = SB(ph, "a_sgain", [64, 4], F32)
                cw = SB(ph, "a_cw", [128, 2, 3], F32)
                hs = [SB(ph, "a_hs%d" % i, [128, 512], F32) for i in range(2)]
                gbs = [SB(ph, "a_gbs%d" % i, [128, 512], F32) for i in range(2)]
                zce = SB(ph, "a_zce", [128, 2, 514], F32)
                yc = [SB(ph, "a_yc%d" % i, [128, 512], F32) for i in range(2)]
                ycb = [SB(ph, "a_ycb%d" % i, [128, 512], BF16) for i in range(2)]
                psA = [PS(ph, "a_ps%d" % i, [128, 512]) for i in range(7)]
                psW = psA[6][:].rearrange("p (g t) -> p g t", g=4)

                for dc in range(8):
                    S.op("gpsimd", lambda e, dc=dc: e.dma_start(out=win[:, dc, :], in_=w_in[l, dc * 128:(dc + 1) * 128, :]),
                         writes=["win"], dma="win")
                S.op("sync", lambda e: e.dma_start(out=wsf[:], in_=sgu_w[l].rearrange("g t s -> t g s")), writes=["wsf"], dma="wsf")
                for g in range(4):
                    S.op("gpsimd", lambda e, g=g: e.affine_select(out=wsf[:, g, :], in_=wsf[:, g, :], pattern=[[-1, 128]],
                                                                  compare_op=ALU.is_ge, fill=0.0, base=0, channel_multiplier=1),
                         reads=["wsf"], writes=["wsf"])
                for g in range(4):
                    S.op("tensor", lambda e, g=g: e.transpose(psW[:, g, :], wsf[:, g, :], cst[:, C_ID:C_ID + 128]),
                         reads=["wsf", "cst"], writes=["psA6"])
                S.op("vector", lambda e: e.tensor_copy(wsT[:], psW), reads=["psA6"], writes=["wsT"])
                for g in range(4):
                    S.op("sync", lambda e, g=g: e.dma_start(out=bsb[:, g, :], in_=sgu_b[l, g:g + 1, :].partition_broadcast(64)),
                         writes=["bsb"], dma="bsb")
                S.op("sync", lambda e: e.dma_start(out=sgain[:], in_=sgu_gain[l].rearrange("(g c) -> c g", g=4),
                                                   allow_slow_non_contiguous=True), writes=["sgain"], dma="sgain")
                for cc in range(2):
                    for k in range(3):
                        S.op("sync", lambda e, cc=cc, k=k: e.dma_start(out=cw[:, cc, k:k + 1], in_=conv_w[l, k:k + 1, cc * 128:(cc + 1) * 128].rearrange("k p -> p k"),
                                                                       allow_slow_non_contiguous=True), writes=["cw"], dma="cw")
                S.op("vector", lambda e: e.memset(zce[:], 0.0), writes=["zce"])

                psn = [0]

                def next_ps():
                    i = psn[0] % 7
                    psn[0] += 1
                    return i

                def gelu(src_ap, src_key, out_ap, out_key, P, N, bi):
                    a = ga[bi][0:P, 0:N]
                    t = gt[bi][0:P, 0:N]
                    ak, tk = "ga%d" % bi, "gt%d" % bi
                    S.op("scalar", lambda e: e.copy(a, src_ap), reads=[src_key], writes=[ak])
                    S.op("gpsimd", lambda e: e.tensor_tensor(out=t, in0=a, in1=a, op=ALU.mult), reads=[ak], writes=[tk])
                    S.op("vector", lambda e: e.tensor_scalar(t, t, 0.044715, 1.0, ALU.mult, ALU.add), reads=[tk], writes=[tk])
                    S.op("vector", lambda e: e.tensor_tensor(out=t, in0=t, in1=a, op=ALU.mult), reads=[tk, ak], writes=[tk])
                    S.op("scalar", lambda e: e.activation(out=t, in_=t, func=AF.Sigmoid, bias=zeroc[0:P, :], scale=1.5957691216),
                         reads=[tk, "zeroc"], writes=[tk])
                    S.op("vector", lambda e: e.tensor_tensor(out=out_ap, in0=a, in1=t, op=ALU.mult), reads=[ak, tk], writes=[out_key])

                gcount = [0]
                def a_load(i):
                    S.op("sync", lambda e: e.dma_start(out=xt[i % 2][:], in_=xT.rearrange("c p t -> p c t")[:, :, i * 512:(i + 1) * 512]),
                         reads=["xT_dram"], writes=["xt%d" % (i % 2)], dma="xt%d" % (i % 2))
                    S.op("sync", lambda e: e.dma_start(out=cs[i % 2][:], in_=ropecos[:, i * 512:(i + 1) * 512]),
                         writes=["cs%d" % (i % 2)], dma="cs%d" % (i % 2))
                    S.op("sync", lambda e: e.dma_start(out=sn[i % 2][:], in_=ropesin[:, i * 512:(i + 1) * 512]),
                         writes=["sn%d" % (i % 2)], dma="sn%d" % (i % 2))

                a_load(0)
                for i in range(NG):
                    tsl = slice(i * 512, (i + 1) * 512)
                    xk = "xt%d" % (i % 2)
                    xti = xt[i % 2]
                    csk, snk = "cs%d" % (i % 2), "sn%d" % (i % 2)
                    def qk_main(qi, xti=xti):
                        c = qi % 4
                        isq = qi < 4
                        col0 = (0 if isq else 512) + c * 128
                        scale = 0.125 if isq else 1.0
                        p = next_ps()
                        pk = "psA%d" % p
                        for dc in range(8):
                            S.op("tensor", lambda e, dc=dc: e.matmul(
                                psA[p][:], win[:, dc, col0:col0 + 128], xti[:, dc, :], start=(dc == 0), stop=(dc == 7)),
                                reads=["win", xk], writes=[pk])
                        b2 = qi % 2
                        S.op("scalar", lambda e: e.activation(
                            out=qbf[b2][:], in_=psA[p][:], func=AF.Identity, bias=zeroc[:], scale=scale),
                            reads=[pk, "zeroc"], writes=["qbf%d" % b2])
                        return p

                    def qk_rope(qi, p, i=i, xti=xti):
                        c = qi % 4
                        isq = qi < 4
                        scale = 0.125 if isq else 1.0
                        pk = "psA%d" % p
                        b2 = qi % 2
                        S.op("vector", lambda e: e.scalar_tensor_tensor(
                            out=t2[b2][:], in0=psA[p][:], scalar=scale, in1=cs[i % 2][:], op0=ALU.mult, op1=ALU.mult),
                            reads=[pk, csk], writes=["t2%d" % b2])
                        p2 = next_ps()
                        if p2 == p:
                            p2 = next_ps()
                        pk2 = "psA%d" % p2
                        S.op("tensor", lambda e: e.matmul(psA[p2][:], perm_bf[:], qbf[b2][:], start=True, stop=True),
                             reads=["perm_bf", "qbf%d" % b2], writes=[pk2])
                        S.op("vector", lambda e: e.tensor_tensor(
                            out=t1[b2][:], in0=psA[p2][:], in1=sn[i % 2][:], op=ALU.mult),
                            reads=[pk2, snk], writes=["t1%d" % b2])
                        S.op("gpsimd", lambda e: e.tensor_tensor(out=qr[b2][:], in0=t1[b2][:], in1=t2[b2][:], op=ALU.add),
                             reads=["t1%d" % b2, "t2%d" % b2], writes=["qr%d" % b2])
                        dst = QT if isq else KT
                        S.op("sync", lambda e: e.dma_start(
                            out=dst[c, :, i * 512:(i + 1) * 512], in_=qr[b2][:]),
                            reads=["qr%d" % b2], writes=["QK_dram"], dma="qr%d" % b2)

                    pend = None
                    for qi in range(8):
                        p_ = qk_main(qi)
                        if pend is not None:
                            qk_rope(*pend)
                        pend = (qi, p_)
                    if i + 1 < NG:
                        a_load(i + 1)
                    vb = i % 2
                    for sub in range(4):
                        p = next_ps()
                        pk = "psA%d" % p
                        for dc in range(8):
                            S.op("tensor", lambda e, p=p, dc=dc, sub=sub, xti=xti: e.matmul(
                                psA[p][:], xti[:, dc, sub * 128:(sub + 1) * 128], win[:, dc, 1024:1536], start=(dc == 0), stop=(dc == 7)),
                                reads=["win", xk], writes=[pk])
                        S.op("scalar", lambda e, p=p, sub=sub, vb=vb: e.copy(vsb[vb][:, sub, :], psA[p][:]),
                             reads=[pk], writes=["vsb%d" % vb])
                    S.op("sync", lambda e, vb=vb, i=i: e.dma_start(
                        out=Vd[i * 512:(i + 1) * 512, :].rearrange("(s p) f -> p s f", p=128), in_=vsb[vb][:]),
                        reads=["vsb%d" % vb], writes=["V_dram"], dma="vsb%d" % vb)
                    qk_rope(*pend)
                    for sub in range(4):
                        p = next_ps()
                        pk = "psA%d" % p
                        for dc in range(8):
                            S.op("tensor", lambda e, p=p, dc=dc, sub=sub, xti=xti: e.matmul(
                                psA[p][:, 0:256], xti[:, dc, sub * 128:(sub + 1) * 128], win[:, dc, 1792:2048], start=(dc == 0), stop=(dc == 7)),
                                reads=["win", xk], writes=[pk])
                        zb = sub % 2
                        zk = "zg%d" % zb
                        gelu(psA[p][:, 0:256], pk, zg[zb][:], zk, 128, 256, gcount[0] % 2)
                        gcount[0] += 1
                        z3 = zg[zb][:].rearrange("p (g c) -> p g c", g=4)
                        S.op("vector", lambda e, z3=z3: e.tensor_reduce(zst[:, 0:4], z3, axis=AX.X, op=ALU.add), reads=[zk], writes=["zst"])
                        S.op("gpsimd", lambda e, zb=zb: e.tensor_tensor(out=zsq[:], in0=zg[zb][:], in1=zg[zb][:], op=ALU.mult), reads=[zk], writes=["zsq"])
                        S.op("vector", lambda e: e.tensor_reduce(zst[:, 4:8], zsq[:].rearrange("p (g c) -> p g c", g=4), axis=AX.X, op=ALU.add),
                             reads=["zsq"], writes=["zst"])
                        S.op("vector", lambda e: e.tensor_scalar(zst[:, 0:4], zst[:, 0:4], 1.0 / 64, None, ALU.mult), reads=["zst"], writes=["zst"])
                        S.op("vector", lambda e: e.tensor_tensor(out=zst[:, 8:12], in0=zst[:, 0:4], in1=zst[:, 0:4], op=ALU.mult), reads=["zst"], writes=["zst"])
                        S.op("vector", lambda e: e.scalar_tensor_tensor(out=zst[:, 4:8], in0=zst[:, 4:8], scalar=1.0 / 64, in1=zst[:, 8:12],
                                                                        op0=ALU.mult, op1=ALU.subtract), reads=["zst"], writes=["zst"])
                        S.op("scalar", lambda e: e.activation(out=zst[:, 4:8], in_=zst[:, 4:8], func=AF.Sqrt, bias=epsc[:], scale=1.0),
                             reads=["zst", "epsc"], writes=["zst"])
                        S.op("vector", lambda e: e.reciprocal(zst[:, 4:8], zst[:, 4:8]), reads=["zst"], writes=["zst"])
                        S.op("vector", lambda e, z3=z3: e.tensor_tensor(out=z3, in0=z3, in1=zst[:, 0:4].unsqueeze(2).to_broadcast([128, 4, 64]), op=ALU.subtract),
                             reads=[zk, "zst"], writes=[zk])
                        S.op("vector", lambda e, z3=z3, sub=sub: e.tensor_tensor(out=zn[sub][:], in0=z3, in1=zst[:, 4:8].unsqueeze(2).to_broadcast([128, 4, 64]), op=ALU.mult),
                             reads=[zk, "zst"], writes=["zn%d" % sub])
                    for g in range(4):
                        p = next_ps()
                        pk = "psA%d" % p
                        for dc in range(8):
                            S.op("tensor", lambda e, p=p, dc=dc, g=g, xti=xti: e.matmul(
                                psA[p][0:64, :], win[:, dc, 1536 + 64 * g:1536 + 64 * (g + 1)], xti[:, dc, :], start=(dc == 0), stop=(dc == 7)),
                                reads=["win", xk], writes=[pk])
                        gelu(psA[p][0:64, :], pk, ug[g][:], "ug%d" % g, 64, 512, gcount[0] % 2)
                        gcount[0] += 1
                    for cc in range(2):
                        pgb, pgc, ph_ = next_ps(), next_ps(), next_ps()
                        for (p, col0) in ((pgb, 2048), (pgc, 2304), (ph_, 2560)):
                            for dc in range(8):
                                S.op("tensor", lambda e, p=p, dc=dc, col0=col0, cc=cc, xti=xti: e.matmul(
                                    psA[p][:], win[:, dc, col0 + cc * 128:col0 + (cc + 1) * 128], xti[:, dc, :], start=(dc == 0), stop=(dc == 7)),
                                    reads=["win", xk], writes=["psA%d" % p])
                        S.op("scalar", lambda e, cc=cc, ph_=ph_: e.copy(hs[cc][:], psA[ph_][:]), reads=["psA%d" % ph_], writes=["hs%d" % cc])
                        S.op("scalar", lambda e, cc=cc, pgb=pgb: e.copy(gbs[cc][:], psA[pgb][:]), reads=["psA%d" % pgb], writes=["gbs%d" % cc])
                        zk = "zce%d" % cc
                        if i > 0:
                            S.op("gpsimd", lambda e, cc=cc: e.tensor_copy(zce[:, cc, 0:2], zce[:, cc, 512:514]), reads=[zk, "zce"], writes=[zk])
                        S.op("vector", lambda e, cc=cc, pgc=pgc: e.tensor_tensor(out=zce[:, cc, 2:514], in0=psA[pgc][:], in1=hs[cc][:], op=ALU.mult),
                             reads=["psA%d" % pgc, "hs%d" % cc, "zce"], writes=[zk])
                        S.op("scalar", lambda e, cc=cc: e.activation(out=yc[cc][:], in_=zce[:, cc, 2:514], func=AF.Identity, bias=zeroc[:], scale=cw[:, cc, 2:3]),
                             reads=[zk, "cw", "zeroc"], writes=["yc%d" % cc])
                        S.op("vector", lambda e, cc=cc: e.scalar_tensor_tensor(out=yc[cc][:], in0=zce[:, cc, 1:513], scalar=cw[:, cc, 1:2], in1=yc[cc][:],
                                                                               op0=ALU.mult, op1=ALU.add), reads=[zk, "cw", "yc%d" % cc], writes=["yc%d" % cc])
                        S.op("vector", lambda e, cc=cc: e.scalar_tensor_tensor(out=yc[cc][:], in0=zce[:, cc, 0:512], scalar=cw[:, cc, 0:1], in1=yc[cc][:],
                                                                               op0=ALU.mult, op1=ALU.add), reads=[zk, "cw", "yc%d" % cc], writes=["yc%d" % cc])
                        S.op("gpsimd", lambda e, cc=cc: e.tensor_tensor(out=ycb[cc][:], in0=gbs[cc][:], in1=yc[cc][:], op=ALU.mult),
                             reads=["gbs%d" % cc, "yc%d" % cc], writes=["ycb%d" % cc])
                        S.op("sync", lambda e, cc=cc, i=i: e.dma_start(out=mixT[6 + cc, :, i * 512:(i + 1) * 512], in_=ycb[cc][:]),
                             reads=["ycb%d" % cc], writes=["mix_dram"], dma="ycb%d" % cc)
                    for g in range(4):
                        p = next_ps()
                        pk = "psA%d" % p
                        for sub in range(4):
                            S.op("tensor", lambda e, p=p, g=g, sub=sub: e.matmul(
                                psA[p][0:64, sub * 128:(sub + 1) * 128], zn[sub][:, g, :], wsT[:, g, :], start=True, stop=True),
                                reads=["zn%d" % sub, "wsT"], writes=[pk])
                        sb2 = g % 2
                        S.op("vector", lambda e, p=p, g=g, sb2=sb2: e.scalar_tensor_tensor(
                            out=spv[sb2][:].rearrange("p (s t) -> p s t", s=4), in0=psA[p][0:64, :].rearrange("p (s t) -> p s t", s=4),
                            scalar=sgain[:, g:g + 1], in1=bsb[:, g:g + 1, :].to_broadcast([64, 4, 128]), op0=ALU.mult, op1=ALU.add),
                            reads=[pk, "sgain", "bsb"], writes=["spv%d" % sb2])
                        S.op("gpsimd", lambda e, g=g, sb2=sb2: e.tensor_tensor(out=yb[sb2][:], in0=spv[sb2][:], in1=ug[g][:], op=ALU.mult),
                             reads=["spv%d" % sb2, "ug%d" % g], writes=["yb%d" % sb2])
                        S.op("sync", lambda e, g=g, sb2=sb2, i=i: e.dma_start(
                            out=mixT[4 + g // 2, (g % 2) * 64:(g % 2) * 64 + 64, i * 512:(i + 1) * 512], in_=yb[sb2][:]),
                            reads=["yb%d" % sb2], writes=["mix_dram"], dma="yb%d" % sb2)
                S.barrier()
                S.emit()

            with ExitStack() as ph:
                qn = SB(ph, "b_qn", [128, S_LEN], BF16)
                kn = SB(ph, "b_kn", [128, S_LEN], BF16)
                qd = SB(ph, "b_qd", [128, S_LEN], BF16)
                kd = SB(ph, "b_kd", [128, S_LEN], BF16)
                vas = [SB(ph, "b_va%d" % i, [128, NT, 2, 65], BF16) for i in range(2)]
                acc = SB(ph, "b_acc", [65, 2, S_LEN], F32)
                pt = [SB(ph, "b_pt%d" % i, [128, 2, 256], BF16) for i in range(4)]
                m01 = SB(ph, "b_m01", [128, 2, 256], BF16)
                rec = [SB(ph, "b_rec%d" % i, [64, 512], F32) for i in range(2)]
                yaT = [SB(ph, "b_yaT%d" % i, [64, 2048], BF16) for i in range(2)]
                sel_f = cst[0:65, C_SEL:C_SEL + 64]
                ps_s = [PS(ph, "b_pss%d" % i, [128, 2, 256]) for i in range(3)]
                ps_o = [PS(ph, "b_pso%d" % i, [65, 512]) for i in range(3)]
                ps_d = [PS(ph, "b_psd%d" % i, [64, 512]) for i in range(2)]
                for s_ in range(2):
                    S.op("vector", lambda e, s_=s_: e.tensor_scalar(m01[:, s_, :], cst[:, C_MASK:C_MASK + 256], 0.0, None, ALU.is_equal),
                         reads=["cst"], writes=["m01"])
                S.op("vector", lambda e: e.memset(vas[0][:], 1.0), writes=["va0"])
                S.op("gpsimd", lambda e: e.memset(vas[1][:], 1.0), writes=["va1"])
                pcount = [0]

                def load_v(job):
                    c_, bi_ = divmod(job, 3)
                    d_ = BRANCH_D[bi_]
                    nb_ = NT // d_
                    vi = job % 2
                    vsrc = Vd.rearrange("(n j r) f -> j r n f", j=128, r=d_)
                    for r in range(d_):
                        for h2 in range(2):
                            S.op("sync", lambda e, r=r, h2=h2: e.dma_start(
                                out=vas[vi][:, r * nb_:(r + 1) * nb_, h2, 0:64],
                                in_=vsrc[:, r, :, c_ * 128 + h2 * 64:c_ * 128 + (h2 + 1) * 64]),
                                reads=["V_dram"], writes=["va%d" % vi], dma="va%d" % vi)

                load_v(0)

                def load_qk(c_):
                    S.op("sync", lambda e: e.dma_start(out=qn[:], in_=QT[c_]), reads=["QK_dram"], writes=["qn"], dma="qn")
                    S.op("sync", lambda e: e.dma_start(out=kn[:], in_=KT[c_]), reads=["QK_dram"], writes=["kn"], dma="kn")

                def deint(d_):
                    S.op("vector", lambda e: e.tensor_copy(qd[:].rearrange("p (r m) -> p r m", r=d_),
                                                           qn[:].rearrange("p (m r) -> p r m", r=d_)), reads=["qn"], writes=["qd"])
                    S.op("gpsimd", lambda e: e.tensor_copy(kd[:].rearrange("p (r m) -> p r m", r=d_),
                                                           kn[:].rearrange("p (m r) -> p r m", r=d_)), reads=["kn"], writes=["kd"])

                load_qk(0)
                for c in range(4):
                    for bi, d in enumerate(BRANCH_D):
                        nb = NT // d
                        if bi == 0:
                            deint(4)
                            qs, ks, qsk, ksk = qn, kn, "qn", "kn"
                        elif bi == 1:
                            qs, ks, qsk, ksk = qd, kd, "qd", "kd"
                        else:
                            qs, ks, qsk, ksk = qd, kd, "qd", "kd"
                            deint(16)
                            if c + 1 < 4:
                                load_qk(c + 1)
                        job = c * 3 + bi
                        if job + 1 < 12:
                            load_v(job + 1)
                        va = vas[job % 2]
                        vak = "va%d" % (job % 2)
                        for hh in range(2):
                            P0 = hh * 64
                            started = {}

                            def emit_scores(kp, P0=P0, qs=qs, ks=ks, qsk=qsk, ksk=ksk, nb=nb):
                                sb_i = kp % 3
                                sk = "pss%d" % sb_i
                                for T in (2 * kp, 2 * kp + 1):
                                    n = T % nb
                                    N = 256 if n < nb - 1 else 128
                                    slot = T % 2
                                    S.op("tensor", lambda e, sb_i=sb_i, slot=slot, N=N, T=T: e.matmul(
                                        ps_s[sb_i][:, slot, 0:N], ks[P0:P0 + 64, T * 128:(T + 1) * 128], qs[P0:P0 + 64, T * 128:T * 128 + N],
                                        start=True, stop=True, skip_group_check=True), reads=[ksk, qsk], writes=[sk])
                                pi = pcount[0] % 4
                                pcount[0] += 1
                                S.op("scalar", lambda e, sb_i=sb_i, pi=pi: e.activation(out=pt[pi][:], in_=ps_s[sb_i][:], func=AF.Exp,
                                                                                        bias=zeroc[:], scale=1.0),
                                     reads=[sk, "zeroc"], writes=["pt%d" % pi])
                                meng = "vector" if (kp % 3 == 0) else "gpsimd"
                                S.op(meng, lambda e, pi=pi: e.tensor_tensor(out=pt[pi][:], in0=pt[pi][:], in1=m01[:], op=ALU.mult),
                                     reads=["pt%d" % pi, "m01"], writes=["pt%d" % pi])
                                return pi

                            def emit_pv(kp, pi, hh=hh, nb=nb, d=d, bi=bi, started=started, va=va, vak=vak):
                                ptk = "pt%d" % pi
                                for T2 in (2 * kp, 2 * kp + 1):
                                    n2 = T2 % nb
                                    N2 = 256 if n2 < nb - 1 else 128
                                    sl2 = T2 % 2
                                    if N2 == 256 and (T2 % 4) != 3:
                                        pieces = [(T2, 0, 256)]
                                    else:
                                        pieces = [(T2, 0, 128)]
                                        if N2 == 256:
                                            pieces.append((T2 + 1, 128, 128))
                                    for (qb, off, wdt) in pieces:
                                        B = qb // 4
                                        ob = B % 3
                                        first = B not in started
                                        started[B] = True
                                        S.op("tensor", lambda e, ob=ob, qb=qb, off=off, wdt=wdt, T2=T2, sl2=sl2, first=first: e.matmul(
                                            ps_o[ob][:, (qb % 4) * 128:(qb % 4) * 128 + wdt], va[:, T2, hh, :], pt[pi][:, sl2, off:off + wdt],
                                            start=first, stop=False, skip_group_check=True),
                                            reads=[vak, ptk], writes=["pso%d" % ob])
                                    if T2 % 4 == 3:
                                        B = T2 // 4
                                        ob = B % 3
                                        pos0 = B * 512
                                        r = pos0 // (S_LEN // d)
                                        m0 = pos0 % (S_LEN // d)
                                        dstv = acc[:, hh, :].rearrange("p (m r) -> p r m", r=d)[:, r, m0:m0 + 512]
                                        if bi == 0:
                                            S.op("vector", lambda e, ob=ob, dstv=dstv: e.tensor_copy(dstv, ps_o[ob][:]),
                                                 reads=["pso%d" % ob], writes=["acc%d" % hh])
                                        else:
                                            S.op("vector", lambda e, ob=ob, dstv=dstv: e.tensor_tensor(out=dstv, in0=ps_o[ob][:], in1=dstv, op=ALU.add),
                                                 reads=["pso%d" % ob, "acc%d" % hh], writes=["acc%d" % hh])

                            hist = []
                            for kp in range(NT // 2):
                                pi = emit_scores(kp)
                                hist.append((kp, pi))
                                if len(hist) > 2:
                                    emit_pv(*hist[-3])
                            emit_pv(*hist[-2])
                            emit_pv(*hist[-1])
                    for hh in range(2):
                        for B in range(NG):
                            di = B % 2
                            S.op("tensor", lambda e, di=di, hh=hh, B=B: e.matmul(ps_d[di][:], sel_f, acc[:, hh, B * 512:(B + 1) * 512], start=True, stop=True),
                                 reads=["cst", "acc%d" % hh], writes=["psd%d" % di])
                            S.op("vector", lambda e, di=di: e.reciprocal(rec[di][:], ps_d[di][:]), reads=["psd%d" % di], writes=["rec%d" % di])
                            yi_ = (B // 4) % 2
                            S.op("gpsimd", lambda e, di=di, hh=hh, B=B, yi_=yi_: e.tensor_tensor(out=yaT[yi_][:, (B % 4) * 512:(B % 4 + 1) * 512], in0=acc[0:64, hh, B * 512:(B + 1) * 512],
                                                                                                  in1=rec[di][:], op=ALU.mult),
                                 reads=["rec%d" % di, "acc%d" % hh], writes=["yaT%d" % yi_])
                            if B % 4 == 3:
                                S.op("sync", lambda e, c=c, hh=hh, B=B, yi_=yi_: e.dma_start(
                                    out=mixT[c, hh * 64:(hh + 1) * 64, (B // 4) * 2048:(B // 4 + 1) * 2048], in_=yaT[yi_][:]),
                                    reads=["yaT%d" % yi_], writes=["mix_dram"], dma="yaT%d" % yi_)
                S.barrier()
                S.emit()

            with ExitStack() as ph:
                wo_f = SB(ph, "c_wof", [128, DM], F32)
                wo = SB(ph, "c_wo", [128, 8, DM], BF16)
                bg = SB(ph, "c_bg", [128, 8], F32)
                lng = SB(ph, "c_lng", [128, DM], F32)
                lnb = SB(ph, "c_lnb", [128, DM], F32)
                wr_f = SB(ph, "c_wrf", [128, 8, 36], F32)
                wr = SB(ph, "c_wr", [128, 8, 36], BF16)
                rb = SB(ph, "c_rb", [128, 36], F32)
                mx = [SB(ph, "c_mx%d" % i, [128, 8, 512], BF16) for i in range(2)]
                sq = SB(ph, "c_sq", [128, 8, 512], BF16)
                xa = [SB(ph, "c_xa%d" % i, [128, DM], F32) for i in range(4)]
                accs = [SB(ph, "c_acc%d" % i, [128, DM], F32) for i in range(2)]
                junk = SB(ph, "c_junk", [128, DM], F32)
                st = [SB(ph, "c_st%d" % i, [128, 16], F32) for i in range(2)]
                rs3 = [SB(ph, "c_rs3%d" % i, [128, 3], F32) for i in range(2)]
                bst = [SB(ph, "c_bst%d" % i, [128, 2, 6], F32) for i in range(2)]
                x1 = [SB(ph, "c_x1%d" % i, [128, DM], F32) for i in range(2)]
                x1b = [SB(ph, "c_x1b%d" % i, [128, DM], BF16) for i in range(2)]
                x1T = SB(ph, "c_x1T", [128, 8, 128], BF16)
                lg = [SB(ph, "c_lg%d" % i, [128, 36], F32) for i in range(2)]
                rt = SB(ph, "c_rt", [128, 48], F32)
                elm = SB(ph, "c_elm", [128, NE], F32)
                top8 = SB(ph, "c_top8", [128, 8], F32)
                Abf = SB(ph, "c_Abf", [128, NE], BF16)
                rk = SB(ph, "c_rk", [128, NE], F32)
                tmp32 = SB(ph, "c_tmp32", [128, NE], F32)
                base = SB(ph, "c_base", [128, NE], F32)
                ps_z = [PS(ph, "c_psz%d" % i, [128, DM]) for i in range(2)]
                ps_ss = PS(ph, "c_psss", [128, 512])
                ps_tr = PS(ph, "c_pstr", [128, 8, 128], BF16)
                ps_lg = PS(ph, "c_pslg", [128, 512])
                ps_cnt = PS(ph, "c_pscnt", [128, 512])

                S.op("sync", lambda e: e.dma_start(out=bg[:], in_=branch_gain[l].rearrange("(c p) -> p c", p=128), allow_slow_non_contiguous=True),
                     writes=["bg"], dma="bg")
                for ch in range(8):
                    S.op("sync", lambda e, ch=ch: e.dma_start(out=wo_f[:], in_=w_out[l, ch * 128:(ch + 1) * 128, :]), writes=["wo_f"], dma="wo_f")
                    S.op("vector", lambda e, ch=ch: e.tensor_scalar(wo[:, ch, :], wo_f[:], bg[:, ch:ch + 1], None, ALU.mult),
                         reads=["wo_f", "bg"], writes=["wo"])
                S.op("sync", lambda e: e.dma_start(out=lng[:], in_=ln_gain[l, 0:1, :].partition_broadcast(128)), writes=["lng"], dma="lng")
                S.op("sync", lambda e: e.dma_start(out=lnb[:], in_=ln_bias[l, 0:1, :].partition_broadcast(128)), writes=["lnb"], dma="lnb")
                S.op("sync", lambda e: e.dma_start(out=wr_f[:, :, 0:4], in_=rgw[l].rearrange("(c p) g -> p c g", p=128), allow_slow_non_contiguous=True),
                     writes=["wr_f"], dma="wr_f")
                S.op("sync", lambda e: e.dma_start(out=wr_f[:, :, 4:36], in_=rew[l].rearrange("(c p) g -> p c g", p=128), allow_slow_non_contiguous=True),
                     writes=["wr_f"], dma="wr_f")
                S.op("vector", lambda e: e.tensor_copy(wr[:], wr_f[:]), reads=["wr_f"], writes=["wr"])
                S.op("sync", lambda e: e.dma_start(out=rb[:, 0:4], in_=rgb[l:l + 1, :].partition_broadcast(128)), writes=["rb"], dma="rb")
                S.op("sync", lambda e: e.dma_start(out=rb[:, 4:36], in_=reb[l:l + 1, :].partition_broadcast(128)), writes=["rb"], dma="rb")
                S.op("vector", lambda e: e.memset(base[:], 0.0), writes=["base"])

                parts = ((0, 4), (4, 6), (6, 8))

                def c_s0(tt):
                    g, sub = divmod(tt, 4)
                    if sub == 0:
                        mk = "mx%d" % (g % 2)
                        S.op("sync", lambda e, g=g: e.dma_start(out=mx[g % 2][:], in_=mixT.rearrange("c p t -> p c t")[:, :, g * 512:(g + 1) * 512]),
                             reads=["mix_dram"], writes=[mk], dma=mk)
                    b4 = tt % 4
                    S.op("sync", lambda e, tt=tt, b4=b4: e.dma_start(out=xa[b4][:], in_=Xin[tt * 128:(tt + 1) * 128, :]),
                         writes=["xa%d" % b4], dma="xa%d" % b4)

                def c_s0b(tt):
                    g, sub = divmod(tt, 4)
                    mk = "mx%d" % (g % 2)
                    mxg = mx[g % 2]
                    b2 = tt % 2
                    tok = slice(sub * 128, (sub + 1) * 128)
                    rk_ = "rs3_%d" % b2
                    if sub == 0:
                        S.op("scalar", lambda e: e.activation(out=sq[:], in_=mxg[:], func=AF.Square, bias=zeroc[:], scale=1.0),
                             reads=[mk, "zeroc"], writes=["sq"])
                    for pi_, (c0, c1) in enumerate(parts):
                        for ch in range(c0, c1):
                            S.op("tensor", lambda e, pi_=pi_, ch=ch, c0=c0, c1=c1: e.matmul(
                                ps_ss[:, pi_:pi_ + 1], sq[:, ch, tok], ones_bf[:, 0:1], start=(ch == c0), stop=(ch == c1 - 1), skip_group_check=True),
                                reads=["sq", "ones_bf"], writes=["ps_ss"])
                    S.op("vector", lambda e: e.tensor_tensor(out=rs3[b2][:], in0=ps_ss[:, 0:3], in1=cst[:, C_INVW:C_INVW + 3], op=ALU.mult),
                         reads=["ps_ss", "cst"], writes=[rk_])
                    S.op("scalar", lambda e: e.activation(out=rs3[b2][:], in_=rs3[b2][:], func=AF.Sqrt, bias=epsA2[:], scale=ALPHA * ALPHA),
                         reads=[rk_, "epsA2"], writes=[rk_])
                    S.op("vector", lambda e: e.reciprocal(rs3[b2][:], rs3[b2][:]), reads=[rk_], writes=[rk_])

                def c_s1(tt):
                    g, sub = divmod(tt, 4)
                    mk = "mx%d" % (g % 2)
                    mxg = mx[g % 2]
                    b2 = tt % 2
                    b4 = tt % 4
                    stt = st[b2]
                    sk_ = "st_%d" % b2
                    rk_ = "rs3_%d" % b2
                    tok = slice(sub * 128, (sub + 1) * 128)
                    ak = "accs%d" % b2
                    for pi_, (c0, c1) in enumerate(parts):
                        zi = (tt * 3 + pi_) % 2
                        zk = "psz%d" % zi
                        for half in range(2):
                            for ch in range(c0, c1):
                                S.op("tensor", lambda e, zi=zi, half=half, ch=ch, c0=c0, c1=c1: e.matmul(
                                    ps_z[zi][:, half * 512:(half + 1) * 512], mxg[:, ch, tok], wo[:, ch, half * 512:(half + 1) * 512],
                                    start=(ch == c0), stop=(ch == c1 - 1)), reads=[mk, "wo"], writes=[zk])
                        if pi_ == 0:
                            S.op("vector", lambda e, zi=zi, pi_=pi_: e.scalar_tensor_tensor(
                                out=accs[b2][:], in0=ps_z[zi][:], scalar=rs3[b2][:, pi_:pi_ + 1], in1=xa[b4][:], op0=ALU.mult, op1=ALU.add),
                                reads=[zk, rk_, "xa%d" % b4], writes=[ak])
                        else:
                            S.op("vector", lambda e, zi=zi, pi_=pi_: e.scalar_tensor_tensor(
                                out=accs[b2][:], in0=ps_z[zi][:], scalar=rs3[b2][:, pi_:pi_ + 1], in1=accs[b2][:], op0=ALU.mult, op1=ALU.add),
                                reads=[zk, rk_, ak], writes=[ak])
                    S.op("vector", lambda e: e.bn_stats(bst[b2][:, 0, :], accs[b2][:, 0:512]), reads=[ak], writes=["bst%d" % b2])
                    S.op("vector", lambda e: e.bn_stats(bst[b2][:, 1, :], accs[b2][:, 512:1024]), reads=[ak], writes=["bst%d" % b2])
                    S.op("vector", lambda e: e.bn_aggr(stt[:, 0:2], bst[b2][:]), reads=["bst%d" % b2], writes=[sk_])
                    S.op("scalar", lambda e: e.activation(out=stt[:, 2:3], in_=stt[:, 1:2], func=AF.Sqrt, bias=epsLN[:], scale=1.0), reads=[sk_, "epsLN"], writes=[sk_])
                    S.op("vector", lambda e: e.reciprocal(stt[:, 2:3], stt[:, 2:3]), reads=[sk_], writes=[sk_])
                    S.op("vector", lambda e: e.scalar_tensor_tensor(out=stt[:, 3:4], in0=stt[:, 0:1], scalar=-1.0, in1=stt[:, 2:3], op0=ALU.mult, op1=ALU.mult),
                         reads=[sk_], writes=[sk_])

                def c_s2(tt):
                    b2 = tt % 2
                    stt = st[b2]
                    sk_ = "st_%d" % b2
                    ak = "accs%d" % b2
                    x1k = "x1%d" % b2
                    S.op("scalar", lambda e: e.activation(out=x1[b2][:], in_=accs[b2][:], func=AF.Identity, bias=stt[:, 3:4], scale=stt[:, 2:3]),
                         reads=[ak, sk_], writes=[x1k])
                    S.op("vector", lambda e: e.tensor_tensor(out=x1[b2][:], in0=x1[b2][:], in1=lng[:], op=ALU.mult), reads=[x1k, "lng"], writes=[x1k])
                    S.op("gpsimd", lambda e: e.tensor_tensor(out=x1[b2][:], in0=x1[b2][:], in1=lnb[:], op=ALU.add), reads=[x1k, "lnb"], writes=[x1k])
                    S.op("sync", lambda e: e.dma_start(out=X1[tt * 128:(tt + 1) * 128, :], in_=x1[b2][:]), reads=[x1k], writes=["X1_dram"], dma="x1o%d" % b2)
                    xbk = "x1b%d" % b2
                    S.op("gpsimd", lambda e: e.tensor_copy(x1b[b2][:], x1[b2][:]), reads=[x1k], writes=[xbk])
                    S.op("sync", lambda e: e.dma_start(out=X1b[tt * 128:(tt + 1) * 128, :], in_=x1b[b2][:]), reads=[xbk], writes=["X1b_dram"], dma="x1bo%d" % b2)

                def c_s2b(tt):
                    b2 = tt % 2
                    xbk = "x1b%d" % b2
                    for j in range(8):
                        S.op("tensor", lambda e, j=j: e.transpose(ps_tr[:, j, :], x1b[b2][:, j * 128:(j + 1) * 128], ident_bf[:]),
                             reads=[xbk, "ident_bf"], writes=["ps_tr"])
                    S.op("scalar", lambda e: e.copy(x1T[:], ps_tr[:]), reads=["ps_tr"], writes=["x1T"])
                    for j in range(8):
                        S.op("tensor", lambda e, j=j: e.matmul(ps_lg[:, 0:36], x1T[:, j, :], wr[:, j, :], start=(j == 0), stop=(j == 7), skip_group_check=True),
                             reads=["x1T", "wr"], writes=["ps_lg"])
                    S.op("vector", lambda e: e.tensor_tensor(out=lg[b2][:], in0=ps_lg[:, 0:36], in1=rb[:], op=ALU.add), reads=["ps_lg", "rb"], writes=["lg%d" % b2])

                def c_s3(tt):
                    b2 = tt % 2
                    lgt = lg[b2]
                    lk = "lg%d" % b2
                    S.op("vector", lambda e: e.reduce_max(rt[:, 0:1], lgt[:, 0:4], axis=AX.X), reads=[lk], writes=["rt"])
                    S.op("vector", lambda e: e.tensor_scalar(rt[:, 1:2], rt[:, 0:1], -1.0, None, ALU.mult), reads=["rt"], writes=["rt"])
                    S.op("vector", lambda e: e.memset(rt[:, 2:3], 0.0), reads=["rt"], writes=["rt"])
                    S.op("scalar", lambda e: e.activation(out=rt[:, 4:8], in_=lgt[:, 0:4], func=AF.Exp, bias=rt[:, 1:2], scale=1.0, accum_out=rt[:, 2:3]),
                         reads=[lk, "rt"], writes=["rt"])
                    S.op("vector", lambda e: e.reciprocal(rt[:, 3:4], rt[:, 2:3]), reads=["rt"], writes=["rt"])
                    S.op("vector", lambda e: e.tensor_scalar(rt[:, 8:12], lgt[:, 0:4], rt[:, 0:1], None, ALU.is_equal), reads=[lk, "rt"], writes=["rt"])
                    S.op("vector", lambda e: e.tensor_scalar(rt[:, 12:16], rt[:, 8:12], 1e30, -1e30, ALU.mult, ALU.add), reads=["rt"], writes=["rt"])
                    S.op("vector", lambda e: e.tensor_tensor(out=elm[:].rearrange("p (g j) -> p g j", g=4), in0=lgt[:, 4:36].rearrange("p (g j) -> p g j", g=4),
                                                             in1=rt[:, 12:16].unsqueeze(2).to_broadcast([128, 4, 8]), op=ALU.add),
                         reads=[lk, "rt"], writes=["elm"])
                    S.op("vector", lambda e: e.max(out=top8[:], in_=elm[:]), reads=["elm"], writes=["top8"])
                    S.op("vector", lambda e: e.tensor_scalar(M1all[:, tt, :], elm[:], top8[:, 0:1], None, ALU.is_equal), reads=["elm", "top8"], writes=["M1"])
                    S.op("vector", lambda e: e.tensor_scalar(M2all[:, tt, :], elm[:], top8[:, 1:2], None, ALU.is_equal), reads=["elm", "top8"], writes=["M2"])
                    S.op("vector", lambda e: e.tensor_tensor(out=rt[:, 16:17], in0=top8[:, 0:1], in1=top8[:, 1:2], op=ALU.subtract), reads=["top8"], writes=["rt"])
                    S.op("vector", lambda e: e.tensor_tensor(out=rt[:, 17:18], in0=top8[:, 1:2], in1=top8[:, 0:1], op=ALU.subtract), reads=["top8"], writes=["rt"])
                    S.op("scalar", lambda e: e.activation(out=rt[:, 18:20], in_=rt[:, 16:18], func=AF.Sigmoid, bias=zeroc[:], scale=1.0), reads=["rt", "zeroc"], writes=["rt"])
                    S.op("vector", lambda e: e.tensor_scalar(W01[:, tt, :], rt[:, 18:20], rt[:, 3:4], 1.0 / ALPHA, ALU.mult, ALU.mult), reads=["rt"], writes=["W01"])
                    S.op("vector", lambda e: e.tensor_tensor(out=Abf[:], in0=M1all[:, tt, :], in1=M2all[:, tt, :], op=ALU.add), reads=["M1", "M2"], writes=["Abf"])
                    S.op("tensor", lambda e: e.matmul(ps_lg[:, 64:96], ls_bf[:], Abf[:], start=True, stop=True, skip_group_check=True),
                         reads=["ls_bf", "Abf"], writes=["ps_lg"])
                    S.op("tensor", lambda e: e.matmul(ps_lg[:, 96:128], ones_bf[:], Abf[:], start=False, stop=True, skip_group_check=True),
                         reads=["ones_bf", "Abf"], writes=["ps_lg"])
                    S.op("tensor", lambda e: e.matmul(ps_cnt[0:32, 0:1], Abf[:], ones_bf[:, 0:1], start=(tt == 0), stop=(tt == NT - 1), skip_group_check=True),
                         reads=["Abf", "ones_bf"], writes=["ps_cnt"])
                    S.op("vector", lambda e: e.tensor_tensor(out=rk[:], in0=ps_lg[:, 64:96], in1=base[:], op=ALU.add), reads=["ps_lg", "base"], writes=["rk"])
                    S.op("vector", lambda e: e.tensor_tensor(out=base[:], in0=ps_lg[:, 96:128], in1=base[:], op=ALU.add), reads=["ps_lg", "base", "rk"], writes=["base"])
                    S.op("vector", lambda e: e.tensor_tensor(out=tmp32[:], in0=rk[:], in1=M1all[:, tt, :], op=ALU.mult), reads=["rk", "M1"], writes=["tmp32"])
                    S.op("vector", lambda e: e.reduce_sum(RK[:, tt, 0:1], tmp32[:], axis=AX.X), reads=["tmp32"], writes=["RK"])
                    S.op("vector", lambda e: e.tensor_tensor(out=tmp32[:], in0=rk[:], in1=M2all[:, tt, :], op=ALU.mult), reads=["rk", "M2"], writes=["tmp32"])
                    S.op("vector", lambda e: e.reduce_sum(RK[:, tt, 1:2], tmp32[:], axis=AX.X), reads=["tmp32"], writes=["RK"])

                c_s0(0)
                c_s0(1)
                c_s0b(0)
                for step in range(NT + 3):
                    if step + 2 < NT:
                        c_s0(step + 2)
                    if step + 1 < NT:
                        c_s0b(step + 1)
                    if step < NT:
                        c_s1(step)
                    if 0 <= step - 1 < NT:
                        c_s2(step - 1)
                    if 0 <= step - 2 < NT:
                        c_s2b(step - 2)
                    if 0 <= step - 3 < NT:
                        c_s3(step - 3)

                cntT = SB(ph, "c_cntT", [32, 1], F32)
                cmp_ = SB(ph, "c_cmp", [32, 33], F32)
                nblkT = SB(ph, "c_nblkT", [32, 1], F32)
                nbl_b = SB(ph, "c_nblb", [32, 128], F32)
                pe_ps = SB(ph, "c_peps", [128, 64], F32)
                cmp2 = SB(ph, "c_cmp2", [128, NBLK, NE], F32)
                ebf = SB(ph, "c_ebf", [128, NBLK], F32)
                ebf2 = SB(ph, "c_ebf2", [128, NBLK, 2], F32)
                trail = SB(ph, "c_trail", [128, NBLK], F32)
                pstart = SB(ph, "c_pstart", [128, NE], F32)
                big = SB(ph, "c_big", [128, NT, NE], F32)
                dstf = SB(ph, "c_dstf", [128, NT, 2], F32)
                S.op("vector", lambda e: e.tensor_copy(cntT[:], ps_cnt[0:32, 0:1]), reads=["ps_cnt"], writes=["cntT"])
                S.op("vector", lambda e: e.tensor_scalar(cmp_[:], cst[0:32, C_THR:C_THR + 33], cntT[:, 0:1], None, ALU.is_lt), reads=["cst", "cntT"], writes=["cmp_"])
                S.op("vector", lambda e: e.reduce_sum(nblkT[:], cmp_[:], axis=AX.X), reads=["cmp_"], writes=["nblkT"])
                S.op("vector", lambda e: e.tensor_copy(nbl_b[:], nblkT[:, 0:1].to_broadcast([32, 128])), reads=["nblkT"], writes=["nbl_b"])
                S.op("tensor", lambda e: e.matmul(ps_ss[:, 0:64], nbl_b[:], cst[0:32, C_UCAT:C_UCAT + 64], start=True, stop=True, skip_group_check=True),
                     reads=["nbl_b", "cst"], writes=["ps_ss"])
                S.op("vector", lambda e: e.tensor_copy(pe_ps[:], ps_ss[:, 0:64]), reads=["ps_ss"], writes=["pe_ps"])
                S.op("vector", lambda e: e.tensor_tensor(out=pstart[:], in0=pe_ps[:, 0:32], in1=pe_ps[:, 32:64], op=ALU.subtract), reads=["pe_ps"], writes=["pstart"])
                S.op("vector", lambda e: e.tensor_tensor(out=cmp2[:], in0=pe_ps[:, 0:32].unsqueeze(1).to_broadcast([128, NBLK, NE]),
                                                         in1=cst[:, C_BV:C_BV + NBLK].unsqueeze(2).to_broadcast([128, NBLK, NE]), op=ALU.is_le),
                     reads=["pe_ps", "cst"], writes=["cmp2"])
                S.op("vector", lambda e: e.tensor_reduce(ebf[:], cmp2[:], axis=AX.X, op=ALU.add), reads=["cmp2"], writes=["ebf"])
                S.op("vector", lambda e: e.tensor_scalar(trail[:], ebf[:], 31.5, 4.0e6, ALU.is_ge, ALU.mult), reads=["ebf"], writes=["trail"])
                S.op("vector", lambda e: e.tensor_scalar(ebf[:], ebf[:], 31.0, 128.0, ALU.min, ALU.mult), reads=["ebf"], writes=["ebf"])
                S.op("vector", lambda e: e.tensor_scalar(ebf[:], ebf[:], cst[:, C_IP:C_IP + 1], float(l * NE * 128), ALU.add, ALU.add), reads=["ebf", "cst"], writes=["ebf"])
                S.op("vector", lambda e: e.tensor_copy(GIDX[:], ebf[:]), reads=["ebf"], writes=["GIDX"])
                for h in range(2):
                    S.op("vector", lambda e, h=h: e.tensor_scalar(ebf2[:, :, h], ebf[:], 2.0, float(h), ALU.mult, ALU.add), reads=["ebf"], writes=["ebf2"])
                    S.op("vector", lambda e, h=h: e.tensor_tensor(out=ebf2[:, :, h], in0=ebf2[:, :, h], in1=trail[:], op=ALU.add), reads=["ebf2", "trail"], writes=["ebf2"])
                S.op("vector", lambda e: e.tensor_copy(GIDX2[:], ebf2[:]), reads=["ebf2"], writes=["GIDX2"])
                for k, Mk in enumerate((M1all, M2all)):
                    S.op("vector", lambda e, Mk=Mk: e.tensor_tensor(out=big[:], in0=Mk[:], in1=pstart[:].unsqueeze(1).to_broadcast([128, NT, NE]), op=ALU.mult),
                         reads=["M1", "M2", "pstart"], writes=["big"])
                    S.op("vector", lambda e, k=k: e.tensor_reduce(dstf[:, :, k], big[:], axis=AX.X, op=ALU.add), reads=["big"], writes=["dstf"])
                S.op("vector", lambda e: e.scalar_tensor_tensor(out=dstf[:], in0=dstf[:], scalar=float(BLK), in1=RK[:], op0=ALU.mult, op1=ALU.add),
                     reads=["dstf", "RK"], writes=["dstf"])
                S.op("vector", lambda e: e.tensor_copy(DSTi[:], dstf[:]), reads=["dstf"], writes=["DSTi"])
                S.barrier()
                S.emit()

            with ExitStack() as ph:
                xr = [SB(ph, "d_xr%d" % i, [128, DM], BF16) for i in range(8)]
                for tt in range(NT):
                    b4 = tt % 8
                    k = "xr%d" % b4
                    S.op("sync", lambda e, tt=tt, b4=b4: e.dma_start(out=xr[b4][:], in_=X1b[tt * 128:(tt + 1) * 128, :]), writes=[k], dma=k)
                    for kk in range(2):
                        S.op("gpsimd", lambda e, tt=tt, b4=b4, kk=kk: e.indirect_dma_start(
                            out=Xs, out_offset=bass.IndirectOffsetOnAxis(ap=DSTi[:, tt, kk:kk + 1], axis=0), in_=xr[b4][:], in_offset=None),
                            reads=[k], writes=["Xs_dram"], dma="sc%d" % b4)
                S.barrier()
                S.emit()

            with ExitStack() as ph:
                wg = [SB(ph, "e_wg%d" % i, [128, 8, DE], BF16) for i in range(2)]
                wu = [SB(ph, "e_wu%d" % i, [128, 8, DE], BF16) for i in range(2)]
                wd = [SB(ph, "e_wd%d" % i, [128, 4, DM], BF16) for i in range(2)]
                xs = [SB(ph, "e_xs%d" % i, [128, 4, DM], BF16) for i in range(2)]
                xsT = [SB(ph, "e_xsT%d" % i, [128, 8, 512], BF16) for i in range(2)]
                sg = [SB(ph, "e_sg%d" % i, [128, 512], F32) for i in range(2)]
                hT = [SB(ph, "e_hT%d" % i, [128, 4, 512], BF16) for i in range(2)]
                ysb = [SB(ph, "e_y%d" % i, [128, 4, DM], BF16) for i in range(2)]
                ps_t = [PS(ph, "e_pst%d" % i, [128, 8, 128], BF16) for i in range(2)]
                ps_g = [PS(ph, "e_psg%d" % i, [128, 512]) for i in range(2)]
                ps_u = [PS(ph, "e_psu%d" % i, [128, 512]) for i in range(2)]
                ps_y = [PS(ph, "e_psy%d" % i, [128, 512]) for i in range(2)]
                stg = [SB(ph, "e_stg%d" % i, [128, 2048], F32) for i in range(6)]
                wgv = ewg.rearrange("l e (p h j) f -> (l e p h) (j f)", h=2, j=4)
                wuv = ewu.rearrange("l e (p h j) f -> (l e p h) (j f)", h=2, j=4)
                wdv = ewd.rearrange("l e (p h j) n -> (l e p h) (j n)", h=2, j=2)
                piece = [0]
                yc_ = [0]
                def load_weights(b, which):
                    wb = b % 2
                    for (wt, wv, nm, jn) in which:
                        for h in range(2):
                            si = piece[0] % 6
                            ceng = ("vector", "scalar")[piece[0] % 2]
                            piece[0] += 1
                            S.op("gpsimd", lambda e, wv=wv, si=si, h=h: e.indirect_dma_start(
                                out=stg[si][:], out_offset=None, in_=wv,
                                in_offset=bass.IndirectOffsetOnAxis(ap=GIDX2[:, b, h:h + 1], axis=0),
                                bounds_check=S.breg, oob_is_err=False),
                                writes=["stg%d" % si], dma="stg%d" % si)
                            dst = wt[wb][:, h * jn:(h + 1) * jn, :].rearrange("p j f -> p (j f)")
                            if ceng == "vector":
                                S.op("vector", lambda e, dst=dst, si=si: e.tensor_copy(dst, stg[si][:]), reads=["stg%d" % si], writes=["%s%d" % (nm, wb)])
                            else:
                                S.op("scalar", lambda e, dst=dst, si=si: e.copy(dst, stg[si][:]), reads=["stg%d" % si], writes=["%s%d" % (nm, wb)])

                W_GU = ((wg, wgv, "wg", 4), (wu, wuv, "wu", 4))
                W_D = ((wd, wdv, "wd", 2),)

                def load_xs(b):
                    bb = b % 2
                    S.op("sync", lambda e: e.dma_start(out=xs[bb][:], in_=Xs[b * BLK:(b + 1) * BLK, :].rearrange("(s p) f -> p s f", p=128)),
                         reads=["Xs_dram"], writes=["xs%d" % bb], dma="xs%d" % bb)

                def e_front(b):
                    bb = b % 2
                    wb = bb
                    if b + 1 < NBLK:
                        load_xs(b + 1)
                    for sub in range(4):
                        ti = sub % 2
                        for j in range(8):
                            S.op("tensor", lambda e, ti=ti, j=j, sub=sub: e.transpose(
                                ps_t[ti][:, j, :], xs[bb][:, sub, :].rearrange("t (p j) -> t j p", j=8)[:, j, :], ident_bf[:]),
                                reads=["xs%d" % bb, "ident_bf"], writes=["e_pst%d" % ti])
                        S.op("vector", lambda e, ti=ti, sub=sub: e.tensor_copy(xsT[bb][:, :, sub * 128:(sub + 1) * 128], ps_t[ti][:]),
                             reads=["e_pst%d" % ti], writes=["xsT%d" % bb])
                    for jf in range(4):
                        gi = jf % 2
                        for (pst_, wt, nm) in ((ps_g, wg, "wg"), (ps_u, wu, "wu")):
                            for jd in range(8):
                                S.op("tensor", lambda e, pst_=pst_, wt=wt, gi=gi, jd=jd, jf=jf: e.matmul(
                                    pst_[gi][:], wt[wb][:, jd, :].rearrange("p (pf jf) -> p jf pf", jf=4)[:, jf, :], xsT[bb][:, jd, :],
                                    start=(jd == 0), stop=(jd == 7)), reads=["%s%d" % (nm, wb), "xsT%d" % bb], writes=["e_%s%d" % (nm, gi)])
                        S.op("scalar", lambda e, gi=gi: e.activation(out=sg[gi][:], in_=ps_g[gi][:], func=AF.Silu, bias=zeroc[:], scale=1.0),
                             reads=["e_wg%d" % gi, "zeroc"], writes=["sg%d" % gi])
                        S.op("vector", lambda e, gi=gi, jf=jf: e.tensor_tensor(out=hT[bb][:, jf, :], in0=ps_u[gi][:], in1=sg[gi][:], op=ALU.mult),
                             reads=["e_wu%d" % gi, "sg%d" % gi], writes=["hT%d" % bb])

                def e_back(b):
                    bb = b % 2
                    wb = bb
                    for sub in range(4):
                        for half in range(2):
                            yi = yc_[0] % 2
                            yc_[0] += 1
                            for jf in range(4):
                                S.op("tensor", lambda e, yi=yi, jf=jf, sub=sub, half=half: e.matmul(
                                    ps_y[yi][:], hT[bb][:, jf, sub * 128:(sub + 1) * 128], wd[wb][:, jf, half * 512:(half + 1) * 512],
                                    start=(jf == 0), stop=(jf == 3)), reads=["hT%d" % bb, "wd%d" % wb], writes=["e_psy%d" % yi])
                            S.op("scalar", lambda e, yi=yi, sub=sub, half=half: e.copy(ysb[bb][:, sub, half * 512:(half + 1) * 512], ps_y[yi][:]),
                                 reads=["e_psy%d" % yi], writes=["ysb%d" % bb])
                    S.op("sync", lambda e: e.dma_start(out=Ys[b * BLK:(b + 1) * BLK, :].rearrange("(s p) f -> p s f", p=128), in_=ysb[bb][:]),
                         reads=["ysb%d" % bb], writes=["Ys_dram"], dma="ysb%d" % bb)

                load_weights(0, W_GU)
                load_weights(0, W_D)
                load_xs(0)
                for b in range(NBLK + 1):
                    if b < NBLK:
                        e_front(b)
                        if b + 1 < NBLK:
                            load_weights(b + 1, W_GU)
                    if b >= 1:
                        e_back(b - 1)
                    if b + 1 < NBLK:
                        load_weights(b + 1, W_D)
                S.barrier()
                S.emit(want_breg=True)

            with ExitStack() as ph:
                lng = SB(ph, "f_lng", [128, DM], F32)
                lnb = SB(ph, "f_lnb", [128, DM], F32)
                y0 = [SB(ph, "f_y0%d" % i, [128, DM], BF16) for i in range(4)]
                y1 = [SB(ph, "f_y1%d" % i, [128, DM], BF16) for i in range(4)]
                xa = [SB(ph, "f_xa%d" % i, [128, DM], F32) for i in range(4)]
                accs = [SB(ph, "f_acc%d" % i, [128, DM], F32) for i in range(2)]
                junk = SB(ph, "f_junk", [128, DM], F32)
                st = [SB(ph, "f_st%d" % i, [128, 16], F32) for i in range(2)]
                bst = [SB(ph, "f_bst%d" % i, [128, 2, 6], F32) for i in range(2)]
                x2 = [SB(ph, "f_x2%d" % i, [128, DM], F32) for i in range(2)]
                xb = [SB(ph, "f_xb%d" % i, [128, DM], BF16) for i in range(4)]
                ps_t = [PS(ph, "f_pst%d" % i, [128, 8, 128], BF16) for i in range(2)]
                xTg = SB(ph, "f_xTg", [128, 8, 512], BF16)
                S.op("sync", lambda e: e.dma_start(out=lng[:], in_=ln_gain[l, 1:2, :].partition_broadcast(128)), writes=["lng"], dma="lng")
                S.op("sync", lambda e: e.dma_start(out=lnb[:], in_=ln_bias[l, 1:2, :].partition_broadcast(128)), writes=["lnb"], dma="lnb")
                last = (l == L - 1)

                def f_s0(tt):
                    b4 = tt % 4
                    S.op("sync", lambda e: e.dma_start(out=xa[b4][:], in_=X1[tt * 128:(tt + 1) * 128, :]),
                         reads=["X1_dram"], writes=["xa%d" % b4], dma="xa%d" % b4)
                    for kk, yt in enumerate((y0, y1)):
                        S.op("gpsimd", lambda e, kk=kk, yt=yt: e.indirect_dma_start(
                            out=yt[b4][:], out_offset=None, in_=Ys, in_offset=bass.IndirectOffsetOnAxis(ap=DSTi[:, tt, kk:kk + 1], axis=0)),
                            reads=["Ys_dram"], writes=["y%d_%d" % (kk, b4)], dma="y%d_%d" % (kk, b4))

                def f_s1(tt):
                    b2 = tt % 2
                    b4 = tt % 4
                    stt = st[b2]
                    sk_ = "st_%d" % b2
                    ak = "accs%d" % b2
                    S.op("vector", lambda e: e.scalar_tensor_tensor(out=accs[b2][:], in0=y0[b4][:], scalar=W01[:, tt, 0:1], in1=xa[b4][:],
                                                                    op0=ALU.mult, op1=ALU.add), reads=["y0_%d" % b4, "xa%d" % b4], writes=[ak])
                    S.op("vector", lambda e: e.scalar_tensor_tensor(out=accs[b2][:], in0=y1[b4][:], scalar=W01[:, tt, 1:2], in1=accs[b2][:],
                                                                    op0=ALU.mult, op1=ALU.add), reads=["y1_%d" % b4, ak], writes=[ak])
                    S.op("vector", lambda e: e.bn_stats(bst[b2][:, 0, :], accs[b2][:, 0:512]), reads=[ak], writes=["bst%d" % b2])
                    S.op("vector", lambda e: e.bn_stats(bst[b2][:, 1, :], accs[b2][:, 512:1024]), reads=[ak], writes=["bst%d" % b2])
                    S.op("vector", lambda e: e.bn_aggr(stt[:, 0:2], bst[b2][:]), reads=["bst%d" % b2], writes=[sk_])
                    S.op("scalar", lambda e: e.activation(out=stt[:, 2:3], in_=stt[:, 1:2], func=AF.Sqrt, bias=epsLN[:], scale=1.0), reads=[sk_, "epsLN"], writes=[sk_])
                    S.op("vector", lambda e: e.reciprocal(stt[:, 2:3], stt[:, 2:3]), reads=[sk_], writes=[sk_])
                    S.op("vector", lambda e: e.scalar_tensor_tensor(out=stt[:, 3:4], in0=stt[:, 0:1], scalar=-1.0, in1=stt[:, 2:3], op0=ALU.mult, op1=ALU.mult),
                         reads=[sk_], writes=[sk_])

                def f_s2(tt):
                    g, sub = divmod(tt, 4)
                    b2 = tt % 2
                    stt = st[b2]
                    sk_ = "st_%d" % b2
                    ak = "accs%d" % b2
                    x2k = "x2%d" % b2
                    S.op("scalar", lambda e: e.activation(out=x2[b2][:], in_=accs[b2][:], func=AF.Identity, bias=stt[:, 3:4], scale=stt[:, 2:3]),
                         reads=[ak, sk_], writes=[x2k])
                    S.op("vector", lambda e: e.tensor_tensor(out=x2[b2][:], in0=x2[b2][:], in1=lng[:], op=ALU.mult), reads=[x2k, "lng"], writes=[x2k])
                    S.op("gpsimd", lambda e: e.tensor_tensor(out=x2[b2][:], in0=x2[b2][:], in1=lnb[:], op=ALU.add), reads=[x2k, "lnb"], writes=[x2k])
                    S.op("sync", lambda e: e.dma_start(out=Xout[tt * 128:(tt + 1) * 128, :], in_=x2[b2][:]), reads=[x2k], writes=["X2_dram"], dma="x2o%d" % b2)
                    if not last:
                        S.op("scalar", lambda e: e.copy(xb[sub][:], x2[b2][:]), reads=[x2k], writes=["xb%d" % sub])
                        if sub == 3:
                            emit_xT_group((ps_t, xTg), lambda s_: "xb%d" % s_, lambda s_: xb[s_], g)

                f_s0(0)
                f_s0(1)
                for step in range(NT + 1):
                    if step + 2 < NT:
                        f_s0(step + 2)
                    if step < NT:
                        f_s1(step)
                    if 0 <= step - 1 < NT:
                        f_s2(step - 1)
                S.barrier()
                S.emit()
    return nc


def make_consts():
    c = np.zeros((128, C_END), np.float32)
    c[:, C_ID:C_ID + 128] = np.eye(128, dtype=np.float32)
    m = np.arange(128)
    sw = np.where((m % 64) < 32, m + 32, m - 32)
    perm = np.zeros((128, 128), np.float32)
    perm[sw, m] = 1.0
    c[:, C_PERM:C_PERM + 128] = perm
    j = np.arange(128)[:, None]
    i = np.arange(128)[None, :]
    c[:, C_MASK:C_MASK + 128] = np.where(i >= j, 0.0, NEG)
    c[:, C_MASK + 128:C_MASK + 256] = np.where(j >= i, 0.0, NEG)
    c[:, C_LS:C_LS + 128] = (j < i).astype(np.float32)
    e1 = np.arange(32)[:, None]
    e2 = np.arange(32)[None, :]
    c[0:32, C_UCAT:C_UCAT + 32] = (e1 <= e2).astype(np.float32)
    c[0:32, C_UCAT + 32:C_UCAT + 64] = np.eye(32, dtype=np.float32)
    c[:, C_THR:C_THR + 33] = (np.arange(33) * BLK).astype(np.float32)[None, :]
    c[:, C_BV:C_BV + NBLK] = np.arange(NBLK, dtype=np.float32)[None, :]
    c[:, C_IE:C_IE + 32] = np.arange(32, dtype=np.float32)[None, :]
    c[:, C_IP] = np.arange(128, dtype=np.float32)
    c[64, C_SEL:C_SEL + 64] = 1.0
    c[:, C_INVW:C_INVW + 3] = np.array([1.0 / 512, 1.0 / 256, 1.0 / 256], np.float32)[None, :]
    half = 32
    inv_freq = (np.float32(10000.0) ** (-np.arange(half, dtype=np.float32) / np.float32(half))).astype(np.float32)
    ang = (np.arange(S_LEN, dtype=np.float32)[:, None] * inv_freq[None, :]).astype(np.float32)
    cos = np.cos(ang).astype(np.float32).T
    sin = np.sin(ang).astype(np.float32).T
    rc = np.zeros((128, S_LEN), np.float32)
    rs = np.zeros((128, S_LEN), np.float32)
    for p in range(128):
        cc = p % 64
        rc[p] = cos[cc % 32]
        rs[p] = -sin[cc] if cc < 32 else sin[cc - 32]
    return c, rc, rs


_CACHE = {}


def kernel(**inputs):
    depth = DEPTH
    if "nc" not in _CACHE:
        _CACHE["nc"] = build_program(depth)
        _CACHE["consts"] = make_consts()
    nc = _CACHE["nc"]
    c, rc, rs = _CACHE["consts"]
    x = np.ascontiguousarray(np.asarray(inputs["x"], dtype=np.float32))
    shared = {k: np.ascontiguousarray(np.asarray(v, dtype=np.float32)) for k, v in inputs.items() if k != "x"}
    shared["consts"] = c
    shared["ropecos"] = rc
    shared["ropesin"] = rs
    in_maps = []
    for i in range(NCORES):
        m = dict(shared)
        m["x"] = x[i]
        in_maps.append(m)
    res = run_bass_kernel_spmd(nc, in_maps, core_ids=list(range(NCORES)))
    return np.stack([np.asarray(r["out"], dtype=np.float32) for r in res.results], axis=0)
```

```python
import os
import numpy as np
from contextlib import ExitStack
import concourse.bass as bass
import concourse.mybir as mybir
from concourse.bass_utils import run_bass_kernel_spmd

F32 = mybir.dt.float32
BF16 = mybir.dt.bfloat16
I32 = mybir.dt.int32
AF = mybir.ActivationFunctionType
ALU = mybir.AluOpType
AX = mybir.AxisListType

NCORES = 8
S_LEN = 8192
DM = 1024
DEPTH = 4
PW = 2816
NE = 32
DE = 512
NT = S_LEN // 128
NG = S_LEN // 512
BLK = 512
NBLK = 63
NSLOT = NBLK * BLK
ALPHA = (2.0 * DEPTH) ** 0.25
EPS = 1e-5
BRANCH_D = (1, 4, 16)
NEG = -30000.0

C_ID, C_PERM, C_MASK, C_LS, C_UCAT, C_THR, C_BV, C_IE, C_IP, C_SEL, C_INVW, C_END = (
    0, 128, 256, 512, 640, 704, 737, 801, 833, 834, 898, 904)

ENGS = ["tensor", "vector", "scalar", "gpsimd", "sync"]


class Sched:
    def __init__(self, nc, stack):
        self.nc = nc
        self.stack = stack
        self.q = {e: [] for e in ENGS}
        self.sems = {}
        self.cnt = {}
        self.seen = {e: {} for e in ENGS}
        self.last_w = {}
        self.readers = {}
        self.epoch = 0
        for e in ENGS:
            self._sem("E_%s_0" % e)
        self.n_ops = 0
        self.breg = None
        self.breg_val = None

    def new_epoch(self):
        self.epoch += 1
        self._sem("E_tensor_%d" % self.epoch)

    def _sem(self, name):
        if name not in self.sems:
            self.sems[name] = self.stack.enter_context(self.nc.semaphore(name))
            self.cnt[name] = 0
        return self.sems[name]

    def op(self, eng, fn, reads=(), writes=(), dma=None):
        pr = [k for k in reads if k.startswith(("ps", "e_ps", "e_wg", "e_wu"))]
        if pr:
            writes = list(writes) + pr
        deps = {}

        def add(ev):
            if ev is not None and deps.get(ev[0], 0) < ev[1]:
                deps[ev[0]] = ev[1]

        for k in reads:
            add(self.last_w.get(k))
        for k in writes:
            add(self.last_w.get(k))
            for ev in self.readers.get(k, ()):
                add(ev)
        waits = []
        seen = self.seen[eng]
        for s, v in deps.items():
            if eng == "tensor" and s.startswith("E_tensor"):
                continue
            if seen.get(s, 0) < v:
                seen[s] = v
                waits.append((self.sems[s], v))
        if dma is None:
            sname, inc = "E_%s_%d" % (eng, self.epoch if eng == "tensor" else 0), 1
        else:
            sname, inc = "D_" + dma, 16
            self._sem(sname)
        self.cnt[sname] += inc
        ev = (sname, self.cnt[sname])
        self.q[eng].append((waits, fn, self.sems[sname], inc))
        for k in reads:
            self.readers.setdefault(k, []).append(ev)
        for k in writes:
            self.last_w[k] = ev
            self.readers[k] = []
        self.n_ops += 1
        return ev

    def barrier(self):
        for e in ENGS:
            waits = []
            for s, c in self.cnt.items():
                if c > 0 and self.seen[e].get(s, 0) < c:
                    self.seen[e][s] = c
                    waits.append((self.sems[s], c))
            if waits:
                self.q[e].append((waits, None, None, 0))
        self.last_w = {}
        self.readers = {}

    def emit(self, want_breg=False):
        with self.nc.Block() as block:
            for e in ENGS:
                items = self.q[e]

                def body(engine, items=items, e=e):
                    if e == "gpsimd" and want_breg:
                        self.breg = engine.to_reg(self.breg_val)
                    for waits, fn, sem, inc in items:
                        for s, v in waits:
                            engine.wait_ge(s, v)
                        if fn is not None:
                            fn(engine).then_inc(sem, inc)

                getattr(block, e)(body)
        self.q = {e: [] for e in ENGS}


def build_program(depth=DEPTH, dbg=None):
    nc = bass.Bass("TRN2", target_bir_lowering=False)
    dt_in = lambda name, shape, dt=F32: nc.dram_tensor(name, shape, dt, kind="ExternalInput").ap()
    dt_sc = lambda name, shape, dt: nc.dram_tensor(name, shape, dt, kind=("ExternalOutput" if dbg else "Internal")).ap()
    L = depth
    x_in = dt_in("x", [S_LEN, DM])
    w_in = dt_in("w_in", [L, DM, PW])
    w_out = dt_in("w_out", [L, DM, DM])
    branch_gain = dt_in("branch_gain", [L, DM])
    sgu_gain = dt_in("sgu_gain", [L, 256])
    sgu_w = dt_in("sgu_w", [L, 4, 128, 128])
    sgu_b = dt_in("sgu_b", [L, 4, 128])
    conv_w = dt_in("conv_w", [L, 3, 256])
    ln_gain = dt_in("ln_gain", [L, 2, DM])
    ln_bias = dt_in("ln_bias", [L, 2, DM])
    rgw = dt_in("router_group_w", [L, DM, 4])
    rgb = dt_in("router_group_b", [L, 4])
    rew = dt_in("router_expert_w", [L, DM, NE])
    reb = dt_in("router_expert_b", [L, NE])
    ewg = dt_in("expert_w_gate", [L, NE, DM, DE])
    ewu = dt_in("expert_w_up", [L, NE, DM, DE])
    ewd = dt_in("expert_w_down", [L, NE, DE, DM])
    consts = dt_in("consts", [128, C_END])
    ropecos = dt_in("ropecos", [128, S_LEN])
    ropesin = dt_in("ropesin", [128, S_LEN])
    out = nc.dram_tensor("out", [S_LEN, DM], F32, kind="ExternalOutput").ap()

    xT = dt_sc("xT", [8, 128, S_LEN], BF16)
    QT = dt_sc("QT", [4, 128, S_LEN], BF16)
    KT = dt_sc("KT", [4, 128, S_LEN], BF16)
    Vd = dt_sc("Vd", [S_LEN, 512], BF16)
    mixT = dt_sc("mixT", [8, 128, S_LEN], BF16)
    X1 = dt_sc("X1", [S_LEN, DM], F32)
    X1b = dt_sc("X1b", [S_LEN, DM], BF16)
    X2 = dt_sc("X2", [S_LEN, DM], F32)
    Xs = dt_sc("Xs", [NSLOT, DM], BF16)
    Ys = dt_sc("Ys", [NSLOT, DM], BF16)

    with ExitStack() as top:
        S = Sched(nc, top)
        S.breg_val = L * NE * 128 * 2 - 1

        uniq = [0]

        def SB(st, name, shape, dt):
            uniq[0] += 1
            return st.enter_context(nc.sbuf_tensor("%s_%d" % (name, uniq[0]), shape, dt))

        def PS(st, name, shape, dt=F32):
            uniq[0] += 1
            return st.enter_context(nc.psum_tensor("%s_%d" % (name, uniq[0]), shape, dt))

        cst = SB(top, "cst", [128, C_END], F32)
        ident_bf = SB(top, "ident_bf", [128, 128], BF16)
        perm_bf = SB(top, "perm_bf", [128, 128], BF16)
        mask_bf = SB(top, "mask_bf", [128, 256], BF16)
        ls_bf = SB(top, "ls_bf", [128, 128], BF16)
        ones_bf = SB(top, "ones_bf", [128, 128], BF16)
        zeroc = SB(top, "zeroc", [128, 1], F32)
        epsc = SB(top, "epsc", [128, 1], F32)
        epsA2 = SB(top, "epsA2", [128, 1], F32)
        epsLN = SB(top, "epsLN", [128, 1], F32)
        M1all = SB(top, "M1all", [128, NT, NE], F32)
        M2all = SB(top, "M2all", [128, NT, NE], F32)
        RK = SB(top, "RK", [128, NT, 2], F32)
        W01 = SB(top, "W01", [128, NT, 2], F32)
        DSTi = SB(top, "DSTi", [128, NT, 2], I32)
        GIDX = SB(top, "GIDX", [128, NBLK], I32)
        GIDX2 = SB(top, "GIDX2", [128, NBLK, 2], I32)

        S.op("sync", lambda e: e.dma_start(out=cst[:], in_=consts), writes=["cst"], dma="cst")
        S.op("vector", lambda e: e.tensor_copy(ident_bf[:], cst[:, C_ID:C_ID + 128]), reads=["cst"], writes=["ident_bf"])
        S.op("vector", lambda e: e.tensor_copy(perm_bf[:], cst[:, C_PERM:C_PERM + 128]), reads=["cst"], writes=["perm_bf"])
        S.op("vector", lambda e: e.tensor_copy(mask_bf[:], cst[:, C_MASK:C_MASK + 256]), reads=["cst"], writes=["mask_bf"])
        S.op("vector", lambda e: e.tensor_copy(ls_bf[:], cst[:, C_LS:C_LS + 128]), reads=["cst"], writes=["ls_bf"])
        S.op("vector", lambda e: e.memset(ones_bf[:], 1.0), writes=["ones_bf"])
        S.op("vector", lambda e: e.memset(zeroc[:], 0.0), writes=["zeroc"])
        S.op("vector", lambda e: e.memset(epsc[:], EPS), writes=["epsc"])
        S.op("vector", lambda e: e.memset(epsA2[:], EPS * ALPHA * ALPHA), writes=["epsA2"])
        S.op("vector", lambda e: e.memset(epsLN[:], EPS / (ALPHA * ALPHA)), writes=["epsLN"])
        S.barrier()
        S.emit()

        def emit_xT_group(st_tiles, src_key_fn, src_ap_fn, g):
            ps_t, xTg = st_tiles
            for sub in range(4):
                pk = "ps_xt%d" % (sub % 2)
                for j in range(8):
                    S.op("tensor", lambda e, sub=sub, j=j: e.transpose(
                        ps_t[sub % 2][:, j, :], src_ap_fn(sub)[:, j * 128:(j + 1) * 128], ident_bf[:]),
                        reads=[src_key_fn(sub), "ident_bf"], writes=[pk])
                S.op("scalar", lambda e, sub=sub: e.copy(xTg[:, :, sub * 128:(sub + 1) * 128], ps_t[sub % 2][:]),
                     reads=[pk], writes=["xTg"])
            S.op("sync", lambda e: e.dma_start(
                out=xT.rearrange("c p t -> p c t")[:, :, g * 512:(g + 1) * 512], in_=xTg[:]),
                reads=["xTg"], writes=["xT_dram"], dma="xTg")

        with ExitStack() as ph:
            xin = [SB(ph, "p_xin%d" % i, [128, DM], F32) for i in range(2)]
            xb = [SB(ph, "p_xb%d" % i, [128, DM], BF16) for i in range(4)]
            ps_t = [PS(ph, "p_pst%d" % i, [128, 8, 128], BF16) for i in range(2)]
            xTg = SB(ph, "p_xTg", [128, 8, 512], BF16)
            for g in range(NG):
                for sub in range(4):
                    tt = g * 4 + sub
                    k = "xin%d" % (tt % 2)
                    S.op("sync", lambda e, tt=tt: e.dma_start(out=xin[tt % 2][:], in_=x_in[tt * 128:(tt + 1) * 128, :]),
                         writes=[k], dma=k)
                    S.op("vector", lambda e, tt=tt, sub=sub: e.tensor_copy(xb[sub][:], xin[tt % 2][:]),
                         reads=[k], writes=["xb%d" % sub])
                emit_xT_group((ps_t, xTg), lambda sub: "xb%d" % sub, lambda sub: xb[sub], g)
            S.barrier()
            S.emit()

        for l in range(L):
            if l > 0:
                S.new_epoch()
            Xin = x_in if l == 0 else X2
            Xout = out if l == L - 1 else X2
            with ExitStack() as ph:
                win = SB(ph, "a_win", [128, 8, PW], BF16)
                xt = [SB(ph, "a_xt%d" % i, [128, 8, 512], BF16) for i in range(2)]
                cs = [SB(ph, "a_cos%d" % i, [128, 512], F32) for i in range(2)]
                sn = [SB(ph, "a_sin%d" % i, [128, 512], F32) for i in range(2)]
                qbf = [SB(ph, "a_qbf%d" % i, [128, 512], BF16) for i in range(2)]
                t1 = [SB(ph, "a_t1%d" % i, [128, 512], F32) for i in range(2)]
                t2 = [SB(ph, "a_t2%d" % i, [128, 512], F32) for i in range(2)]
                qr = [SB(ph, "a_qr%d" % i, [128, 512], BF16) for i in range(2)]
                vsb = [SB(ph, "a_v%d" % i, [128, 4, 512], BF16) for i in range(2)]
                ga = [SB(ph, "a_ga%d" % i, [128, 512], F32) for i in range(2)]
                gt = [SB(ph, "a_gt%d" % i, [128, 512], F32) for i in range(2)]
                ug = [SB(ph, "a_ug%d" % i, [64, 512], F32) for i in range(4)]
                zg = [SB(ph, "a_zg%d" % i, [128, 256], F32) for i in range(2)]
                zsq = SB(ph, "a_zsq", [128, 256], F32)
                zst = SB(ph, "a_zst", [128, 16], F32)
                zn = [SB(ph, "a_zn%d" % i, [128, 4, 64], BF16) for i in range(4)]
                spv = [SB(ph, "a_spv%d" % i, [64, 512], F32) for i in range(2)]
                yb = [SB(ph, "a_yb%d" % i, [64, 512], BF16) for i in range(2)]
                wsf = SB(ph, "a_wsf", [128, 4, 128], F32)
                wsT = SB(ph, "a_wsT", [128, 4, 128], BF16)
                bsb = SB(ph, "a_bsb", [64, 4, 128], F32)
                sgain = SB(ph, "a_sgain", [64, 4], F32)
                cw = SB(ph, "a_cw", [128, 2, 3], F32)
                hs = [SB(ph, "a_hs%d" % i, [128, 512], F32) for i in range(2)]
                gbs = [SB(ph, "a_gbs%d" % i, [128, 512], F32) for i in range(2)]
                zce = SB(ph, "a_zce", [128, 2, 514], F32)
                yc = [SB(ph, "a_yc%d" % i, [128, 512], F32) for i in range(2)]
                ycb = [SB(ph, "a_ycb%d" % i, [128, 512], BF16) for i in range(2)]
                psA = [PS(ph, "a_ps%d" % i, [128, 512]) for i in range(7)]
                psW = psA[6][:].rearrange("p (g t) -> p g t", g=4)

                for dc in range(8):
                    S.op("gpsimd", lambda e, dc=dc: e.dma_start(out=win[:, dc, :], in_=w_in[l, dc * 128:(dc + 1) * 128, :]),
                         writes=["win"], dma="win")
                S.op("sync", lambda e: e.dma_start(out=wsf[:], in_=sgu_w[l].rearrange("g t s -> t g s")), writes=["wsf"], dma="wsf")
                for g in range(4):
                    S.op("gpsimd", lambda e, g=g: e.affine_select(out=wsf[:, g, :], in_=wsf[:, g, :], pattern=[[-1, 128]],
                                                                  compare_op=ALU.is_ge, fill=0.0, base=0, channel_multiplier=1),
                         reads=["wsf"], writes=["wsf"])
                for g in range(4):
                    S.op("tensor", lambda e, g=g: e.transpose(psW[:, g, :], wsf[:, g, :], cst[:, C_ID:C_ID + 128]),
                         reads=["wsf", "cst"], writes=["psA6"])
                S.op("vector", lambda e: e.tensor_copy(wsT[:], psW), reads=["psA6"], writes=["wsT"])
                for g in range(4):
                    S.op("sync", lambda e, g=g: e.dma_start(out=bsb[:, g, :], in_=sgu_b[l, g:g + 1, :].partition_broadcast(64)),
                         writes=["bsb"], dma="bsb")
                S.op("sync", lambda e: e.dma_start(out=sgain[:], in_=sgu_gain[l].rearrange("(g c) -> c g", g=4),
                                                   allow_slow_non_contiguous=True), writes=["sgain"], dma="sgain")
                for cc in range(2):
                    for k in range(3):
                        S.op("sync", lambda e, cc=cc, k=k: e.dma_start(out=cw[:, cc, k:k + 1], in_=conv_w[l, k:k + 1, cc * 128:(cc + 1) * 128].rearrange("k p -> p k"),
                                                                       allow_slow_non_contiguous=True), writes=["cw"], dma="cw")
                S.op("vector", lambda e: e.memset(zce[:], 0.0), writes=["zce"])

                psn = [0]

                def next_ps():
                    i = psn[0] % 7
                    psn[0] += 1
                    return i

                def gelu(src_ap, src_key, out_ap, out_key, P, N, bi):
                    a = ga[bi][0:P, 0:N]
                    t = gt[bi][0:P, 0:N]
                    ak, tk = "ga%d" % bi, "gt%d" % bi
                    S.op("scalar", lambda e: e.copy(a, src_ap), reads=[src_key], writes=[ak])
                    S.op("gpsimd", lambda e: e.tensor_tensor(out=t, in0=a, in1=a, op=ALU.mult), reads=[ak], writes=[tk])
                    S.op("vector", lambda e: e.tensor_scalar(t, t, 0.044715, 1.0, ALU.mult, ALU.add), reads=[tk], writes=[tk])
                    S.op("vector", lambda e: e.tensor_tensor(out=t, in0=t, in1=a, op=ALU.mult), reads=[tk, ak], writes=[tk])
                    S.op("scalar", lambda e: e.activation(out=t, in_=t, func=AF.Sigmoid, bias=zeroc[0:P, :], scale=1.5957691216),
                         reads=[tk, "zeroc"], writes=[tk])
                    S.op("vector", lambda e: e.tensor_tensor(out=out_ap, in0=a, in1=t, op=ALU.mult), reads=[ak, tk], writes=[out_key])

                gcount = [0]
                def a_load(i):
                    S.op("sync", lambda e: e.dma_start(out=xt[i % 2][:], in_=xT.rearrange("c p t -> p c t")[:, :, i * 512:(i + 1) * 512]),
                         reads=["xT_dram"], writes=["xt%d" % (i % 2)], dma="xt%d" % (i % 2))
                    S.op("sync", lambda e: e.dma_start(out=cs[i % 2][:], in_=ropecos[:, i * 512:(i + 1) * 512]),
                         writes=["cs%d" % (i % 2)], dma="cs%d" % (i % 2))
                    S.op("sync", lambda e: e.dma_start(out=sn[i % 2][:], in_=ropesin[:, i * 512:(i + 1) * 512]),
                         writes=["sn%d" % (i % 2)], dma="sn%d" % (i % 2))

                a_load(0)
                for i in range(NG):
                    tsl = slice(i * 512, (i + 1) * 512)
                    xk = "xt%d" % (i % 2)
                    xti = xt[i % 2]
                    csk, snk = "cs%d" % (i % 2), "sn%d" % (i % 2)
                    def qk_main(qi, xti=xti):
                        c = qi % 4
                        isq = qi < 4
                        col0 = (0 if isq else 512) + c * 128
                        scale = 0.125 if isq else 1.0
                        p = next_ps()
                        pk = "psA%d" % p
                        for dc in range(8):
                            S.op("tensor", lambda e, dc=dc: e.matmul(
                                psA[p][:], win[:, dc, col0:col0 + 128], xti[:, dc, :], start=(dc == 0), stop=(dc == 7)),
                                reads=["win", xk], writes=[pk])
                        b2 = qi % 2
                        S.op("scalar", lambda e: e.activation(
                            out=qbf[b2][:], in_=psA[p][:], func=AF.Identity, bias=zeroc[:], scale=scale),
                            reads=[pk, "zeroc"], writes=["qbf%d" % b2])
                        return p

                    def qk_rope(qi, p, i=i, xti=xti):
                        c = qi % 4
                        isq = qi < 4
                        scale = 0.125 if isq else 1.0
                        pk = "psA%d" % p
                        b2 = qi % 2
                        S.op("vector", lambda e: e.scalar_tensor_tensor(
                            out=t2[b2][:], in0=psA[p][:], scalar=scale, in1=cs[i % 2][:], op0=ALU.mult, op1=ALU.mult),
                            reads=[pk, csk], writes=["t2%d" % b2])
                        p2 = next_ps()
                        if p2 == p:
                            p2 = next_ps()
                        pk2 = "psA%d" % p2
                        S.op("tensor", lambda e: e.matmul(psA[p2][:], perm_bf[:], qbf[b2][:], start=True, stop=True),
                             reads=["perm_bf", "qbf%d" % b2], writes=[pk2])
                        S.op("vector", lambda e: e.tensor_tensor(
                            out=t1[b2][:], in0=psA[p2][:], in1=sn[i % 2][:], op=ALU.mult),
                            reads=[pk2, snk], writes=["t1%d" % b2])
                        S.op("gpsimd", lambda e: e.tensor_tensor(out=qr[b2][:], in0=t1[b2][:], in1=t2[b2][:], op=ALU.add),
                             reads=["t1%d" % b2, "t2%d" % b2], writes=["qr%d" % b2])
                        dst = QT if isq else KT
                        S.op("sync", lambda e: e.dma_start(
                            out=dst[c, :, i * 512:(i + 1) * 512], in_=qr[b2][:]),
                            reads=["qr%d" % b2], writes=["QK_dram"], dma="qr%d" % b2)

                    pend = None
                    for qi in range(8):
                        p_ = qk_main(qi)
                        if pend is not None:
                            qk_rope(*pend)
                        pend = (qi, p_)
                    if i + 1 < NG:
                        a_load(i + 1)
                    vb = i % 2
                    for sub in range(4):
                        p = next_ps()
                        pk = "psA%d" % p
                        for dc in range(8):
                            S.op("tensor", lambda e, p=p, dc=dc, sub=sub, xti=xti: e.matmul(
                                psA[p][:], xti[:, dc, sub * 128:(sub + 1) * 128], win[:, dc, 1024:1536], start=(dc == 0), stop=(dc == 7)),
                                reads=["win", xk], writes=[pk])
                        S.op("scalar", lambda e, p=p, sub=sub, vb=vb: e.copy(vsb[vb][:, sub, :], psA[p][:]),
                             reads=[pk], writes=["vsb%d" % vb])
                    S.op("sync", lambda e, vb=vb, i=i: e.dma_start(
                        out=Vd[i * 512:(i + 1) * 512, :].rearrange("(s p) f -> p s f", p=128), in_=vsb[vb][:]),
                        reads=["vsb%d" % vb], writes=["V_dram"], dma="vsb%d" % vb)
                    qk_rope(*pend)
                    for sub in range(4):
                        p = next_ps()
                        pk = "psA%d" % p
                        for dc in range(8):
                            S.op("tensor", lambda e, p=p, dc=dc, sub=sub, xti=xti: e.matmul(
                                psA[p][:, 0:256], xti[:, dc, sub * 128:(sub + 1) * 128], win[:, dc, 1792:2048], start=(dc == 0), stop=(dc == 7)),
                                reads=["win", xk], writes=[pk])
                        zb = sub % 2
                        zk = "zg%d" % zb
                        gelu(psA[p][:, 0:256], pk, zg[zb][:], zk, 128, 256, gcount[0] % 2)
                        gcount[0] += 1
                        z3 = zg[zb][:].rearrange("p (g c) -> p g c", g=4)
                        S.op("vector", lambda e, z3=z3: e.tensor_reduce(zst[:, 0:4], z3, axis=AX.X, op=ALU.add), reads=[zk], writes=["zst"])
                        S.op("gpsimd", lambda e, zb=zb: e.tensor_tensor(out=zsq[:], in0=zg[zb][:], in1=zg[zb][:], op=ALU.mult), reads=[zk], writes=["zsq"])
                        S.op("vector", lambda e: e.tensor_reduce(zst[:, 4:8], zsq[:].rearrange("p (g c) -> p g c", g=4), axis=AX.X, op=ALU.add),
                             reads=["zsq"], writes=["zst"])
                        S.op("vector", lambda e: e.tensor_scalar(zst[:, 0:4], zst[:, 0:4], 1.0 / 64, None, ALU.mult), reads=["zst"], writes=["zst"])
                        S.op("vector", lambda e: e.tensor_tensor(out=zst[:, 8:12], in0=zst[:, 0:4], in1=zst[:, 0:4], op=ALU.mult), reads=["zst"], writes=["zst"])
                        S.op("vector", lambda e: e.scalar_tensor_tensor(out=zst[:, 4:8], in0=zst[:, 4:8], scalar=1.0 / 64, in1=zst[:, 8:12],
                                                                        op0=ALU.mult, op1=ALU.subtract), reads=["zst"], writes=["zst"])
                        S.op("scalar", lambda e: e.activation(out=zst[:, 4:8], in_=zst[:, 4:8], func=AF.Sqrt, bias=epsc[:], scale=1.0),
                             reads=["zst", "epsc"], writes=["zst"])
                        S.op("vector", lambda e: e.reciprocal(zst[:, 4:8], zst[:, 4:8]), reads=["zst"], writes=["zst"])
                        S.op("vector", lambda e, z3=z3: e.tensor_tensor(out=z3, in0=z3, in1=zst[:, 0:4].unsqueeze(2).to_broadcast([128, 4, 64]), op=ALU.subtract),
                             reads=[zk, "zst"], writes=[zk])
                        S.op("vector", lambda e, z3=z3, sub=sub: e.tensor_tensor(out=zn[sub][:], in0=z3, in1=zst[:, 4:8].unsqueeze(2).to_broadcast([128, 4, 64]), op=ALU.mult),
                             reads=[zk, "zst"], writes=["zn%d" % sub])
                    for g in range(4):
                        p = next_ps()
                        pk = "psA%d" % p
                        for dc in range(8):
                            S.op("tensor", lambda e, p=p, dc=dc, g=g, xti=xti: e.matmul(
                                psA[p][0:64, :], win[:, dc, 1536 + 64 * g:1536 + 64 * (g + 1)], xti[:, dc, :], start=(dc == 0), stop=(dc == 7)),
                                reads=["win", xk], writes=[pk])
                        gelu(psA[p][0:64, :], pk, ug[g][:], "ug%d" % g, 64, 512, gcount[0] % 2)
                        gcount[0] += 1
                    for cc in range(2):
                        pgb, pgc, ph_ = next_ps(), next_ps(), next_ps()
                        for (p, col0) in ((pgb, 2048), (pgc, 2304), (ph_, 2560)):
                            for dc in range(8):
                                S.op("tensor", lambda e, p=p, dc=dc, col0=col0, cc=cc, xti=xti: e.matmul(
                                    psA[p][:], win[:, dc, col0 + cc * 128:col0 + (cc + 1) * 128], xti[:, dc, :], start=(dc == 0), stop=(dc == 7)),
                                    reads=["win", xk], writes=["psA%d" % p])
                        S.op("scalar", lambda e, cc=cc, ph_=ph_: e.copy(hs[cc][:], psA[ph_][:]), reads=["psA%d" % ph_], writes=["hs%d" % cc])
                        S.op("scalar", lambda e, cc=cc, pgb=pgb: e.copy(gbs[cc][:], psA[pgb][:]), reads=["psA%d" % pgb], writes=["gbs%d" % cc])
                        zk = "zce%d" % cc
                        if i > 0:
                            S.op("gpsimd", lambda e, cc=cc: e.tensor_copy(zce[:, cc, 0:2], zce[:, cc, 512:514]), reads=[zk, "zce"], writes=[zk])
                        S.op("vector", lambda e, cc=cc, pgc=pgc: e.tensor_tensor(out=zce[:, cc, 2:514], in0=psA[pgc][:], in1=hs[cc][:], op=ALU.mult),
                             reads=["psA%d" % pgc, "hs%d" % cc, "zce"], writes=[zk])
                        S.op("scalar", lambda e, cc=cc: e.activation(out=yc[cc][:], in_=zce[:, cc, 2:514], func=AF.Identity, bias=zeroc[:], scale=cw[:, cc, 2:3]),
                             reads=[zk, "cw", "zeroc"], writes=["yc%d" % cc])
                        S.op("vector", lambda e, cc=cc: e.scalar_tensor_tensor(out=yc[cc][:], in0=zce[:, cc, 1:513], scalar=cw[:, cc, 1:2], in1=yc[cc][:],
                                                                               op0=ALU.mult, op1=ALU.add), reads=[zk, "cw", "yc%d" % cc], writes=["yc%d" % cc])
                        S.op("vector", lambda e, cc=cc: e.scalar_tensor_tensor(out=yc[cc][:], in0=zce[:, cc, 0:512], scalar=cw[:, cc, 0:1], in1=yc[cc][:],
                                                                               op0=ALU.mult, op1=ALU.add), reads=[zk, "cw", "yc%d" % cc], writes=["yc%d" % cc])
                        S.op("gpsimd", lambda e, cc=cc: e.tensor_tensor(out=ycb[cc][:], in0=gbs[cc][:], in1=yc[cc][:], op=ALU.mult),
                             reads=["gbs%d" % cc, "yc%d" % cc], writes=["ycb%d" % cc])
                        S.op("sync", lambda e, cc=cc, i=i: e.dma_start(out=mixT[6 + cc, :, i * 512:(i + 1) * 512], in_=ycb[cc][:]),
                             reads=["ycb%d" % cc], writes=["mix_dram"], dma="ycb%d" % cc)
                    for g in range(4):
                        p = next_ps()
                        pk = "psA%d" % p
                        for sub in range(4):
                            S.op("tensor", lambda e, p=p, g=g, sub=sub: e.matmul(
                                psA[p][0:64, sub * 128:(sub + 1) * 128], zn[sub][:, g, :], wsT[:, g, :], start=True, stop=True),
                                reads=["zn%d" % sub, "wsT"], writes=[pk])
                        sb2 = g % 2
                        S.op("vector", lambda e, p=p, g=g, sb2=sb2: e.scalar_tensor_tensor(
                            out=spv[sb2][:].rearrange("p (s t) -> p s t", s=4), in0=psA[p][0:64, :].rearrange("p (s t) -> p s t", s=4),
                            scalar=sgain[:, g:g + 1], in1=bsb[:, g:g + 1, :].to_broadcast([64, 4, 128]), op0=ALU.mult, op1=ALU.add),
                            reads=[pk, "sgain", "bsb"], writes=["spv%d" % sb2])
                        S.op("gpsimd", lambda e, g=g, sb2=sb2: e.tensor_tensor(out=yb[sb2][:], in0=spv[sb2][:], in1=ug[g][:], op=ALU.mult),
                             reads=["spv%d" % sb2, "ug%d" % g], writes=["yb%d" % sb2])
                        S.op("sync", lambda e, g=g, sb2=sb2, i=i: e.dma_start(
                            out=mixT[4 + g // 2, (g % 2) * 64:(g % 2) * 64 + 64, i * 512:(i + 1) * 512], in_=yb[sb2][:]),
                            reads=["yb%d" % sb2], writes=["mix_dram"], dma="yb%d" % sb2)
                S.barrier()
                S.emit()

            with ExitStack() as ph:
                qn = SB(ph, "b_qn", [128, S_LEN], BF16)
                kn = SB(ph, "b_kn", [128, S_LEN], BF16)
                qd = SB(ph, "b_qd", [128, S_LEN], BF16)
                kd = SB(ph, "b_kd", [128, S_LEN], BF16)
                vas = [SB(ph, "b_va%d" % i, [128, NT, 2, 65], BF16) for i in range(2)]
                acc = SB(ph, "b_acc", [65, 2, S_LEN], F32)
                pt = [SB(ph, "b_pt%d" % i, [128, 2, 256], BF16) for i in range(4)]
                m01 = SB(ph, "b_m01", [128, 2, 256], BF16)
                rec = [SB(ph, "b_rec%d" % i, [64, 512], F32) for i in range(2)]
                yaT = [SB(ph, "b_yaT%d" % i, [64, 2048], BF16) for i in range(2)]
                sel_f = cst[0:65, C_SEL:C_SEL + 64]
                ps_s = [PS(ph, "b_pss%d" % i, [128, 2, 256]) for i in range(3)]
                ps_o = [PS(ph, "b_pso%d" % i, [65, 512]) for i in range(3)]
                ps_d = [PS(ph, "b_psd%d" % i, [64, 512]) for i in range(2)]
                for s_ in range(2):
                    S.op("vector", lambda e, s_=s_: e.tensor_scalar(m01[:, s_, :], cst[:, C_MASK:C_MASK + 256], 0.0, None, ALU.is_equal),
                         reads=["cst"], writes=["m01"])
                S.op("vector", lambda e: e.memset(vas[0][:], 1.0), writes=["va0"])
                S.op("gpsimd", lambda e: e.memset(vas[1][:], 1.0), writes=["va1"])
                pcount = [0]

                def load_v(job):
                    c_, bi_ = divmod(job, 3)
                    d_ = BRANCH_D[bi_]
                    nb_ = NT // d_
                    vi = job % 2
                    vsrc = Vd.rearrange("(n j r) f -> j r n f", j=128, r=d_)
                    for r in range(d_):
                        for h2 in range(2):
                            S.op("sync", lambda e, r=r, h2=h2: e.dma_start(
                                out=vas[vi][:, r * nb_:(r + 1) * nb_, h2, 0:64],
                                in_=vsrc[:, r, :, c_ * 128 + h2 * 64:c_ * 128 + (h2 + 1) * 64]),
                                reads=["V_dram"], writes=["va%d" % vi], dma="va%d" % vi)

                load_v(0)

                def load_qk(c_):
                    S.op("sync", lambda e: e.dma_start(out=qn[:], in_=QT[c_]), reads=["QK_dram"], writes=["qn"], dma="qn")
                    S.op("sync", lambda e: e.dma_start(out=kn[:], in_=KT[c_]), reads=["QK_dram"], writes=["kn"], dma="kn")

                def deint(d_):
                    S.op("vector", lambda e: e.tensor_copy(qd[:].rearrange("p (r m) -> p r m", r=d_),
                                                           qn[:].rearrange("p (m r) -> p r m", r=d_)), reads=["qn"], writes=["qd"])
                    S.op("gpsimd", lambda e: e.tensor_copy(kd[:].rearrange("p (r m) -> p r m", r=d_),
                                                           kn[:].rearrange("p (m r) -> p r m", r=d_)), reads=["kn"], writes=["kd"])

                load_qk(0)
                for c in range(4):
                    for bi, d in enumerate(BRANCH_D):
                        nb = NT // d
                        if bi == 0:
                            deint(4)
                            qs, ks, qsk, ksk = qn, kn, "qn", "kn"
                        elif bi == 1:
                            qs, ks, qsk, ksk = qd, kd, "qd", "kd"
                        else:
                            qs, ks, qsk, ksk = qd, kd, "qd", "kd"
                            deint(16)
                            if c + 1 < 4:
                                load_qk(c + 1)
                        job = c * 3 + bi
                        if job + 1 < 12:
                            load_v(job + 1)
                        va = vas[job % 2]
                        vak = "va%d" % (job % 2)
                        for hh in range(2):
                            P0 = hh * 64
                            started = {}

                            def emit_scores(kp, P0=P0, qs=qs, ks=ks, qsk=qsk, ksk=ksk, nb=nb):
                                sb_i = kp % 3
                                sk = "pss%d" % sb_i
                                for T in (2 * kp, 2 * kp + 1):
                                    n = T % nb
                                    N = 256 if n < nb - 1 else 128
                                    slot = T % 2
                                    S.op("tensor", lambda e, sb_i=sb_i, slot=slot, N=N, T=T: e.matmul(
                                        ps_s[sb_i][:, slot, 0:N], ks[P0:P0 + 64, T * 128:(T + 1) * 128], qs[P0:P0 + 64, T * 128:T * 128 + N],
                                        start=True, stop=True, skip_group_check=True), reads=[ksk, qsk], writes=[sk])
                                pi = pcount[0] % 4
                                pcount[0] += 1
                                S.op("scalar", lambda e, sb_i=sb_i, pi=pi: e.activation(out=pt[pi][:], in_=ps_s[sb_i][:], func=AF.Exp,
                                                                                        bias=zeroc[:], scale=1.0),
                                     reads=[sk, "zeroc"], writes=["pt%d" % pi])
                                meng = "vector" if (kp % 3 == 0) else "gpsimd"
                                S.op(meng, lambda e, pi=pi: e.tensor_tensor(out=pt[pi][:], in0=pt[pi][:], in1=m01[:], op=ALU.mult),
                                     reads=["pt%d" % pi, "m01"], writes=["pt%d" % pi])
                                return pi

                            def emit_pv(kp, pi, hh=hh, nb=nb, d=d, bi=bi, started=started, va=va, vak=vak):
                                ptk = "pt%d" % pi
                                for T2 in (2 * kp, 2 * kp + 1):
                                    n2 = T2 % nb
                                    N2 = 256 if n2 < nb - 1 else 128
                                    sl2 = T2 % 2
                                    if N2 == 256 and (T2 % 4) != 3:
                                        pieces = [(T2, 0, 256)]
                                    else:
                                        pieces = [(T2, 0, 128)]
                                        if N2 == 256:
                                            pieces.append((T2 + 1, 128, 128))
                                    for (qb, off, wdt) in pieces:
                                        B = qb // 4
                                        ob = B % 3
                                        first = B not in started
                                        started[B] = True
                                        S.op("tensor", lambda e, ob=ob, qb=qb, off=off, wdt=wdt, T2=T2, sl2=sl2, first=first: e.matmul(
                                            ps_o[ob][:, (qb % 4) * 128:(qb % 4) * 128 + wdt], va[:, T2, hh, :], pt[pi][:, sl2, off:off + wdt],
                                            start=first, stop=False, skip_group_check=True),
                                            reads=[vak, ptk], writes=["pso%d" % ob])
                                    if T2 % 4 == 3:
                                        B = T2 // 4
                                        ob = B % 3
                                        pos0 = B * 512
                                        r = pos0 // (S_LEN // d)
                                        m0 = pos0 % (S_LEN // d)
                                        dstv = acc[:, hh, :].rearrange("p (m r) -> p r m", r=d)[:, r, m0:m0 + 512]
                                        if bi == 0:
                                            S.op("vector", lambda e, ob=ob, dstv=dstv: e.tensor_copy(dstv, ps_o[ob][:]),
                                                 reads=["pso%d" % ob], writes=["acc%d" % hh])
                                        else:
                                            S.op("vector", lambda e, ob=ob, dstv=dstv: e.tensor_tensor(out=dstv, in0=ps_o[ob][:], in1=dstv, op=ALU.add),
                                                 reads=["pso%d" % ob, "acc%d" % hh], writes=["acc%d" % hh])

                            hist = []
                            for kp in range(NT // 2):
                                pi = emit_scores(kp)
                                hist.append((kp, pi))
                                if len(hist) > 2:
                                    emit_pv(*hist[-3])
                            emit_pv(*hist[-2])
                            emit_pv(*hist[-1])
                    for hh in range(2):
                        for B in range(NG):
                            di = B % 2
                            S.op("tensor", lambda e, di=di, hh=hh, B=B: e.matmul(ps_d[di][:], sel_f, acc[:, hh, B * 512:(B + 1) * 512], start=True, stop=True),
                                 reads=["cst", "acc%d" % hh], writes=["psd%d" % di])
                            S.op("vector", lambda e, di=di: e.reciprocal(rec[di][:], ps_d[di][:]), reads=["psd%d" % di], writes=["rec%d" % di])
                            yi_ = (B // 4) % 2
                            S.op("gpsimd", lambda e, di=di, hh=hh, B=B, yi_=yi_: e.tensor_tensor(out=yaT[yi_][:, (B % 4) * 512:(B % 4 + 1) * 512], in0=acc[0:64, hh, B * 512:(B + 1) * 512],
                                                                                                  in1=rec[di][:], op=ALU.mult),
                                 reads=["rec%d" % di, "acc%d" % hh], writes=["yaT%d" % yi_])
                            if B % 4 == 3:
                                S.op("sync", lambda e, c=c, hh=hh, B=B, yi_=yi_: e.dma_start(
                                    out=mixT[c, hh * 64:(hh + 1) * 64, (B // 4) * 2048:(B // 4 + 1) * 2048], in_=yaT[yi_][:]),
                                    reads=["yaT%d" % yi_], writes=["mix_dram"], dma="yaT%d" % yi_)
                S.barrier()
                S.emit()

            with ExitStack() as ph:
                wo_f = SB(ph, "c_wof", [128, DM], F32)
                wo = SB(ph, "c_wo", [128, 8, DM], BF16)
                bg = SB(ph, "c_bg", [128, 8], F32)
                lng = SB(ph, "c_lng", [128, DM], F32)
                lnb = SB(ph, "c_lnb", [128, DM], F32)
                wr_f = SB(ph, "c_wrf", [128, 8, 36], F32)
                wr = SB(ph, "c_wr", [128, 8, 36], BF16)
                rb = SB(ph, "c_rb", [128, 36], F32)
                mx = [SB(ph, "c_mx%d" % i, [128, 8, 512], BF16) for i in range(2)]
                sq = SB(ph, "c_sq", [128, 8, 512], BF16)
                xa = [SB(ph, "c_xa%d" % i, [128, DM], F32) for i in range(4)]
                accs = [SB(ph, "c_acc%d" % i, [128, DM], F32) for i in range(2)]
                junk = SB(ph, "c_junk", [128, DM], F32)
                st = [SB(ph, "c_st%d" % i, [128, 16], F32) for i in range(2)]
                rs3 = [SB(ph, "c_rs3%d" % i, [128, 3], F32) for i in range(2)]
                bst = [SB(ph, "c_bst%d" % i, [128, 2, 6], F32) for i in range(2)]
                x1 = [SB(ph, "c_x1%d" % i, [128, DM], F32) for i in range(2)]
                x1b = [SB(ph, "c_x1b%d" % i, [128, DM], BF16) for i in range(2)]
                x1T = SB(ph, "c_x1T", [128, 8, 128], BF16)
                lg = [SB(ph, "c_lg%d" % i, [128, 36], F32) for i in range(2)]
                rt = SB(ph, "c_rt", [128, 48], F32)
                elm = SB(ph, "c_elm", [128, NE], F32)
                top8 = SB(ph, "c_top8", [128, 8], F32)
                Abf = SB(ph, "c_Abf", [128, NE], BF16)
                rk = SB(ph, "c_rk", [128, NE], F32)
                tmp32 = SB(ph, "c_tmp32", [128, NE], F32)
                base = SB(ph, "c_base", [128, NE], F32)
                ps_z = [PS(ph, "c_psz%d" % i, [128, DM]) for i in range(2)]
                ps_ss = PS(ph, "c_psss", [128, 512])
                ps_tr = PS(ph, "c_pstr", [128, 8, 128], BF16)
                ps_lg = PS(ph, "c_pslg", [128, 512])
                ps_cnt = PS(ph, "c_pscnt", [128, 512])

                S.op("sync", lambda e: e.dma_start(out=bg[:], in_=branch_gain[l].rearrange("(c p) -> p c", p=128), allow_slow_non_contiguous=True),
                     writes=["bg"], dma="bg")
                for ch in range(8):
                    S.op("sync", lambda e, ch=ch: e.dma_start(out=wo_f[:], in_=w_out[l, ch * 128:(ch + 1) * 128, :]), writes=["wo_f"], dma="wo_f")
                    S.op("vector", lambda e, ch=ch: e.tensor_scalar(wo[:, ch, :], wo_f[:], bg[:, ch:ch + 1], None, ALU.mult),
                         reads=["wo_f", "bg"], writes=["wo"])
                S.op("sync", lambda e: e.dma_start(out=lng[:], in_=ln_gain[l, 0:1, :].partition_broadcast(128)), writes=["lng"], dma="lng")
                S.op("sync", lambda e: e.dma_start(out=lnb[:], in_=ln_bias[l, 0:1, :].partition_broadcast(128)), writes=["lnb"], dma="lnb")
                S.op("sync", lambda e: e.dma_start(out=wr_f[:, :, 0:4], in_=rgw[l].rearrange("(c p) g -> p c g", p=128), allow_slow_non_contiguous=True),
                     writes=["wr_f"], dma="wr_f")
                S.op("sync", lambda e: e.dma_start(out=wr_f[:, :, 4:36], in_=rew[l].rearrange("(c p) g -> p c g", p=128), allow_slow_non_contiguous=True),
                     writes=["wr_f"], dma="wr_f")
                S.op("vector", lambda e: e.tensor_copy(wr[:], wr_f[:]), reads=["wr_f"], writes=["wr"])
                S.op("sync", lambda e: e.dma_start(out=rb[:, 0:4], in_=rgb[l:l + 1, :].partition_broadcast(128)), writes=["rb"], dma="rb")
                S.op("sync", lambda e: e.dma_start(out=rb[:, 4:36], in_=reb[l:l + 1, :].partition_broadcast(128)), writes=["rb"], dma="rb")
                S.op("vector", lambda e: e.memset(base[:], 0.0), writes=["base"])

                parts = ((0, 4), (4, 6), (6, 8))

                def c_s0(tt):
                    g, sub = divmod(tt, 4)
                    if sub == 0:
                        mk = "mx%d" % (g % 2)
                        S.op("sync", lambda e, g=g: e.dma_start(out=mx[g % 2][:], in_=mixT.rearrange("c p t -> p c t")[:, :, g * 512:(g + 1) * 512]),
                             reads=["mix_dram"], writes=[mk], dma=mk)
                    b4 = tt % 4
                    S.op("sync", lambda e, tt=tt, b4=b4: e.dma_start(out=xa[b4][:], in_=Xin[tt * 128:(tt + 1) * 128, :]),
                         writes=["xa%d" % b4], dma="xa%d" % b4)

                def c_s0b(tt):
                    g, sub = divmod(tt, 4)
                    mk = "mx%d" % (g % 2)
                    mxg = mx[g % 2]
                    b2 = tt % 2
                    tok = slice(sub * 128, (sub + 1) * 128)
                    rk_ = "rs3_%d" % b2
                    if sub == 0:
                        S.op("scalar", lambda e: e.activation(out=sq[:], in_=mxg[:], func=AF.Square, bias=zeroc[:], scale=1.0),
                             reads=[mk, "zeroc"], writes=["sq"])
                    for pi_, (c0, c1) in enumerate(parts):
                        for ch in range(c0, c1):
                            S.op("tensor", lambda e, pi_=pi_, ch=ch, c0=c0, c1=c1: e.matmul(
                                ps_ss[:, pi_:pi_ + 1], sq[:, ch, tok], ones_bf[:, 0:1], start=(ch == c0), stop=(ch == c1 - 1), skip_group_check=True),
                                reads=["sq", "ones_bf"], writes=["ps_ss"])
                    S.op("vector", lambda e: e.tensor_tensor(out=rs3[b2][:], in0=ps_ss[:, 0:3], in1=cst[:, C_INVW:C_INVW + 3], op=ALU.mult),
                         reads=["ps_ss", "cst"], writes=[rk_])
                    S.op("scalar", lambda e: e.activation(out=rs3[b2][:], in_=rs3[b2][:], func=AF.Sqrt, bias=epsA2[:], scale=ALPHA * ALPHA),
                         reads=[rk_, "epsA2"], writes=[rk_])
                    S.op("vector", lambda e: e.reciprocal(rs3[b2][:], rs3[b2][:]), reads=[rk_], writes=[rk_])

                def c_s1(tt):
                    g, sub = divmod(tt, 4)
                    mk = "mx%d" % (g % 2)
                    mxg = mx[g % 2]
                    b2 = tt % 2
                    b4 = tt % 4
                    stt = st[b2]
                    sk_ = "st_%d" % b2
                    rk_ = "rs3_%d" % b2
                    tok = slice(sub * 128, (sub + 1) * 128)
                    ak = "accs%d" % b2
                    for pi_, (c0, c1) in enumerate(parts):
                        zi = (tt * 3 + pi_) % 2
                        zk = "psz%d" % zi
                        for half in range(2):
                            for ch in range(c0, c1):
                                S.op("tensor", lambda e, zi=zi, half=half, ch=ch, c0=c0, c1=c1: e.matmul(
                                    ps_z[zi][:, half * 512:(half + 1) * 512], mxg[:, ch, tok], wo[:, ch, half * 512:(half + 1) * 512],
                                    start=(ch == c0), stop=(ch == c1 - 1)), reads=[mk, "wo"], writes=[zk])
                        if pi_ == 0:
                            S.op("vector", lambda e, zi=zi, pi_=pi_: e.scalar_tensor_tensor(
                                out=accs[b2][:], in0=ps_z[zi][:], scalar=rs3[b2][:, pi_:pi_ + 1], in1=xa[b4][:], op0=ALU.mult, op1=ALU.add),
                                reads=[zk, rk_, "xa%d" % b4], writes=[ak])
                        else:
                            S.op("vector", lambda e, zi=zi, pi_=pi_: e.scalar_tensor_tensor(
                                out=accs[b2][:], in0=ps_z[zi][:], scalar=rs3[b2][:, pi_:pi_ + 1], in1=accs[b2][:], op0=ALU.mult, op1=ALU.add),
                                reads=[zk, rk_, ak], writes=[ak])
                    S.op("vector", lambda e: e.bn_stats(bst[b2][:, 0, :], accs[b2][:, 0:512]), reads=[ak], writes=["bst%d" % b2])
                    S.op("vector", lambda e: e.bn_stats(bst[b2][:, 1, :], accs[b2][:, 512:1024]), reads=[ak], writes=["bst%d" % b2])
                    S.op("vector", lambda e: e.bn_aggr(stt[:, 0:2], bst[b2][:]), reads=["bst%d" % b2], writes=[sk_])
                    S.op("scalar", lambda e: e.activation(out=stt[:, 2:3], in_=stt[:, 1:2], func=AF.Sqrt, bias=epsLN[:], scale=1.0), reads=[sk_, "epsLN"], writes=[sk_])
                    S.op("vector", lambda e: e.reciprocal(stt[:, 2:3], stt[:, 2:3]), reads=[sk_], writes=[sk_])

                def c_s2(tt):
                    b2 = tt % 2
                    stt = st[b2]
                    sk_ = "st_%d" % b2
                    ak = "accs%d" % b2
                    x1k = "x1%d" % b2
                    S.op("vector", lambda e: e.tensor_scalar(x1[b2][:], accs[b2][:], stt[:, 0:1], stt[:, 2:3], ALU.subtract, ALU.mult),
                         reads=[ak, sk_], writes=[x1k])
                    S.op("vector", lambda e: e.tensor_tensor(out=x1[b2][:], in0=x1[b2][:], in1=lng[:], op=ALU.mult), reads=[x1k, "lng"], writes=[x1k])
                    S.op("gpsimd", lambda e: e.tensor_tensor(out=x1[b2][:], in0=x1[b2][:], in1=lnb[:], op=ALU.add), reads=[x1k, "lnb"], writes=[x1k])
                    S.op("sync", lambda e: e.dma_start(out=X1[tt * 128:(tt + 1) * 128, :], in_=x1[b2][:]), reads=[x1k], writes=["X1_dram"], dma="x1o%d" % b2)
                    xbk = "x1b%d" % b2
                    S.op("gpsimd", lambda e: e.tensor_copy(x1b[b2][:], x1[b2][:]), reads=[x1k], writes=[xbk])
                    S.op("sync", lambda e: e.dma_start(out=X1b[tt * 128:(tt + 1) * 128, :], in_=x1b[b2][:]), reads=[xbk], writes=["X1b_dram"], dma="x1bo%d" % b2)

                def c_s2b(tt):
                    b2 = tt % 2
                    xbk = "x1b%d" % b2
                    for j in range(8):
                        S.op("tensor", lambda e, j=j: e.transpose(ps_tr[:, j, :], x1b[b2][:, j * 128:(j + 1) * 128], ident_bf[:]),
                             reads=[xbk, "ident_bf"], writes=["ps_tr"])
                    S.op("scalar", lambda e: e.copy(x1T[:], ps_tr[:]), reads=["ps_tr"], writes=["x1T"])
                    for j in range(8):
                        S.op("tensor", lambda e, j=j: e.matmul(ps_lg[:, 0:36], x1T[:, j, :], wr[:, j, :], start=(j == 0), stop=(j == 7), skip_group_check=True),
                             reads=["x1T", "wr"], writes=["ps_lg"])
                    S.op("vector", lambda e: e.tensor_tensor(out=lg[b2][:], in0=ps_lg[:, 0:36], in1=rb[:], op=ALU.add), reads=["ps_lg", "rb"], writes=["lg%d" % b2])

                def c_s3(tt):
                    b2 = tt % 2
                    lgt = lg[b2]
                    lk = "lg%d" % b2
                    S.op("vector", lambda e: e.reduce_max(rt[:, 0:1], lgt[:, 0:4], axis=AX.X), reads=[lk], writes=["rt"])
                    S.op("vector", lambda e: e.tensor_scalar(rt[:, 1:2], rt[:, 0:1], -1.0, None, ALU.mult), reads=["rt"], writes=["rt"])
                    S.op("vector", lambda e: e.memset(rt[:, 2:3], 0.0), reads=["rt"], writes=["rt"])
                    S.op("scalar", lambda e: e.activation(out=rt[:, 4:8], in_=lgt[:, 0:4], func=AF.Exp, bias=rt[:, 1:2], scale=1.0, accum_out=rt[:, 2:3]),
                         reads=[lk, "rt"], writes=["rt"])
                    S.op("vector", lambda e: e.reciprocal(rt[:, 3:4], rt[:, 2:3]), reads=["rt"], writes=["rt"])
                    S.op("vector", lambda e: e.tensor_scalar(rt[:, 8:12], lgt[:, 0:4], rt[:, 0:1], None, ALU.is_equal), reads=[lk, "rt"], writes=["rt"])
                    S.op("vector", lambda e: e.tensor_scalar(rt[:, 12:16], rt[:, 8:12], 1e30, -1e30, ALU.mult, ALU.add), reads=["rt"], writes=["rt"])
                    S.op("vector", lambda e: e.tensor_tensor(out=elm[:].rearrange("p (g j) -> p g j", g=4), in0=lgt[:, 4:36].rearrange("p (g j) -> p g j", g=4),
                                                             in1=rt[:, 12:16].unsqueeze(2).to_broadcast([128, 4, 8]), op=ALU.add),
                         reads=[lk, "rt"], writes=["elm"])
                    S.op("vector", lambda e: e.max(out=top8[:], in_=elm[:]), reads=["elm"], writes=["top8"])
                    S.op("vector", lambda e: e.tensor_scalar(M1all[:, tt, :], elm[:], top8[:, 0:1], None, ALU.is_equal), reads=["elm", "top8"], writes=["M1"])
                    S.op("vector", lambda e: e.tensor_scalar(M2all[:, tt, :], elm[:], top8[:, 1:2], None, ALU.is_equal), reads=["elm", "top8"], writes=["M2"])
                    S.op("vector", lambda e: e.tensor_tensor(out=rt[:, 16:17], in0=top8[:, 0:1], in1=top8[:, 1:2], op=ALU.subtract), reads=["top8"], writes=["rt"])
                    S.op("vector", lambda e: e.tensor_tensor(out=rt[:, 17:18], in0=top8[:, 1:2], in1=top8[:, 0:1], op=ALU.subtract), reads=["top8"], writes=["rt"])
                    S.op("scalar", lambda e: e.activation(out=rt[:, 18:20], in_=rt[:, 16:18], func=AF.Sigmoid, bias=zeroc[:], scale=1.0), reads=["rt", "zeroc"], writes=["rt"])
                    S.op("vector", lambda e: e.tensor_scalar(W01[:, tt, :], rt[:, 18:20], rt[:, 3:4], 1.0 / ALPHA, ALU.mult, ALU.mult), reads=["rt"], writes=["W01"])
                    S.op("vector", lambda e: e.tensor_tensor(out=Abf[:], in0=M1all[:, tt, :], in1=M2all[:, tt, :], op=ALU.add), reads=["M1", "M2"], writes=["Abf"])
                    S.op("tensor", lambda e: e.matmul(ps_lg[:, 64:96], ls_bf[:], Abf[:], start=True, stop=True, skip_group_check=True),
                         reads=["ls_bf", "Abf"], writes=["ps_lg"])
                    S.op("tensor", lambda e: e.matmul(ps_lg[:, 96:128], ones_bf[:], Abf[:], start=False, stop=True, skip_group_check=True),
                         reads=["ones_bf", "Abf"], writes=["ps_lg"])
                    S.op("tensor", lambda e: e.matmul(ps_cnt[0:32, 0:1], Abf[:], ones_bf[:, 0:1], start=(tt == 0), stop=(tt == NT - 1), skip_group_check=True),
                         reads=["Abf", "ones_bf"], writes=["ps_cnt"])
                    S.op("vector", lambda e: e.tensor_tensor(out=rk[:], in0=ps_lg[:, 64:96], in1=base[:], op=ALU.add), reads=["ps_lg", "base"], writes=["rk"])
                    S.op("vector", lambda e: e.tensor_tensor(out=base[:], in0=ps_lg[:, 96:128], in1=base[:], op=ALU.add), reads=["ps_lg", "base", "rk"], writes=["base"])
                    S.op("vector", lambda e: e.tensor_tensor(out=tmp32[:], in0=rk[:], in1=M1all[:, tt, :], op=ALU.mult), reads=["rk", "M1"], writes=["tmp32"])
                    S.op("vector", lambda e: e.reduce_sum(RK[:, tt, 0:1], tmp32[:], axis=AX.X), reads=["tmp32"], writes=["RK"])
                    S.op("vector", lambda e: e.tensor_tensor(out=tmp32[:], in0=rk[:], in1=M2all[:, tt, :], op=ALU.mult), reads=["rk", "M2"], writes=["tmp32"])
                    S.op("vector", lambda e: e.reduce_sum(RK[:, tt, 1:2], tmp32[:], axis=AX.X), reads=["tmp32"], writes=["RK"])

                c_s0(0)
                c_s0(1)
                c_s0b(0)
                for step in range(NT + 3):
                    if step + 2 < NT:
                        c_s0(step + 2)
                    if step + 1 < NT:
                        c_s0b(step + 1)
                    if step < NT:
                        c_s1(step)
                    if 0 <= step - 1 < NT:
                        c_s2(step - 1)
                    if 0 <= step - 2 < NT:
                        c_s2b(step - 2)
                    if 0 <= step - 3 < NT:
                        c_s3(step - 3)

                cntT = SB(ph, "c_cntT", [32, 1], F32)
                cmp_ = SB(ph, "c_cmp", [32, 33], F32)
                nblkT = SB(ph, "c_nblkT", [32, 1], F32)
                nbl_b = SB(ph, "c_nblb", [32, 128], F32)
                pe_ps = SB(ph, "c_peps", [128, 64], F32)
                cmp2 = SB(ph, "c_cmp2", [128, NBLK, NE], F32)
                ebf = SB(ph, "c_ebf", [128, NBLK], F32)
                ebf2 = SB(ph, "c_ebf2", [128, NBLK, 2], F32)
                trail = SB(ph, "c_trail", [128, NBLK], F32)
                pstart = SB(ph, "c_pstart", [128, NE], F32)
                big = SB(ph, "c_big", [128, NT, NE], F32)
                dstf = SB(ph, "c_dstf", [128, NT, 2], F32)
                S.op("vector", lambda e: e.tensor_copy(cntT[:], ps_cnt[0:32, 0:1]), reads=["ps_cnt"], writes=["cntT"])
                S.op("vector", lambda e: e.tensor_scalar(cmp_[:], cst[0:32, C_THR:C_THR + 33], cntT[:, 0:1], None, ALU.is_lt), reads=["cst", "cntT"], writes=["cmp_"])
                S.op("vector", lambda e: e.reduce_sum(nblkT[:], cmp_[:], axis=AX.X), reads=["cmp_"], writes=["nblkT"])
                S.op("vector", lambda e: e.tensor_copy(nbl_b[:], nblkT[:, 0:1].to_broadcast([32, 128])), reads=["nblkT"], writes=["nbl_b"])
                S.op("tensor", lambda e: e.matmul(ps_ss[:, 0:64], nbl_b[:], cst[0:32, C_UCAT:C_UCAT + 64], start=True, stop=True, skip_group_check=True),
                     reads=["nbl_b", "cst"], writes=["ps_ss"])
                S.op("vector", lambda e: e.tensor_copy(pe_ps[:], ps_ss[:, 0:64]), reads=["ps_ss"], writes=["pe_ps"])
                S.op("vector", lambda e: e.tensor_tensor(out=pstart[:], in0=pe_ps[:, 0:32], in1=pe_ps[:, 32:64], op=ALU.subtract), reads=["pe_ps"], writes=["pstart"])
                S.op("vector", lambda e: e.tensor_tensor(out=cmp2[:], in0=pe_ps[:, 0:32].unsqueeze(1).to_broadcast([128, NBLK, NE]),
                                                         in1=cst[:, C_BV:C_BV + NBLK].unsqueeze(2).to_broadcast([128, NBLK, NE]), op=ALU.is_le),
                     reads=["pe_ps", "cst"], writes=["cmp2"])
                S.op("vector", lambda e: e.tensor_reduce(ebf[:], cmp2[:], axis=AX.X, op=ALU.add), reads=["cmp2"], writes=["ebf"])
                S.op("vector", lambda e: e.tensor_scalar(trail[:], ebf[:], 31.5, 4.0e6, ALU.is_ge, ALU.mult), reads=["ebf"], writes=["trail"])
                S.op("vector", lambda e: e.tensor_scalar(ebf[:], ebf[:], 31.0, 128.0, ALU.min, ALU.mult), reads=["ebf"], writes=["ebf"])
                S.op("vector", lambda e: e.tensor_scalar(ebf[:], ebf[:], cst[:, C_IP:C_IP + 1], float(l * NE * 128), ALU.add, ALU.add), reads=["ebf", "cst"], writes=["ebf"])
                S.op("vector", lambda e: e.tensor_copy(GIDX[:], ebf[:]), reads=["ebf"], writes=["GIDX"])
                for h in range(2):
                    S.op("vector", lambda e, h=h: e.tensor_scalar(ebf2[:, :, h], ebf[:], 2.0, float(h), ALU.mult, ALU.add), reads=["ebf"], writes=["ebf2"])
                    S.op("vector", lambda e, h=h: e.tensor_tensor(out=ebf2[:, :, h], in0=ebf2[:, :, h], in1=trail[:], op=ALU.add), reads=["ebf2", "trail"], writes=["ebf2"])
                S.op("vector", lambda e: e.tensor_copy(GIDX2[:], ebf2[:]), reads=["ebf2"], writes=["GIDX2"])
                for k, Mk in enumerate((M1all, M2all)):
                    S.op("vector", lambda e, Mk=Mk: e.tensor_tensor(out=big[:], in0=Mk[:], in1=pstart[:].unsqueeze(1).to_broadcast([128, NT, NE]), op=ALU.mult),
                         reads=["M1", "M2", "pstart"], writes=["big"])
                    S.op("vector", lambda e, k=k: e.tensor_reduce(dstf[:, :, k], big[:], axis=AX.X, op=ALU.add), reads=["big"], writes=["dstf"])
                S.op("vector", lambda e: e.scalar_tensor_tensor(out=dstf[:], in0=dstf[:], scalar=float(BLK), in1=RK[:], op0=ALU.mult, op1=ALU.add),
                     reads=["dstf", "RK"], writes=["dstf"])
                S.op("vector", lambda e: e.tensor_copy(DSTi[:], dstf[:]), reads=["dstf"], writes=["DSTi"])
                S.barrier()
                S.emit()

            with ExitStack() as ph:
                xr = [SB(ph, "d_xr%d" % i, [128, DM], BF16) for i in range(8)]
                for tt in range(NT):
                    b4 = tt % 8
                    k = "xr%d" % b4
                    S.op("sync", lambda e, tt=tt, b4=b4: e.dma_start(out=xr[b4][:], in_=X1b[tt * 128:(tt + 1) * 128, :]), writes=[k], dma=k)
                    for kk in range(2):
                        S.op("gpsimd", lambda e, tt=tt, b4=b4, kk=kk: e.indirect_dma_start(
                            out=Xs, out_offset=bass.IndirectOffsetOnAxis(ap=DSTi[:, tt, kk:kk + 1], axis=0), in_=xr[b4][:], in_offset=None),
                            reads=[k], writes=["Xs_dram"], dma="sc%d" % b4)
                S.barrier()
                S.emit()

            with ExitStack() as ph:
                wg = [SB(ph, "e_wg%d" % i, [128, 8, DE], BF16) for i in range(2)]
                wu = [SB(ph, "e_wu%d" % i, [128, 8, DE], BF16) for i in range(2)]
                wd = [SB(ph, "e_wd%d" % i, [128, 4, DM], BF16) for i in range(2)]
                xs = [SB(ph, "e_xs%d" % i, [128, 4, DM], BF16) for i in range(2)]
                xsT = [SB(ph, "e_xsT%d" % i, [128, 8, 512], BF16) for i in range(2)]
                sg = [SB(ph, "e_sg%d" % i, [128, 512], F32) for i in range(2)]
                hT = [SB(ph, "e_hT%d" % i, [128, 4, 512], BF16) for i in range(2)]
                ysb = [SB(ph, "e_y%d" % i, [128, 4, DM], BF16) for i in range(2)]
                ps_t = [PS(ph, "e_pst%d" % i, [128, 8, 128], BF16) for i in range(2)]
                ps_g = [PS(ph, "e_psg%d" % i, [128, 512]) for i in range(2)]
                ps_u = [PS(ph, "e_psu%d" % i, [128, 512]) for i in range(2)]
                ps_y = [PS(ph, "e_psy%d" % i, [128, 512]) for i in range(2)]
                stg = [SB(ph, "e_stg%d" % i, [128, 2048], F32) for i in range(6)]
                wgv = ewg.rearrange("l e (p h j) f -> (l e p h) (j f)", h=2, j=4)
                wuv = ewu.rearrange("l e (p h j) f -> (l e p h) (j f)", h=2, j=4)
                wdv = ewd.rearrange("l e (p h j) n -> (l e p h) (j n)", h=2, j=2)
                piece = [0]
                yc_ = [0]
                def load_weights(b, which):
                    wb = b % 2
                    for (wt, wv, nm, jn) in which:
                        for h in range(2):
                            si = piece[0] % 6
                            ceng = "vector"
                            piece[0] += 1
                            S.op("gpsimd", lambda e, wv=wv, si=si, h=h: e.indirect_dma_start(
                                out=stg[si][:], out_offset=None, in_=wv,
                                in_offset=bass.IndirectOffsetOnAxis(ap=GIDX2[:, b, h:h + 1], axis=0),
                                bounds_check=S.breg, oob_is_err=False),
                                writes=["stg%d" % si], dma="stg%d" % si)
                            dst = wt[wb][:, h * jn:(h + 1) * jn, :].rearrange("p j f -> p (j f)")
                            if ceng == "vector":
                                S.op("vector", lambda e, dst=dst, si=si: e.tensor_copy(dst, stg[si][:]), reads=["stg%d" % si], writes=["%s%d" % (nm, wb)])
                            else:
                                S.op("scalar", lambda e, dst=dst, si=si: e.copy(dst, stg[si][:]), reads=["stg%d" % si], writes=["%s%d" % (nm, wb)])

                W_GU = ((wg, wgv, "wg", 4), (wu, wuv, "wu", 4))
                W_D = ((wd, wdv, "wd", 2),)

                def load_xs(b):
                    bb = b % 2
                    S.op("sync", lambda e: e.dma_start(out=xs[bb][:], in_=Xs[b * BLK:(b + 1) * BLK, :].rearrange("(s p) f -> p s f", p=128)),
                         reads=["Xs_dram"], writes=["xs%d" % bb], dma="xs%d" % bb)

                def e_front(b):
                    bb = b % 2
                    wb = bb
                    if b + 1 < NBLK:
                        load_xs(b + 1)
                    for sub in range(4):
                        ti = sub % 2
                        for j in range(8):
                            S.op("tensor", lambda e, ti=ti, j=j, sub=sub: e.transpose(
                                ps_t[ti][:, j, :], xs[bb][:, sub, :].rearrange("t (p j) -> t j p", j=8)[:, j, :], ident_bf[:]),
                                reads=["xs%d" % bb, "ident_bf"], writes=["e_pst%d" % ti])
                        S.op("vector", lambda e, ti=ti, sub=sub: e.tensor_copy(xsT[bb][:, :, sub * 128:(sub + 1) * 128], ps_t[ti][:]),
                             reads=["e_pst%d" % ti], writes=["xsT%d" % bb])
                    for jf in range(4):
                        gi = jf % 2
                        for (pst_, wt, nm) in ((ps_g, wg, "wg"), (ps_u, wu, "wu")):
                            for jd in range(8):
                                S.op("tensor", lambda e, pst_=pst_, wt=wt, gi=gi, jd=jd, jf=jf: e.matmul(
                                    pst_[gi][:], wt[wb][:, jd, :].rearrange("p (pf jf) -> p jf pf", jf=4)[:, jf, :], xsT[bb][:, jd, :],
                                    start=(jd == 0), stop=(jd == 7)), reads=["%s%d" % (nm, wb), "xsT%d" % bb], writes=["e_%s%d" % (nm, gi)])
                        S.op("scalar", lambda e, gi=gi: e.activation(out=sg[gi][:], in_=ps_g[gi][:], func=AF.Silu, bias=zeroc[:], scale=1.0),
                             reads=["e_wg%d" % gi, "zeroc"], writes=["sg%d" % gi])
                        S.op("vector", lambda e, gi=gi, jf=jf: e.tensor_tensor(out=hT[bb][:, jf, :], in0=ps_u[gi][:], in1=sg[gi][:], op=ALU.mult),
                             reads=["e_wu%d" % gi, "sg%d" % gi], writes=["hT%d" % bb])

                def e_back(b):
                    bb = b % 2
                    wb = bb
                    for sub in range(4):
                        for half in range(2):
                            yi = yc_[0] % 2
                            yc_[0] += 1
                            for jf in range(4):
                                S.op("tensor", lambda e, yi=yi, jf=jf, sub=sub, half=half: e.matmul(
                                    ps_y[yi][:], hT[bb][:, jf, sub * 128:(sub + 1) * 128], wd[wb][:, jf, half * 512:(half + 1) * 512],
                                    start=(jf == 0), stop=(jf == 3)), reads=["hT%d" % bb, "wd%d" % wb], writes=["e_psy%d" % yi])
                            S.op("scalar", lambda e, yi=yi, sub=sub, half=half: e.copy(ysb[bb][:, sub, half * 512:(half + 1) * 512], ps_y[yi][:]),
                                 reads=["e_psy%d" % yi], writes=["ysb%d" % bb])
                    S.op("sync", lambda e: e.dma_start(out=Ys[b * BLK:(b + 1) * BLK, :].rearrange("(s p) f -> p s f", p=128), in_=ysb[bb][:]),
                         reads=["ysb%d" % bb], writes=["Ys_dram"], dma="ysb%d" % bb)

                load_weights(0, W_GU)
                load_weights(0, W_D)
                load_xs(0)
                for b in range(NBLK + 1):
                    if b < NBLK:
                        e_front(b)
                        if b + 1 < NBLK:
                            load_weights(b + 1, W_GU)
                    if b >= 1:
                        e_back(b - 1)
                    if b + 1 < NBLK:
                        load_weights(b + 1, W_D)
                S.barrier()
                S.emit(want_breg=True)

            with ExitStack() as ph:
                lng = SB(ph, "f_lng", [128, DM], F32)
                lnb = SB(ph, "f_lnb", [128, DM], F32)
                y0 = [SB(ph, "f_y0%d" % i, [128, DM], BF16) for i in range(4)]
                y1 = [SB(ph, "f_y1%d" % i, [128, DM], BF16) for i in range(4)]
                xa = [SB(ph, "f_xa%d" % i, [128, DM], F32) for i in range(4)]
                accs = [SB(ph, "f_acc%d" % i, [128, DM], F32) for i in range(2)]
                junk = SB(ph, "f_junk", [128, DM], F32)
                st = [SB(ph, "f_st%d" % i, [128, 16], F32) for i in range(2)]
                bst = [SB(ph, "f_bst%d" % i, [128, 2, 6], F32) for i in range(2)]
                x2 = [SB(ph, "f_x2%d" % i, [128, DM], F32) for i in range(2)]
                xb = [SB(ph, "f_xb%d" % i, [128, DM], BF16) for i in range(4)]
                ps_t = [PS(ph, "f_pst%d" % i, [128, 8, 128], BF16) for i in range(2)]
                xTg = SB(ph, "f_xTg", [128, 8, 512], BF16)
                S.op("sync", lambda e: e.dma_start(out=lng[:], in_=ln_gain[l, 1:2, :].partition_broadcast(128)), writes=["lng"], dma="lng")
                S.op("sync", lambda e: e.dma_start(out=lnb[:], in_=ln_bias[l, 1:2, :].partition_broadcast(128)), writes=["lnb"], dma="lnb")
                last = (l == L - 1)

                def f_s0(tt):
                    b4 = tt % 4
                    S.op("sync", lambda e: e.dma_start(out=xa[b4][:], in_=X1[tt * 128:(tt + 1) * 128, :]),
                         reads=["X1_dram"], writes=["xa%d" % b4], dma="xa%d" % b4)
                    for kk, yt in enumerate((y0, y1)):
                        S.op("gpsimd", lambda e, kk=kk, yt=yt: e.indirect_dma_start(
                            out=yt[b4][:], out_offset=None, in_=Ys, in_offset=bass.IndirectOffsetOnAxis(ap=DSTi[:, tt, kk:kk + 1], axis=0)),
                            reads=["Ys_dram"], writes=["y%d_%d" % (kk, b4)], dma="y%d_%d" % (kk, b4))

                def f_s1(tt):
                    b2 = tt % 2
                    b4 = tt % 4
                    stt = st[b2]
                    sk_ = "st_%d" % b2
                    ak = "accs%d" % b2
                    S.op("vector", lambda e: e.scalar_tensor_tensor(out=accs[b2][:], in0=y0[b4][:], scalar=W01[:, tt, 0:1], in1=xa[b4][:],
                                                                    op0=ALU.mult, op1=ALU.add), reads=["y0_%d" % b4, "xa%d" % b4], writes=[ak])
                    S.op("vector", lambda e: e.scalar_tensor_tensor(out=accs[b2][:], in0=y1[b4][:], scalar=W01[:, tt, 1:2], in1=accs[b2][:],
                                                                    op0=ALU.mult, op1=ALU.add), reads=["y1_%d" % b4, ak], writes=[ak])
                    S.op("vector", lambda e: e.bn_stats(bst[b2][:, 0, :], accs[b2][:, 0:512]), reads=[ak], writes=["bst%d" % b2])
                    S.op("vector", lambda e: e.bn_stats(bst[b2][:, 1, :], accs[b2][:, 512:1024]), reads=[ak], writes=["bst%d" % b2])
                    S.op("vector", lambda e: e.bn_aggr(stt[:, 0:2], bst[b2][:]), reads=["bst%d" % b2], writes=[sk_])
                    S.op("scalar", lambda e: e.activation(out=stt[:, 2:3], in_=stt[:, 1:2], func=AF.Sqrt, bias=epsLN[:], scale=1.0), reads=[sk_, "epsLN"], writes=[sk_])
                    S.op("vector", lambda e: e.reciprocal(stt[:, 2:3], stt[:, 2:3]), reads=[sk_], writes=[sk_])

                def f_s2(tt):
                    g, sub = divmod(tt, 4)
                    b2 = tt % 2
                    stt = st[b2]
                    sk_ = "st_%d" % b2
                    ak = "accs%d" % b2
                    x2k = "x2%d" % b2
                    S.op("vector", lambda e: e.tensor_scalar(x2[b2][:], accs[b2][:], stt[:, 0:1], stt[:, 2:3], ALU.subtract, ALU.mult),
                         reads=[ak, sk_], writes=[x2k])
                    S.op("vector", lambda e: e.tensor_tensor(out=x2[b2][:], in0=x2[b2][:], in1=lng[:], op=ALU.mult), reads=[x2k, "lng"], writes=[x2k])
                    S.op("gpsimd", lambda e: e.tensor_tensor(out=x2[b2][:], in0=x2[b2][:], in1=lnb[:], op=ALU.add), reads=[x2k, "lnb"], writes=[x2k])
                    S.op("sync", lambda e: e.dma_start(out=Xout[tt * 128:(tt + 1) * 128, :], in_=x2[b2][:]), reads=[x2k], writes=["X2_dram"], dma="x2o%d" % b2)
                    if not last:
                        S.op("scalar", lambda e: e.copy(xb[sub][:], x2[b2][:]), reads=[x2k], writes=["xb%d" % sub])
                        if sub == 3:
                            emit_xT_group((ps_t, xTg), lambda s_: "xb%d" % s_, lambda s_: xb[s_], g)

                f_s0(0)
                f_s0(1)
                for step in range(NT + 1):
                    if step + 2 < NT:
                        f_s0(step + 2)
                    if step < NT:
                        f_s1(step)
                    if 0 <= step - 1 < NT:
                        f_s2(step - 1)
                S.barrier()
                S.emit()
    return nc


def make_consts():
    c = np.zeros((128, C_END), np.float32)
    c[:, C_ID:C_ID + 128] = np.eye(128, dtype=np.float32)
    m = np.arange(128)
    sw = np.where((m % 64) < 32, m + 32, m - 32)
    perm = np.zeros((128, 128), np.float32)
    perm[sw, m] = 1.0
    c[:, C_PERM:C_PERM + 128] = perm
    j = np.arange(128)[:, None]
    i = np.arange(128)[None, :]
    c[:, C_MASK:C_MASK + 128] = np.where(i >= j, 0.0, NEG)
    c[:, C_MASK + 128:C_MASK + 256] = np.where(j >= i, 0.0, NEG)
    c[:, C_LS:C_LS + 128] = (j < i).astype(np.float32)
    e1 = np.arange(32)[:, None]
    e2 = np.arange(32)[None, :]
    c[0:32, C_UCAT:C_UCAT + 32] = (e1 <= e2).astype(np.float32)
    c[0:32, C_UCAT + 32:C_UCAT + 64] = np.eye(32, dtype=np.float32)
    c[:, C_THR:C_THR + 33] = (np.arange(33) * BLK).astype(np.float32)[None, :]
    c[:, C_BV:C_BV + NBLK] = np.arange(NBLK, dtype=np.float32)[None, :]
    c[:, C_IE:C_IE + 32] = np.arange(32, dtype=np.float32)[None, :]
    c[:, C_IP] = np.arange(128, dtype=np.float32)
    c[64, C_SEL:C_SEL + 64] = 1.0
    c[:, C_INVW:C_INVW + 3] = np.array([1.0 / 512, 1.0 / 256, 1.0 / 256], np.float32)[None, :]
    half = 32
    inv_freq = (np.float32(10000.0) ** (-np.arange(half, dtype=np.float32) / np.float32(half))).astype(np.float32)
    ang = (np.arange(S_LEN, dtype=np.float32)[:, None] * inv_freq[None, :]).astype(np.float32)
    cos = np.cos(ang).astype(np.float32).T
    sin = np.sin(ang).astype(np.float32).T
    rc = np.zeros((128, S_LEN), np.float32)
    rs = np.zeros((128, S_LEN), np.float32)
    for p in range(128):
        cc = p % 64
        rc[p] = cos[cc % 32]
        rs[p] = -sin[cc] if cc < 32 else sin[cc - 32]
    return c, rc, rs


_CACHE = {}


def kernel(**inputs):
    depth = DEPTH
    if "nc" not in _CACHE:
        _CACHE["nc"] = build_program(depth)
        _CACHE["consts"] = make_consts()
    nc = _CACHE["nc"]
    c, rc, rs = _CACHE["consts"]
    x = np.ascontiguousarray(np.asarray(inputs["x"], dtype=np.float32))
    shared = {k: np.ascontiguousarray(np.asarray(v, dtype=np.float32)) for k, v in inputs.items() if k != "x"}
    shared["consts"] = c
    shared["ropecos"] = rc
    shared["ropesin"] = rs
    in_maps = []
    for i in range(NCORES):
        m = dict(shared)
        m["x"] = x[i]
        in_maps.append(m)
    res = run_bass_kernel_spmd(nc, in_maps, core_ids=list(range(NCORES)))
    return np.stack([np.asarray(r["out"], dtype=np.float32) for r in res.results], axis=0)
```

```python
import os
import numpy as np
from contextlib import ExitStack
import concourse.bass as bass
import concourse.mybir as mybir
from concourse.bass_utils import run_bass_kernel_spmd

F32 = mybir.dt.float32
BF16 = mybir.dt.bfloat16
I32 = mybir.dt.int32
AF = mybir.ActivationFunctionType
ALU = mybir.AluOpType
AX = mybir.AxisListType

NCORES = 8
S_LEN = 8192
DM = 1024
DEPTH = 4
PW = 2816
NE = 32
DE = 512
NT = S_LEN // 128
NG = S_LEN // 512
BLK = 512
NBLK = 63
NSLOT = NBLK * BLK
ALPHA = (2.0 * DEPTH) ** 0.25
EPS = 1e-5
BRANCH_D = (1, 4, 16)
NEG = -30000.0

C_ID, C_PERM, C_MASK, C_LS, C_UCAT, C_THR, C_BV, C_IE, C_IP, C_SEL, C_INVW, C_END = (
    0, 128, 256, 512, 640, 704, 737, 801, 833, 834, 898, 904)

ENGS = ["tensor", "vector", "scalar", "gpsimd", "sync"]


class Sched:
    def __init__(self, nc, stack):
        self.nc = nc
        self.stack = stack
        self.q = {e: [] for e in ENGS}
        self.sems = {}
        self.cnt = {}
        self.seen = {e: {} for e in ENGS}
        self.last_w = {}
        self.readers = {}
        self.epoch = 0
        for e in ENGS:
            self._sem("E_%s_0" % e)
        self.n_ops = 0
        self.breg = None
        self.breg_val = None

    def new_epoch(self):
        self.epoch += 1
        self._sem("E_tensor_%d" % self.epoch)

    def _sem(self, name):
        if name not in self.sems:
            self.sems[name] = self.stack.enter_context(self.nc.semaphore(name))
            self.cnt[name] = 0
        return self.sems[name]

    def op(self, eng, fn, reads=(), writes=(), dma=None):
        pr = [k for k in reads if k.startswith(("ps", "e_ps", "e_wg", "e_wu"))]
        if pr:
            writes = list(writes) + pr
        deps = {}

        def add(ev):
            if ev is not None and deps.get(ev[0], 0) < ev[1]:
                deps[ev[0]] = ev[1]

        for k in reads:
            add(self.last_w.get(k))
        for k in writes:
            add(self.last_w.get(k))
            for ev in self.readers.get(k, ()):
                add(ev)
        waits = []
        seen = self.seen[eng]
        for s, v in deps.items():
            if eng == "tensor" and s.startswith("E_tensor"):
                continue
            if seen.get(s, 0) < v:
                seen[s] = v
                waits.append((self.sems[s], v))
        if dma is None:
            sname, inc = "E_%s_%d" % (eng, self.epoch if eng == "tensor" else 0), 1
        else:
            sname, inc = "D_" + dma, 16
            self._sem(sname)
        self.cnt[sname] += inc
        ev = (sname, self.cnt[sname])
        self.q[eng].append((waits, fn, self.sems[sname], inc))
        for k in reads:
            self.readers.setdefault(k, []).append(ev)
        for k in writes:
            self.last_w[k] = ev
            self.readers[k] = []
        self.n_ops += 1
        return ev

    def barrier(self):
        for e in ENGS:
            waits = []
            for s, c in self.cnt.items():
                if c > 0 and self.seen[e].get(s, 0) < c:
                    self.seen[e][s] = c
                    waits.append((self.sems[s], c))
            if waits:
                self.q[e].append((waits, None, None, 0))
        self.last_w = {}
        self.readers = {}

    def emit(self, want_breg=False):
        with self.nc.Block() as block:
            for e in ENGS:
                items = self.q[e]

                def body(engine, items=items, e=e):
                    if e == "gpsimd" and want_breg:
                        self.breg = engine.to_reg(self.breg_val)
                    for waits, fn, sem, inc in items:
                        for s, v in waits:
                            engine.wait_ge(s, v)
                        if fn is not None:
                            fn(engine).then_inc(sem, inc)

                getattr(block, e)(body)
        self.q = {e: [] for e in ENGS}


def build_program(depth=DEPTH, dbg=None):
    nc = bass.Bass("TRN2", target_bir_lowering=False)
    dt_in = lambda name, shape, dt=F32: nc.dram_tensor(name, shape, dt, kind="ExternalInput").ap()
    dt_sc = lambda name, shape, dt: nc.dram_tensor(name, shape, dt, kind=("ExternalOutput" if dbg else "Internal")).ap()
    L = depth
    x_in = dt_in("x", [S_LEN, DM])
    w_in = dt_in("w_in", [L, DM, PW])
    w_out = dt_in("w_out", [L, DM, DM])
    branch_gain = dt_in("branch_gain", [L, DM])
    sgu_gain = dt_in("sgu_gain", [L, 256])
    sgu_w = dt_in("sgu_w", [L, 4, 128, 128])
    sgu_b = dt_in("sgu_b", [L, 4, 128])
    conv_w = dt_in("conv_w", [L, 3, 256])
    ln_gain = dt_in("ln_gain", [L, 2, DM])
    ln_bias = dt_in("ln_bias", [L, 2, DM])
    rgw = dt_in("router_group_w", [L, DM, 4])
    rgb = dt_in("router_group_b", [L, 4])
    rew = dt_in("router_expert_w", [L, DM, NE])
    reb = dt_in("router_expert_b", [L, NE])
    ewg = dt_in("expert_w_gate", [L, NE, DM, DE])
    ewu = dt_in("expert_w_up", [L, NE, DM, DE])
    ewd = dt_in("expert_w_down", [L, NE, DE, DM])
    consts = dt_in("consts", [128, C_END])
    ropecos = dt_in("ropecos", [128, S_LEN])
    ropesin = dt_in("ropesin", [128, S_LEN])
    out = nc.dram_tensor("out", [S_LEN, DM], F32, kind="ExternalOutput").ap()

    xT = dt_sc("xT", [8, 128, S_LEN], BF16)
    QT = dt_sc("QT", [4, 128, S_LEN], BF16)
    KT = dt_sc("KT", [4, 128, S_LEN], BF16)
    Vd = dt_sc("Vd", [S_LEN, 512], BF16)
    mixT = dt_sc("mixT", [8, 128, S_LEN], BF16)
    X1 = dt_sc("X1", [S_LEN, DM], F32)
    X1b = dt_sc("X1b", [S_LEN, DM], BF16)
    X2 = dt_sc("X2", [S_LEN, DM], F32)
    Xs = dt_sc("Xs", [NSLOT, DM], BF16)
    Ys = dt_sc("Ys", [NSLOT, DM], BF16)

    with ExitStack() as top:
        S = Sched(nc, top)
        S.breg_val = L * NE * 128 * 2 - 1

        uniq = [0]

        def SB(st, name, shape, dt):
            uniq[0] += 1
            return st.enter_context(nc.sbuf_tensor("%s_%d" % (name, uniq[0]), shape, dt))

        def PS(st, name, shape, dt=F32):
            uniq[0] += 1
            return st.enter_context(nc.psum_tensor("%s_%d" % (name, uniq[0]), shape, dt))

        cst = SB(top, "cst", [128, C_END], F32)
        ident_bf = SB(top, "ident_bf", [128, 128], BF16)
        perm_bf = SB(top, "perm_bf", [128, 128], BF16)
        mask_bf = SB(top, "mask_bf", [128, 256], BF16)
        ls_bf = SB(top, "ls_bf", [128, 128], BF16)
        ones_bf = SB(top, "ones_bf", [128, 128], BF16)
        zeroc = SB(top, "zeroc", [128, 1], F32)
        epsc = SB(top, "epsc", [128, 1], F32)
        epsA2 = SB(top, "epsA2", [128, 1], F32)
        epsLN = SB(top, "epsLN", [128, 1], F32)
        M1all = SB(top, "M1all", [128, NT, NE], F32)
        M2all = SB(top, "M2all", [128, NT, NE], F32)
        RK = SB(top, "RK", [128, NT, 2], F32)
        W01 = SB(top, "W01", [128, NT, 2], F32)
        DSTi = SB(top, "DSTi", [128, NT, 2], I32)
        GIDX = SB(top, "GIDX", [128, NBLK], I32)
        GIDX2 = SB(top, "GIDX2", [128, NBLK, 2], I32)

        S.op("sync", lambda e: e.dma_start(out=cst[:], in_=consts), writes=["cst"], dma="cst")
        S.op("vector", lambda e: e.tensor_copy(ident_bf[:], cst[:, C_ID:C_ID + 128]), reads=["cst"], writes=["ident_bf"])
        S.op("vector", lambda e: e.tensor_copy(perm_bf[:], cst[:, C_PERM:C_PERM + 128]), reads=["cst"], writes=["perm_bf"])
        S.op("vector", lambda e: e.tensor_copy(mask_bf[:], cst[:, C_MASK:C_MASK + 256]), reads=["cst"], writes=["mask_bf"])
        S.op("vector", lambda e: e.tensor_copy(ls_bf[:], cst[:, C_LS:C_LS + 128]), reads=["cst"], writes=["ls_bf"])
        S.op("vector", lambda e: e.memset(ones_bf[:], 1.0), writes=["ones_bf"])
        S.op("vector", lambda e: e.memset(zeroc[:], 0.0), writes=["zeroc"])
        S.op("vector", lambda e: e.memset(epsc[:], EPS), writes=["epsc"])
        S.op("vector", lambda e: e.memset(epsA2[:], EPS * ALPHA * ALPHA), writes=["epsA2"])
        S.op("vector", lambda e: e.memset(epsLN[:], EPS / (ALPHA * ALPHA)), writes=["epsLN"])
        S.barrier()
        S.emit()

        def emit_xT_group(st_tiles, src_key_fn, src_ap_fn, g):
            ps_t, xTg = st_tiles
            for sub in range(4):
                pk = "ps_xt%d" % (sub % 2)
                for j in range(8):
                    S.op("tensor", lambda e, sub=sub, j=j: e.transpose(
                        ps_t[sub % 2][:, j, :], src_ap_fn(sub)[:, j * 128:(j + 1) * 128], ident_bf[:]),
                        reads=[src_key_fn(sub), "ident_bf"], writes=[pk])
                S.op("scalar", lambda e, sub=sub: e.copy(xTg[:, :, sub * 128:(sub + 1) * 128], ps_t[sub % 2][:]),
                     reads=[pk], writes=["xTg"])
            S.op("sync", lambda e: e.dma_start(
                out=xT.rearrange("c p t -> p c t")[:, :, g * 512:(g + 1) * 512], in_=xTg[:]),
                reads=["xTg"], writes=["xT_dram"], dma="xTg")

        with ExitStack() as ph:
            xin = [SB(ph, "p_xin%d" % i, [128, DM], F32) for i in range(2)]
            xb = [SB(ph, "p_xb%d" % i, [128, DM], BF16) for i in range(4)]
            ps_t = [PS(ph, "p_pst%d" % i, [128, 8, 128], BF16) for i in range(2)]
            xTg = SB(ph, "p_xTg", [128, 8, 512], BF16)
            for g in range(NG):
                for sub in range(4):
                    tt = g * 4 + sub
                    k = "xin%d" % (tt % 2)
                    S.op("sync", lambda e, tt=tt: e.dma_start(out=xin[tt % 2][:], in_=x_in[tt * 128:(tt + 1) * 128, :]),
                         writes=[k], dma=k)
                    S.op("vector", lambda e, tt=tt, sub=sub: e.tensor_copy(xb[sub][:], xin[tt % 2][:]),
                         reads=[k], writes=["xb%d" % sub])
                emit_xT_group((ps_t, xTg), lambda sub: "xb%d" % sub, lambda sub: xb[sub], g)
            S.barrier()
            S.emit()

        for l in range(L):
            if l > 0:
                S.new_epoch()
            Xin = x_in if l == 0 else X2
            Xout = out if l == L - 1 else X2
            with ExitStack() as ph:
                win = SB(ph, "a_win", [128, 8, PW], BF16)
                xt = [SB(ph, "a_xt%d" % i, [128, 8, 512], BF16) for i in range(2)]
                cs = [SB(ph, "a_cos%d" % i, [128, 512], F32) for i in range(2)]
                sn = [SB(ph, "a_sin%d" % i, [128, 512], F32) for i in range(2)]
                qbf = [SB(ph, "a_qbf%d" % i, [128, 512], BF16) for i in range(2)]
                t1 = [SB(ph, "a_t1%d" % i, [128, 512], F32) for i in range(2)]
                t2 = [SB(ph, "a_t2%d" % i, [128, 512], F32) for i in range(2)]
                qr = [SB(ph, "a_qr%d" % i, [128, 512], BF16) for i in range(2)]
                vsb = [SB(ph, "a_v%d" % i, [128, 4, 512], BF16) for i in range(2)]
                ga = [SB(ph, "a_ga%d" % i, [128, 512], F32) for i in range(2)]
                gt = [SB(ph, "a_gt%d" % i, [128, 512], F32) for i in range(2)]
                ug = [SB(ph, "a_ug%d" % i, [64, 512], F32) for i in range(4)]
                zg = [SB(ph, "a_zg%d" % i, [128, 256], F32) for i in range(2)]
                zsq = SB(ph, "a_zsq", [128, 256], F32)
                zst = SB(ph, "a_zst", [128, 16], F32)
                zn = [SB(ph, "a_zn%d" % i, [128, 4, 64], BF16) for i in range(4)]
                spv = [SB(ph, "a_spv%d" % i, [64, 512], F32) for i in range(2)]
                yb = [SB(ph, "a_yb%d" % i, [64, 512], BF16) for i in range(2)]
                wsf = SB(ph, "a_wsf", [128, 4, 128], F32)
                wsT = SB(ph, "a_wsT", [128, 4, 128], BF16)
                bsb = SB(ph, "a_bsb", [64, 4, 128], F32)
                sgain = SB(ph, "a_sgain", [64, 4], F32)
                cw = SB(ph, "a_cw", [128, 2, 3], F32)
                hs = [SB(ph, "a_hs%d" % i, [128, 512], F32) for i in range(2)]
                gbs = [SB(ph, "a_gbs%d" % i, [128, 512], F32) for i in range(2)]
                zce = SB(ph, "a_zce", [128, 2, 514], F32)
                yc = [SB(ph, "a_yc%d" % i, [128, 512], F32) for i in range(2)]
                ycb = [SB(ph, "a_ycb%d" % i, [128, 512], BF16) for i in range(2)]
                psA = [PS(ph, "a_ps%d" % i, [128, 512]) for i in range(7)]
                psW = psA[6][:].rearrange("p (g t) -> p g t", g=4)

                for dc in range(8):
                    S.op("gpsimd", lambda e, dc=dc: e.dma_start(out=win[:, dc, :], in_=w_in[l, dc * 128:(dc + 1) * 128, :]),
                         writes=["win"], dma="win")
                S.op("sync", lambda e: e.dma_start(out=wsf[:], in_=sgu_w[l].rearrange("g t s -> t g s")), writes=["wsf"], dma="wsf")
                for g in range(4):
                    S.op("gpsimd", lambda e, g=g: e.affine_select(out=wsf[:, g, :], in_=wsf[:, g, :], pattern=[[-1, 128]],
                                                                  compare_op=ALU.is_ge, fill=0.0, base=0, channel_multiplier=1),
                         reads=["wsf"], writes=["wsf"])
                for g in range(4):
                    S.op("tensor", lambda e, g=g: e.transpose(psW[:, g, :], wsf[:, g, :], cst[:, C_ID:C_ID + 128]),
                         reads=["wsf", "cst"], writes=["psA6"])
                S.op("vector", lambda e: e.tensor_copy(wsT[:], psW), reads=["psA6"], writes=["wsT"])
                for g in range(4):
                    S.op("sync", lambda e, g=g: e.dma_start(out=bsb[:, g, :], in_=sgu_b[l, g:g + 1, :].partition_broadcast(64)),
                         writes=["bsb"], dma="bsb")
                S.op("sync", lambda e: e.dma_start(out=sgain[:], in_=sgu_gain[l].rearrange("(g c) -> c g", g=4),
                                                   allow_slow_non_contiguous=True), writes=["sgain"], dma="sgain")
                for cc in range(2):
                    for k in range(3):
                        S.op("sync", lambda e, cc=cc, k=k: e.dma_start(out=cw[:, cc, k:k + 1], in_=conv_w[l, k:k + 1, cc * 128:(cc + 1) * 128].rearrange("k p -> p k"),
                                                                       allow_slow_non_contiguous=True), writes=["cw"], dma="cw")
                S.op("vector", lambda e: e.memset(zce[:], 0.0), writes=["zce"])

                psn = [0]

                def next_ps():
                    i = psn[0] % 7
                    psn[0] += 1
                    return i

                def gelu(src_ap, src_key, out_ap, out_key, P, N, bi):
                    a = ga[bi][0:P, 0:N]
                    t = gt[bi][0:P, 0:N]
                    ak, tk = "ga%d" % bi, "gt%d" % bi
                    S.op("scalar", lambda e: e.copy(a, src_ap), reads=[src_key], writes=[ak])
                    S.op("gpsimd", lambda e: e.tensor_tensor(out=t, in0=a, in1=a, op=ALU.mult), reads=[ak], writes=[tk])
                    S.op("vector", lambda e: e.tensor_scalar(t, t, 0.044715, 1.0, ALU.mult, ALU.add), reads=[tk], writes=[tk])
                    S.op("vector", lambda e: e.tensor_tensor(out=t, in0=t, in1=a, op=ALU.mult), reads=[tk, ak], writes=[tk])
                    S.op("scalar", lambda e: e.activation(out=t, in_=t, func=AF.Sigmoid, bias=zeroc[0:P, :], scale=1.5957691216),
                         reads=[tk, "zeroc"], writes=[tk])
                    S.op("vector", lambda e: e.tensor_tensor(out=out_ap, in0=a, in1=t, op=ALU.mult), reads=[ak, tk], writes=[out_key])

                gcount = [0]
                def a_load(i):
                    S.op("sync", lambda e: e.dma_start(out=xt[i % 2][:], in_=xT.rearrange("c p t -> p c t")[:, :, i * 512:(i + 1) * 512]),
                         reads=["xT_dram"], writes=["xt%d" % (i % 2)], dma="xt%d" % (i % 2))
                    S.op("sync", lambda e: e.dma_start(out=cs[i % 2][:], in_=ropecos[:, i * 512:(i + 1) * 512]),
                         writes=["cs%d" % (i % 2)], dma="cs%d" % (i % 2))
                    S.op("sync", lambda e: e.dma_start(out=sn[i % 2][:], in_=ropesin[:, i * 512:(i + 1) * 512]),
                         writes=["sn%d" % (i % 2)], dma="sn%d" % (i % 2))

                a_load(0)
                for i in range(NG):
                    tsl = slice(i * 512, (i + 1) * 512)
                    xk = "xt%d" % (i % 2)
                    xti = xt[i % 2]
                    csk, snk = "cs%d" % (i % 2), "sn%d" % (i % 2)
                    def qk_main(qi, xti=xti):
                        c = qi % 4
                        isq = qi < 4
                        col0 = (0 if isq else 512) + c * 128
                        scale = 0.125 if isq else 1.0
                        p = next_ps()
                        pk = "psA%d" % p
                        for dc in range(8):
                            S.op("tensor", lambda e, dc=dc: e.matmul(
                                psA[p][:], win[:, dc, col0:col0 + 128], xti[:, dc, :], start=(dc == 0), stop=(dc == 7)),
                                reads=["win", xk], writes=[pk])
                        b2 = qi % 2
                        S.op("scalar", lambda e: e.activation(
                            out=qbf[b2][:], in_=psA[p][:], func=AF.Identity, bias=zeroc[:], scale=scale),
                            reads=[pk, "zeroc"], writes=["qbf%d" % b2])
                        return p

                    def qk_rope(qi, p, i=i, xti=xti):
                        c = qi % 4
                        isq = qi < 4
                        scale = 0.125 if isq else 1.0
                        pk = "psA%d" % p
                        b2 = qi % 2
                        S.op("vector", lambda e: e.scalar_tensor_tensor(
                            out=t2[b2][:], in0=psA[p][:], scalar=scale, in1=cs[i % 2][:], op0=ALU.mult, op1=ALU.mult),
                            reads=[pk, csk], writes=["t2%d" % b2])
                        p2 = next_ps()
                        if p2 == p:
                            p2 = next_ps()
                        pk2 = "psA%d" % p2
                        S.op("tensor", lambda e: e.matmul(psA[p2][:], perm_bf[:], qbf[b2][:], start=True, stop=True),
                             reads=["perm_bf", "qbf%d" % b2], writes=[pk2])
                        S.op("vector", lambda e: e.tensor_tensor(
                            out=t1[b2][:], in0=psA[p2][:], in1=sn[i % 2][:], op=ALU.mult),
                            reads=[pk2, snk], writes=["t1%d" % b2])
                        S.op("gpsimd", lambda e: e.tensor_tensor(out=qr[b2][:], in0=t1[b2][:], in1=t2[b2][:], op=ALU.add),
                             reads=["t1%d" % b2, "t2%d" % b2], writes=["qr%d" % b2])
                        dst = QT if isq else KT
                        S.op("sync", lambda e: e.dma_start(
                            out=dst[c, :, i * 512:(i + 1) * 512], in_=qr[b2][:]),
                            reads=["qr%d" % b2], writes=["QK_dram"], dma="qr%d" % b2)

                    pend = None
                    for qi in range(8):
                        p_ = qk_main(qi)
                        if pend is not None:
                            qk_rope(*pend)
                        pend = (qi, p_)
                    qk_rope(*pend)
                    for cc in range(2):
                        pgb, pgc, ph_ = next_ps(), next_ps(), next_ps()
                        for (p, col0) in ((pgb, 2048), (pgc, 2304), (ph_, 2560)):
                            for dc in range(8):
                                S.op("tensor", lambda e, p=p, dc=dc, col0=col0, cc=cc, xti=xti: e.matmul(
                                    psA[p][:], win[:, dc, col0 + cc * 128:col0 + (cc + 1) * 128], xti[:, dc, :], start=(dc == 0), stop=(dc == 7)),
                                    reads=["win", xk], writes=["psA%d" % p])
                        S.op("scalar", lambda e, cc=cc, ph_=ph_: e.copy(hs[cc][:], psA[ph_][:]), reads=["psA%d" % ph_], writes=["hs%d" % cc])
                        S.op("scalar", lambda e, cc=cc, pgb=pgb: e.copy(gbs[cc][:], psA[pgb][:]), reads=["psA%d" % pgb], writes=["gbs%d" % cc])
                        zk = "zce%d" % cc
                        if i > 0:
                            S.op("gpsimd", lambda e, cc=cc: e.tensor_copy(zce[:, cc, 0:2], zce[:, cc, 512:514]), reads=[zk, "zce"], writes=[zk])
                        S.op("vector", lambda e, cc=cc, pgc=pgc: e.tensor_tensor(out=zce[:, cc, 2:514], in0=psA[pgc][:], in1=hs[cc][:], op=ALU.mult),
                             reads=["psA%d" % pgc, "hs%d" % cc, "zce"], writes=[zk])
                        S.op("scalar", lambda e, cc=cc: e.activation(out=yc[cc][:], in_=zce[:, cc, 2:514], func=AF.Identity, bias=zeroc[:], scale=cw[:, cc, 2:3]),
                             reads=[zk, "cw", "zeroc"], writes=["yc%d" % cc])
                        S.op("vector", lambda e, cc=cc: e.scalar_tensor_tensor(out=yc[cc][:], in0=zce[:, cc, 1:513], scalar=cw[:, cc, 1:2], in1=yc[cc][:],
                                                                               op0=ALU.mult, op1=ALU.add), reads=[zk, "cw", "yc%d" % cc], writes=["yc%d" % cc])
                        S.op("vector", lambda e, cc=cc: e.scalar_tensor_tensor(out=yc[cc][:], in0=zce[:, cc, 0:512], scalar=cw[:, cc, 0:1], in1=yc[cc][:],
                                                                               op0=ALU.mult, op1=ALU.add), reads=[zk, "cw", "yc%d" % cc], writes=["yc%d" % cc])
                        S.op("gpsimd", lambda e, cc=cc: e.tensor_tensor(out=ycb[cc][:], in0=gbs[cc][:], in1=yc[cc][:], op=ALU.mult),
                             reads=["gbs%d" % cc, "yc%d" % cc], writes=["ycb%d" % cc])
                        S.op("sync", lambda e, cc=cc, i=i: e.dma_start(out=mixT[6 + cc, :, i * 512:(i + 1) * 512], in_=ycb[cc][:]),
                             reads=["ycb%d" % cc], writes=["mix_dram"], dma="ycb%d" % cc)
                    if i + 1 < NG:
                        a_load(i + 1)
                    vb = i % 2
                    for sub in range(4):
                        p = next_ps()
                        pk = "psA%d" % p
                        for dc in range(8):
                            S.op("tensor", lambda e, p=p, dc=dc, sub=sub, xti=xti: e.matmul(
                                psA[p][:], xti[:, dc, sub * 128:(sub + 1) * 128], win[:, dc, 1024:1536], start=(dc == 0), stop=(dc == 7)),
                                reads=["win", xk], writes=[pk])
                        S.op("scalar", lambda e, p=p, sub=sub, vb=vb: e.copy(vsb[vb][:, sub, :], psA[p][:]),
                             reads=[pk], writes=["vsb%d" % vb])
                    S.op("sync", lambda e, vb=vb, i=i: e.dma_start(
                        out=Vd[i * 512:(i + 1) * 512, :].rearrange("(s p) f -> p s f", p=128), in_=vsb[vb][:]),
                        reads=["vsb%d" % vb], writes=["V_dram"], dma="vsb%d" % vb)
                    for sub in range(4):
                        p = next_ps()
                        pk = "psA%d" % p
                        for dc in range(8):
                            S.op("tensor", lambda e, p=p, dc=dc, sub=sub, xti=xti: e.matmul(
                                psA[p][:, 0:256], xti[:, dc, sub * 128:(sub + 1) * 128], win[:, dc, 1792:2048], start=(dc == 0), stop=(dc == 7)),
                                reads=["win", xk], writes=[pk])
                        zb = sub % 2
                        zk = "zg%d" % zb
                        gelu(psA[p][:, 0:256], pk, zg[zb][:], zk, 128, 256, gcount[0] % 2)
                        gcount[0] += 1
                        z3 = zg[zb][:].rearrange("p (g c) -> p g c", g=4)
                        S.op("vector", lambda e, z3=z3: e.tensor_reduce(zst[:, 0:4], z3, axis=AX.X, op=ALU.add), reads=[zk], writes=["zst"])
                        S.op("gpsimd", lambda e, zb=zb: e.tensor_tensor(out=zsq[:], in0=zg[zb][:], in1=zg[zb][:], op=ALU.mult), reads=[zk], writes=["zsq"])
                        S.op("vector", lambda e: e.tensor_reduce(zst[:, 4:8], zsq[:].rearrange("p (g c) -> p g c", g=4), axis=AX.X, op=ALU.add),
                             reads=["zsq"], writes=["zst"])
                        S.op("vector", lambda e: e.tensor_scalar(zst[:, 0:4], zst[:, 0:4], 1.0 / 64, None, ALU.mult), reads=["zst"], writes=["zst"])
                        S.op("vector", lambda e: e.tensor_tensor(out=zst[:, 8:12], in0=zst[:, 0:4], in1=zst[:, 0:4], op=ALU.mult), reads=["zst"], writes=["zst"])
                        S.op("vector", lambda e: e.scalar_tensor_tensor(out=zst[:, 4:8], in0=zst[:, 4:8], scalar=1.0 / 64, in1=zst[:, 8:12],
                                                                        op0=ALU.mult, op1=ALU.subtract), reads=["zst"], writes=["zst"])
                        S.op("scalar", lambda e: e.activation(out=zst[:, 4:8], in_=zst[:, 4:8], func=AF.Sqrt, bias=epsc[:], scale=1.0),
                             reads=["zst", "epsc"], writes=["zst"])
                        S.op("vector", lambda e: e.reciprocal(zst[:, 4:8], zst[:, 4:8]), reads=["zst"], writes=["zst"])
                        S.op("vector", lambda e, z3=z3: e.tensor_tensor(out=z3, in0=z3, in1=zst[:, 0:4].unsqueeze(2).to_broadcast([128, 4, 64]), op=ALU.subtract),
                             reads=[zk, "zst"], writes=[zk])
                        S.op("vector", lambda e, z3=z3, sub=sub: e.tensor_tensor(out=zn[sub][:], in0=z3, in1=zst[:, 4:8].unsqueeze(2).to_broadcast([128, 4, 64]), op=ALU.mult),
                             reads=[zk, "zst"], writes=["zn%d" % sub])
                    for g in range(4):
                        p = next_ps()
                        pk = "psA%d" % p
                        for dc in range(8):
                            S.op("tensor", lambda e, p=p, dc=dc, g=g, xti=xti: e.matmul(
                                psA[p][0:64, :], win[:, dc, 1536 + 64 * g:1536 + 64 * (g + 1)], xti[:, dc, :], start=(dc == 0), stop=(dc == 7)),
                                reads=["win", xk], writes=[pk])
                        gelu(psA[p][0:64, :], pk, ug[g][:], "ug%d" % g, 64, 512, gcount[0] % 2)
                        gcount[0] += 1
                    for g in range(4):
                        p = next_ps()
                        pk = "psA%d" % p
                        for sub in range(4):
                            S.op("tensor", lambda e, p=p, g=g, sub=sub: e.matmul(
                                psA[p][0:64, sub * 128:(sub + 1) * 128], zn[sub][:, g, :], wsT[:, g, :], start=True, stop=True),
                                reads=["zn%d" % sub, "wsT"], writes=[pk])
                        sb2 = g % 2
                        S.op("vector", lambda e, p=p, g=g, sb2=sb2: e.scalar_tensor_tensor(
                            out=spv[sb2][:].rearrange("p (s t) -> p s t", s=4), in0=psA[p][0:64, :].rearrange("p (s t) -> p s t", s=4),
                            scalar=sgain[:, g:g + 1], in1=bsb[:, g:g + 1, :].to_broadcast([64, 4, 128]), op0=ALU.mult, op1=ALU.add),
                            reads=[pk, "sgain", "bsb"], writes=["spv%d" % sb2])
                        S.op("gpsimd", lambda e, g=g, sb2=sb2: e.tensor_tensor(out=yb[sb2][:], in0=spv[sb2][:], in1=ug[g][:], op=ALU.mult),
                             reads=["spv%d" % sb2, "ug%d" % g], writes=["yb%d" % sb2])
                        S.op("sync", lambda e, g=g, sb2=sb2, i=i: e.dma_start(
                            out=mixT[4 + g // 2, (g % 2) * 64:(g % 2) * 64 + 64, i * 512:(i + 1) * 512], in_=yb[sb2][:]),
                            reads=["yb%d" % sb2], writes=["mix_dram"], dma="yb%d" % sb2)
                S.barrier()
                S.emit()

            with ExitStack() as ph:
                qn = SB(ph, "b_qn", [128, S_LEN], BF16)
                kn = SB(ph, "b_kn", [128, S_LEN], BF16)
                qd = SB(ph, "b_qd", [128, S_LEN], BF16)
                kd = SB(ph, "b_kd", [128, S_LEN], BF16)
                vas = [SB(ph, "b_va%d" % i, [128, NT, 2, 65], BF16) for i in range(2)]
                acc = SB(ph, "b_acc", [65, 2, S_LEN], F32)
                pt = [SB(ph, "b_pt%d" % i, [128, 2, 256], BF16) for i in range(4)]
                m01 = SB(ph, "b_m01", [128, 2, 256], BF16)
                rec = [SB(ph, "b_rec%d" % i, [64, 512], F32) for i in range(2)]
                yaT = [SB(ph, "b_yaT%d" % i, [64, 2048], BF16) for i in range(2)]
                sel_f = cst[0:65, C_SEL:C_SEL + 64]
                ps_s = [PS(ph, "b_pss%d" % i, [128, 2, 256]) for i in range(3)]
                ps_o = [PS(ph, "b_pso%d" % i, [65, 512]) for i in range(3)]
                ps_d = [PS(ph, "b_psd%d" % i, [64, 512]) for i in range(2)]
                for s_ in range(2):
                    S.op("vector", lambda e, s_=s_: e.tensor_scalar(m01[:, s_, :], cst[:, C_MASK:C_MASK + 256], 0.0, None, ALU.is_equal),
                         reads=["cst"], writes=["m01"])
                S.op("vector", lambda e: e.memset(vas[0][:], 1.0), writes=["va0"])
                S.op("gpsimd", lambda e: e.memset(vas[1][:], 1.0), writes=["va1"])
                pcount = [0]

                def load_v(job):
                    c_, bi_ = divmod(job, 3)
                    d_ = BRANCH_D[bi_]
                    nb_ = NT // d_
                    vi = job % 2
                    vsrc = Vd.rearrange("(n j r) f -> j r n f", j=128, r=d_)
                    for r in range(d_):
                        for h2 in range(2):
                            S.op("sync", lambda e, r=r, h2=h2: e.dma_start(
                                out=vas[vi][:, r * nb_:(r + 1) * nb_, h2, 0:64],
                                in_=vsrc[:, r, :, c_ * 128 + h2 * 64:c_ * 128 + (h2 + 1) * 64]),
                                reads=["V_dram"], writes=["va%d" % vi], dma="va%d" % vi)

                load_v(0)

                def load_qk(c_):
                    S.op("sync", lambda e: e.dma_start(out=qn[:], in_=QT[c_]), reads=["QK_dram"], writes=["qn"], dma="qn")
                    S.op("sync", lambda e: e.dma_start(out=kn[:], in_=KT[c_]), reads=["QK_dram"], writes=["kn"], dma="kn")

                def deint(d_):
                    S.op("vector", lambda e: e.tensor_copy(qd[:].rearrange("p (r m) -> p r m", r=d_),
                                                           qn[:].rearrange("p (m r) -> p r m", r=d_)), reads=["qn"], writes=["qd"])
                    S.op("gpsimd", lambda e: e.tensor_copy(kd[:].rearrange("p (r m) -> p r m", r=d_),
                                                           kn[:].rearrange("p (m r) -> p r m", r=d_)), reads=["kn"], writes=["kd"])

                load_qk(0)
                for c in range(4):
                    for bi, d in enumerate(BRANCH_D):
                        nb = NT // d
                        if bi == 0:
                            deint(4)
                            qs, ks, qsk, ksk = qn, kn, "qn", "kn"
                        elif bi == 1:
                            qs, ks, qsk, ksk = qd, kd, "qd", "kd"
                        else:
                            qs, ks, qsk, ksk = qd, kd, "qd", "kd"
                            deint(16)
                            if c + 1 < 4:
                                load_qk(c + 1)
                        job = c * 3 + bi
                        if job + 1 < 12:
                            load_v(job + 1)
                        va = vas[job % 2]
                        vak = "va%d" % (job % 2)
                        for hh in range(2):
                            P0 = hh * 64
                            started = {}

                            def emit_scores(kp, P0=P0, qs=qs, ks=ks, qsk=qsk, ksk=ksk, nb=nb):
                                sb_i = kp % 3
                                sk = "pss%d" % sb_i
                                for T in (2 * kp, 2 * kp + 1):
                                    n = T % nb
                                    N = 256 if n < nb - 1 else 128
                                    slot = T % 2
                                    S.op("tensor", lambda e, sb_i=sb_i, slot=slot, N=N, T=T: e.matmul(
                                        ps_s[sb_i][:, slot, 0:N], ks[P0:P0 + 64, T * 128:(T + 1) * 128], qs[P0:P0 + 64, T * 128:T * 128 + N],
                                        start=True, stop=True, skip_group_check=True), reads=[ksk, qsk], writes=[sk])
                                pi = pcount[0] % 4
                                pcount[0] += 1
                                S.op("scalar", lambda e, sb_i=sb_i, pi=pi: e.activation(out=pt[pi][:], in_=ps_s[sb_i][:], func=AF.Exp,
                                                                                        bias=zeroc[:], scale=1.0),
                                     reads=[sk, "zeroc"], writes=["pt%d" % pi])
                                meng = "vector" if (kp % 3 == 0) else "gpsimd"
                                S.op(meng, lambda e, pi=pi: e.tensor_tensor(out=pt[pi][:], in0=pt[pi][:], in1=m01[:], op=ALU.mult),
                                     reads=["pt%d" % pi, "m01"], writes=["pt%d" % pi])
                                return pi

                            def emit_pv(kp, pi, hh=hh, nb=nb, d=d, bi=bi, started=started, va=va, vak=vak):
                                ptk = "pt%d" % pi
                                for T2 in (2 * kp, 2 * kp + 1):
                                    n2 = T2 % nb
                                    N2 = 256 if n2 < nb - 1 else 128
                                    sl2 = T2 % 2
                                    if N2 == 256 and (T2 % 4) != 3:
                                        pieces = [(T2, 0, 256)]
                                    else:
                                        pieces = [(T2, 0, 128)]
                                        if N2 == 256:
                                            pieces.append((T2 + 1, 128, 128))
                                    for (qb, off, wdt) in pieces:
                                        B = qb // 4
                                        ob = B % 3
                                        first = B not in started
                                        started[B] = True
                                        S.op("tensor", lambda e, ob=ob, qb=qb, off=off, wdt=wdt, T2=T2, sl2=sl2, first=first: e.matmul(
                                            ps_o[ob][:, (qb % 4) * 128:(qb % 4) * 128 + wdt], va[:, T2, hh, :], pt[pi][:, sl2, off:off + wdt],
                                            start=first, stop=False, skip_group_check=True),
                                            reads=[vak, ptk], writes=["pso%d" % ob])
                                    if T2 % 4 == 3:
                                        B = T2 // 4
                                        ob = B % 3
                                        pos0 = B * 512
                                        r = pos0 // (S_LEN // d)
                                        m0 = pos0 % (S_LEN // d)
                                        dstv = acc[:, hh, :].rearrange("p (m r) -> p r m", r=d)[:, r, m0:m0 + 512]
                                        if bi == 0:
                                            S.op("vector", lambda e, ob=ob, dstv=dstv: e.tensor_copy(dstv, ps_o[ob][:]),
                                                 reads=["pso%d" % ob], writes=["acc%d" % hh])
                                        else:
                                            S.op("vector", lambda e, ob=ob, dstv=dstv: e.tensor_tensor(out=dstv, in0=ps_o[ob][:], in1=dstv, op=ALU.add),
                                                 reads=["pso%d" % ob, "acc%d" % hh], writes=["acc%d" % hh])

                            hist = []
                            for kp in range(NT // 2):
                                pi = emit_scores(kp)
                                hist.append((kp, pi))
                                if len(hist) > 2:
                                    emit_pv(*hist[-3])
                            emit_pv(*hist[-2])
                            emit_pv(*hist[-1])
                    for hh in range(2):
                        for B in range(NG):
                            di = B % 2
                            S.op("tensor", lambda e, di=di, hh=hh, B=B: e.matmul(ps_d[di][:], sel_f, acc[:, hh, B * 512:(B + 1) * 512], start=True, stop=True),
                                 reads=["cst", "acc%d" % hh], writes=["psd%d" % di])
                            S.op("vector", lambda e, di=di: e.reciprocal(rec[di][:], ps_d[di][:]), reads=["psd%d" % di], writes=["rec%d" % di])
                            yi_ = (B // 4) % 2
                            S.op("gpsimd", lambda e, di=di, hh=hh, B=B, yi_=yi_: e.tensor_tensor(out=yaT[yi_][:, (B % 4) * 512:(B % 4 + 1) * 512], in0=acc[0:64, hh, B * 512:(B + 1) * 512],
                                                                                                  in1=rec[di][:], op=ALU.mult),
                                 reads=["rec%d" % di, "acc%d" % hh], writes=["yaT%d" % yi_])
                            if B % 4 == 3:
                                S.op("sync", lambda e, c=c, hh=hh, B=B, yi_=yi_: e.dma_start(
                                    out=mixT[c, hh * 64:(hh + 1) * 64, (B // 4) * 2048:(B // 4 + 1) * 2048], in_=yaT[yi_][:]),
                                    reads=["yaT%d" % yi_], writes=["mix_dram"], dma="yaT%d" % yi_)
                S.barrier()
                S.emit()

            with ExitStack() as ph:
                wo_f = SB(ph, "c_wof", [128, DM], F32)
                wo = SB(ph, "c_wo", [128, 8, DM], BF16)
                bg = SB(ph, "c_bg", [128, 8], F32)
                lng = SB(ph, "c_lng", [128, DM], F32)
                lnb = SB(ph, "c_lnb", [128, DM], F32)
                wr_f = SB(ph, "c_wrf", [128, 8, 36], F32)
                wr = SB(ph, "c_wr", [128, 8, 36], BF16)
                rb = SB(ph, "c_rb", [128, 36], F32)
                mx = [SB(ph, "c_mx%d" % i, [128, 8, 512], BF16) for i in range(2)]
                sq = SB(ph, "c_sq", [128, 8, 512], BF16)
                xa = [SB(ph, "c_xa%d" % i, [128, DM], F32) for i in range(4)]
                accs = [SB(ph, "c_acc%d" % i, [128, DM], F32) for i in range(2)]
                junk = SB(ph, "c_junk", [128, DM], F32)
                st = [SB(ph, "c_st%d" % i, [128, 16], F32) for i in range(2)]
                rs3 = [SB(ph, "c_rs3%d" % i, [128, 3], F32) for i in range(2)]
                bst = [SB(ph, "c_bst%d" % i, [128, 2, 6], F32) for i in range(2)]
                x1 = [SB(ph, "c_x1%d" % i, [128, DM], F32) for i in range(2)]
                x1b = [SB(ph, "c_x1b%d" % i, [128, DM], BF16) for i in range(2)]
                x1T = SB(ph, "c_x1T", [128, 8, 128], BF16)
                lg = [SB(ph, "c_lg%d" % i, [128, 36], F32) for i in range(2)]
                rt = SB(ph, "c_rt", [128, 48], F32)
                elm = SB(ph, "c_elm", [128, NE], F32)
                top8 = SB(ph, "c_top8", [128, 8], F32)
                Abf = SB(ph, "c_Abf", [128, NE], BF16)
                rk = SB(ph, "c_rk", [128, NE], F32)
                tmp32 = SB(ph, "c_tmp32", [128, NE], F32)
                base = SB(ph, "c_base", [128, NE], F32)
                ps_z = [PS(ph, "c_psz%d" % i, [128, DM]) for i in range(2)]
                ps_ss = PS(ph, "c_psss", [128, 512])
                ps_tr = PS(ph, "c_pstr", [128, 8, 128], BF16)
                ps_lg = PS(ph, "c_pslg", [128, 512])
                ps_cnt = PS(ph, "c_pscnt", [128, 512])

                S.op("sync", lambda e: e.dma_start(out=bg[:], in_=branch_gain[l].rearrange("(c p) -> p c", p=128), allow_slow_non_contiguous=True),
                     writes=["bg"], dma="bg")
                for ch in range(8):
                    S.op("sync", lambda e, ch=ch: e.dma_start(out=wo_f[:], in_=w_out[l, ch * 128:(ch + 1) * 128, :]), writes=["wo_f"], dma="wo_f")
                    S.op("vector", lambda e, ch=ch: e.tensor_scalar(wo[:, ch, :], wo_f[:], bg[:, ch:ch + 1], None, ALU.mult),
                         reads=["wo_f", "bg"], writes=["wo"])
                S.op("sync", lambda e: e.dma_start(out=lng[:], in_=ln_gain[l, 0:1, :].partition_broadcast(128)), writes=["lng"], dma="lng")
                S.op("sync", lambda e: e.dma_start(out=lnb[:], in_=ln_bias[l, 0:1, :].partition_broadcast(128)), writes=["lnb"], dma="lnb")
                S.op("sync", lambda e: e.dma_start(out=wr_f[:, :, 0:4], in_=rgw[l].rearrange("(c p) g -> p c g", p=128), allow_slow_non_contiguous=True),
                     writes=["wr_f"], dma="wr_f")
                S.op("sync", lambda e: e.dma_start(out=wr_f[:, :, 4:36], in_=rew[l].rearrange("(c p) g -> p c g", p=128), allow_slow_non_contiguous=True),
                     writes=["wr_f"], dma="wr_f")
                S.op("vector", lambda e: e.tensor_copy(wr[:], wr_f[:]), reads=["wr_f"], writes=["wr"])
                S.op("sync", lambda e: e.dma_start(out=rb[:, 0:4], in_=rgb[l:l + 1, :].partition_broadcast(128)), writes=["rb"], dma="rb")
                S.op("sync", lambda e: e.dma_start(out=rb[:, 4:36], in_=reb[l:l + 1, :].partition_broadcast(128)), writes=["rb"], dma="rb")
                S.op("vector", lambda e: e.memset(base[:], 0.0), writes=["base"])

                parts = ((0, 4), (4, 6), (6, 8))

                def c_s0(tt):
                    g, sub = divmod(tt, 4)
                    if sub == 0:
                        mk = "mx%d" % (g % 2)
                        S.op("sync", lambda e, g=g: e.dma_start(out=mx[g % 2][:], in_=mixT.rearrange("c p t -> p c t")[:, :, g * 512:(g + 1) * 512]),
                             reads=["mix_dram"], writes=[mk], dma=mk)
                    b4 = tt % 4
                    S.op("sync", lambda e, tt=tt, b4=b4: e.dma_start(out=xa[b4][:], in_=Xin[tt * 128:(tt + 1) * 128, :]),
                         writes=["xa%d" % b4], dma="xa%d" % b4)

                def c_s0b(tt):
                    g, sub = divmod(tt, 4)
                    mk = "mx%d" % (g % 2)
                    mxg = mx[g % 2]
                    b2 = tt % 2
                    tok = slice(sub * 128, (sub + 1) * 128)
                    rk_ = "rs3_%d" % b2
                    if sub == 0:
                        S.op("scalar", lambda e: e.activation(out=sq[:], in_=mxg[:], func=AF.Square, bias=zeroc[:], scale=1.0),
                             reads=[mk, "zeroc"], writes=["sq"])
                    for pi_, (c0, c1) in enumerate(parts):
                        for ch in range(c0, c1):
                            S.op("tensor", lambda e, pi_=pi_, ch=ch, c0=c0, c1=c1: e.matmul(
                                ps_ss[:, pi_:pi_ + 1], sq[:, ch, tok], ones_bf[:, 0:1], start=(ch == c0), stop=(ch == c1 - 1), skip_group_check=True),
                                reads=["sq", "ones_bf"], writes=["ps_ss"])
                    S.op("vector", lambda e: e.tensor_tensor(out=rs3[b2][:], in0=ps_ss[:, 0:3], in1=cst[:, C_INVW:C_INVW + 3], op=ALU.mult),
                         reads=["ps_ss", "cst"], writes=[rk_])
                    S.op("scalar", lambda e: e.activation(out=rs3[b2][:], in_=rs3[b2][:], func=AF.Sqrt, bias=epsA2[:], scale=ALPHA * ALPHA),
                         reads=[rk_, "epsA2"], writes=[rk_])
                    S.op("vector", lambda e: e.reciprocal(rs3[b2][:], rs3[b2][:]), reads=[rk_], writes=[rk_])

                def c_s1(tt):
                    g, sub = divmod(tt, 4)
                    mk = "mx%d" % (g % 2)
                    mxg = mx[g % 2]
                    b2 = tt % 2
                    b4 = tt % 4
                    stt = st[b2]
                    sk_ = "st_%d" % b2
                    rk_ = "rs3_%d" % b2
                    tok = slice(sub * 128, (sub + 1) * 128)
                    ak = "accs%d" % b2
                    for pi_, (c0, c1) in enumerate(parts):
                        zi = (tt * 3 + pi_) % 2
                        zk = "psz%d" % zi
                        for half in range(2):
                            for ch in range(c0, c1):
                                S.op("tensor", lambda e, zi=zi, half=half, ch=ch, c0=c0, c1=c1: e.matmul(
                                    ps_z[zi][:, half * 512:(half + 1) * 512], mxg[:, ch, tok], wo[:, ch, half * 512:(half + 1) * 512],
                                    start=(ch == c0), stop=(ch == c1 - 1)), reads=[mk, "wo"], writes=[zk])
                        if pi_ == 0:
                            S.op("vector", lambda e, zi=zi, pi_=pi_: e.scalar_tensor_tensor(
                                out=accs[b2][:], in0=ps_z[zi][:], scalar=rs3[b2][:, pi_:pi_ + 1], in1=xa[b4][:], op0=ALU.mult, op1=ALU.add),
                                reads=[zk, rk_, "xa%d" % b4], writes=[ak])
                        else:
                            S.op("vector", lambda e, zi=zi, pi_=pi_: e.scalar_tensor_tensor(
                                out=accs[b2][:], in0=ps_z[zi][:], scalar=rs3[b2][:, pi_:pi_ + 1], in1=accs[b2][:], op0=ALU.mult, op1=ALU.add),
                                reads=[zk, rk_, ak], writes=[ak])
                    S.op("vector", lambda e: e.bn_stats(bst[b2][:, 0, :], accs[b2][:, 0:512]), reads=[ak], writes=["bst%d" % b2])
                    S.op("vector", lambda e: e.bn_stats(bst[b2][:, 1, :], accs[b2][:, 512:1024]), reads=[ak], writes=["bst%d" % b2])
                    S.op("vector", lambda e: e.bn_aggr(stt[:, 0:2], bst[b2][:]), reads=["bst%d" % b2], writes=[sk_])
                    S.op("scalar", lambda e: e.activation(out=stt[:, 2:3], in_=stt[:, 1:2], func=AF.Sqrt, bias=epsLN[:], scale=1.0), reads=[sk_, "epsLN"], writes=[sk_])
                    S.op("vector", lambda e: e.reciprocal(stt[:, 2:3], stt[:, 2:3]), reads=[sk_], writes=[sk_])

                def c_s2(tt):
                    b2 = tt % 2
                    stt = st[b2]
                    sk_ = "st_%d" % b2
                    ak = "accs%d" % b2
                    x1k = "x1%d" % b2
                    S.op("vector", lambda e: e.tensor_scalar(x1[b2][:], accs[b2][:], stt[:, 0:1], stt[:, 2:3], ALU.subtract, ALU.mult),
                         reads=[ak, sk_], writes=[x1k])
                    S.op("vector", lambda e: e.tensor_tensor(out=x1[b2][:], in0=x1[b2][:], in1=lng[:], op=ALU.mult), reads=[x1k, "lng"], writes=[x1k])
                    S.op("gpsimd", lambda e: e.tensor_tensor(out=x1[b2][:], in0=x1[b2][:], in1=lnb[:], op=ALU.add), reads=[x1k, "lnb"], writes=[x1k])
                    S.op("sync", lambda e: e.dma_start(out=X1[tt * 128:(tt + 1) * 128, :], in_=x1[b2][:]), reads=[x1k], writes=["X1_dram"], dma="x1o%d" % b2)
                    xbk = "x1b%d" % b2
                    S.op("gpsimd", lambda e: e.tensor_copy(x1b[b2][:], x1[b2][:]), reads=[x1k], writes=[xbk])
                    S.op("sync", lambda e: e.dma_start(out=X1b[tt * 128:(tt + 1) * 128, :], in_=x1b[b2][:]), reads=[xbk], writes=["X1b_dram"], dma="x1bo%d" % b2)

                def c_s2b(tt):
                    b2 = tt % 2
                    xbk = "x1b%d" % b2
                    for j in range(8):
                        S.op("tensor", lambda e, j=j: e.transpose(ps_tr[:, j, :], x1b[b2][:, j * 128:(j + 1) * 128], ident_bf[:]),
                             reads=[xbk, "ident_bf"], writes=["ps_tr"])
                    S.op("scalar", lambda e: e.copy(x1T[:], ps_tr[:]), reads=["ps_tr"], writes=["x1T"])
                    for j in range(8):
                        S.op("tensor", lambda e, j=j: e.matmul(ps_lg[:, 0:36], x1T[:, j, :], wr[:, j, :], start=(j == 0), stop=(j == 7), skip_group_check=True),
                             reads=["x1T", "wr"], writes=["ps_lg"])
                    S.op("vector", lambda e: e.tensor_tensor(out=lg[b2][:], in0=ps_lg[:, 0:36], in1=rb[:], op=ALU.add), reads=["ps_lg", "rb"], writes=["lg%d" % b2])

                def c_s3(tt):
                    b2 = tt % 2
                    lgt = lg[b2]
                    lk = "lg%d" % b2
                    S.op("vector", lambda e: e.reduce_max(rt[:, 0:1], lgt[:, 0:4], axis=AX.X), reads=[lk], writes=["rt"])
                    S.op("vector", lambda e: e.tensor_scalar(rt[:, 1:2], rt[:, 0:1], -1.0, None, ALU.mult), reads=["rt"], writes=["rt"])
                    S.op("vector", lambda e: e.memset(rt[:, 2:3], 0.0), reads=["rt"], writes=["rt"])
                    S.op("scalar", lambda e: e.activation(out=rt[:, 4:8], in_=lgt[:, 0:4], func=AF.Exp, bias=rt[:, 1:2], scale=1.0, accum_out=rt[:, 2:3]),
                         reads=[lk, "rt"], writes=["rt"])
                    S.op("vector", lambda e: e.reciprocal(rt[:, 3:4], rt[:, 2:3]), reads=["rt"], writes=["rt"])
                    S.op("vector", lambda e: e.tensor_scalar(rt[:, 8:12], lgt[:, 0:4], rt[:, 0:1], None, ALU.is_equal), reads=[lk, "rt"], writes=["rt"])
                    S.op("vector", lambda e: e.tensor_scalar(rt[:, 12:16], rt[:, 8:12], 1e30, -1e30, ALU.mult, ALU.add), reads=["rt"], writes=["rt"])
                    S.op("vector", lambda e: e.tensor_tensor(out=elm[:].rearrange("p (g j) -> p g j", g=4), in0=lgt[:, 4:36].rearrange("p (g j) -> p g j", g=4),
                                                             in1=rt[:, 12:16].unsqueeze(2).to_broadcast([128, 4, 8]), op=ALU.add),
                         reads=[lk, "rt"], writes=["elm"])
                    S.op("vector", lambda e: e.max(out=top8[:], in_=elm[:]), reads=["elm"], writes=["top8"])
                    S.op("vector", lambda e: e.tensor_scalar(M1all[:, tt, :], elm[:], top8[:, 0:1], None, ALU.is_equal), reads=["elm", "top8"], writes=["M1"])
                    S.op("vector", lambda e: e.tensor_scalar(M2all[:, tt, :], elm[:], top8[:, 1:2], None, ALU.is_equal), reads=["elm", "top8"], writes=["M2"])
                    S.op("vector", lambda e: e.tensor_tensor(out=rt[:, 16:17], in0=top8[:, 0:1], in1=top8[:, 1:2], op=ALU.subtract), reads=["top8"], writes=["rt"])
                    S.op("vector", lambda e: e.tensor_tensor(out=rt[:, 17:18], in0=top8[:, 1:2], in1=top8[:, 0:1], op=ALU.subtract), reads=["top8"], writes=["rt"])
                    S.op("scalar", lambda e: e.activation(out=rt[:, 18:20], in_=rt[:, 16:18], func=AF.Sigmoid, bias=zeroc[:], scale=1.0), reads=["rt", "zeroc"], writes=["rt"])
                    S.op("vector", lambda e: e.tensor_scalar(W01[:, tt, :], rt[:, 18:20], rt[:, 3:4], 1.0 / ALPHA, ALU.mult, ALU.mult), reads=["rt"], writes=["W01"])
                    S.op("vector", lambda e: e.tensor_tensor(out=Abf[:], in0=M1all[:, tt, :], in1=M2all[:, tt, :], op=ALU.add), reads=["M1", "M2"], writes=["Abf"])
                    S.op("tensor", lambda e: e.matmul(ps_lg[:, 64:96], ls_bf[:], Abf[:], start=True, stop=True, skip_group_check=True),
                         reads=["ls_bf", "Abf"], writes=["ps_lg"])
                    S.op("tensor", lambda e: e.matmul(ps_lg[:, 96:128], ones_bf[:], Abf[:], start=False, stop=True, skip_group_check=True),
                         reads=["ones_bf", "Abf"], writes=["ps_lg"])
                    S.op("tensor", lambda e: e.matmul(ps_cnt[0:32, 0:1], Abf[:], ones_bf[:, 0:1], start=(tt == 0), stop=(tt == NT - 1), skip_group_check=True),
                         reads=["Abf", "ones_bf"], writes=["ps_cnt"])
                    S.op("vector", lambda e: e.tensor_tensor(out=rk[:], in0=ps_lg[:, 64:96], in1=base[:], op=ALU.add), reads=["ps_lg", "base"], writes=["rk"])
                    S.op("vector", lambda e: e.tensor_tensor(out=base[:], in0=ps_lg[:, 96:128], in1=base[:], op=ALU.add), reads=["ps_lg", "base", "rk"], writes=["base"])
                    S.op("vector", lambda e: e.tensor_tensor(out=tmp32[:], in0=rk[:], in1=M1all[:, tt, :], op=ALU.mult), reads=["rk", "M1"], writes=["tmp32"])
                    S.op("vector", lambda e: e.reduce_sum(RK[:, tt, 0:1], tmp32[:], axis=AX.X), reads=["tmp32"], writes=["RK"])
                    S.op("vector", lambda e: e.tensor_tensor(out=tmp32[:], in0=rk[:], in1=M2all[:, tt, :], op=ALU.mult), reads=["rk", "M2"], writes=["tmp32"])
                    S.op("vector", lambda e: e.reduce_sum(RK[:, tt, 1:2], tmp32[:], axis=AX.X), reads=["tmp32"], writes=["RK"])

                c_s0(0)
                c_s0(1)
                c_s0b(0)
                for step in range(NT + 3):
                    if step + 2 < NT:
                        c_s0(step + 2)
                    if step + 1 < NT:
                        c_s0b(step + 1)
                    if step < NT:
                        c_s1(step)
                    if 0 <= step - 1 < NT:
                        c_s2(step - 1)
                    if 0 <= step - 2 < NT:
                        c_s2b(step - 2)
                    if 0 <= step - 3 < NT:
                        c_s3(step - 3)

                cntT = SB(ph, "c_cntT", [32, 1], F32)
                cmp_ = SB(ph, "c_cmp", [32, 33], F32)
                nblkT = SB(ph, "c_nblkT", [32, 1], F32)
                nbl_b = SB(ph, "c_nblb", [32, 128], F32)
                pe_ps = SB(ph, "c_peps", [128, 64], F32)
                cmp2 = SB(ph, "c_cmp2", [128, NBLK, NE], F32)
                ebf = SB(ph, "c_ebf", [128, NBLK], F32)
                ebf2 = SB(ph, "c_ebf2", [128, NBLK, 2], F32)
                trail = SB(ph, "c_trail", [128, NBLK], F32)
                pstart = SB(ph, "c_pstart", [128, NE], F32)
                big = SB(ph, "c_big", [128, NT, NE], F32)
                dstf = SB(ph, "c_dstf", [128, NT, 2], F32)
                S.op("vector", lambda e: e.tensor_copy(cntT[:], ps_cnt[0:32, 0:1]), reads=["ps_cnt"], writes=["cntT"])
                S.op("vector", lambda e: e.tensor_scalar(cmp_[:], cst[0:32, C_THR:C_THR + 33], cntT[:, 0:1], None, ALU.is_lt), reads=["cst", "cntT"], writes=["cmp_"])
                S.op("vector", lambda e: e.reduce_sum(nblkT[:], cmp_[:], axis=AX.X), reads=["cmp_"], writes=["nblkT"])
                S.op("vector", lambda e: e.tensor_copy(nbl_b[:], nblkT[:, 0:1].to_broadcast([32, 128])), reads=["nblkT"], writes=["nbl_b"])
                S.op("tensor", lambda e: e.matmul(ps_ss[:, 0:64], nbl_b[:], cst[0:32, C_UCAT:C_UCAT + 64], start=True, stop=True, skip_group_check=True),
                     reads=["nbl_b", "cst"], writes=["ps_ss"])
                S.op("vector", lambda e: e.tensor_copy(pe_ps[:], ps_ss[:, 0:64]), reads=["ps_ss"], writes=["pe_ps"])
                S.op("vector", lambda e: e.tensor_tensor(out=pstart[:], in0=pe_ps[:, 0:32], in1=pe_ps[:, 32:64], op=ALU.subtract), reads=["pe_ps"], writes=["pstart"])
                S.op("vector", lambda e: e.tensor_tensor(out=cmp2[:], in0=pe_ps[:, 0:32].unsqueeze(1).to_broadcast([128, NBLK, NE]),
                                                         in1=cst[:, C_BV:C_BV + NBLK].unsqueeze(2).to_broadcast([128, NBLK, NE]), op=ALU.is_le),
                     reads=["pe_ps", "cst"], writes=["cmp2"])
                S.op("vector", lambda e: e.tensor_reduce(ebf[:], cmp2[:], axis=AX.X, op=ALU.add), reads=["cmp2"], writes=["ebf"])
                S.op("vector", lambda e: e.tensor_scalar(trail[:], ebf[:], 31.5, 4.0e6, ALU.is_ge, ALU.mult), reads=["ebf"], writes=["trail"])
                S.op("vector", lambda e: e.tensor_scalar(ebf[:], ebf[:], 31.0, 128.0, ALU.min, ALU.mult), reads=["ebf"], writes=["ebf"])
                S.op("vector", lambda e: e.tensor_scalar(ebf[:], ebf[:], cst[:, C_IP:C_IP + 1], float(l * NE * 128), ALU.add, ALU.add), reads=["ebf", "cst"], writes=["ebf"])
                S.op("vector", lambda e: e.tensor_copy(GIDX[:], ebf[:]), reads=["ebf"], writes=["GIDX"])
                for h in range(2):
                    S.op("vector", lambda e, h=h: e.tensor_scalar(ebf2[:, :, h], ebf[:], 2.0, float(h), ALU.mult, ALU.add), reads=["ebf"], writes=["ebf2"])
                    S.op("vector", lambda e, h=h: e.tensor_tensor(out=ebf2[:, :, h], in0=ebf2[:, :, h], in1=trail[:], op=ALU.add), reads=["ebf2", "trail"], writes=["ebf2"])
                S.op("vector", lambda e: e.tensor_copy(GIDX2[:], ebf2[:]), reads=["ebf2"], writes=["GIDX2"])
                for k, Mk in enumerate((M1all, M2all)):
                    S.op("vector", lambda e, Mk=Mk: e.tensor_tensor(out=big[:], in0=Mk[:], in1=pstart[:].unsqueeze(1).to_broadcast([128, NT, NE]), op=ALU.mult),
                         reads=["M1", "M2", "pstart"], writes=["big"])
                    S.op("vector", lambda e, k=k: e.tensor_reduce(dstf[:, :, k], big[:], axis=AX.X, op=ALU.add), reads=["big"], writes=["dstf"])
                S.op("vector", lambda e: e.scalar_tensor_tensor(out=dstf[:], in0=dstf[:], scalar=float(BLK), in1=RK[:], op0=ALU.mult, op1=ALU.add),
                     reads=["dstf", "RK"], writes=["dstf"])
                S.op("vector", lambda e: e.tensor_copy(DSTi[:], dstf[:]), reads=["dstf"], writes=["DSTi"])
                S.barrier()
                S.emit()

            with ExitStack() as ph:
                xr = [SB(ph, "d_xr%d" % i, [128, DM], BF16) for i in range(8)]
                for tt in range(NT):
                    b4 = tt % 8
                    k = "xr%d" % b4
                    S.op("sync", lambda e, tt=tt, b4=b4: e.dma_start(out=xr[b4][:], in_=X1b[tt * 128:(tt + 1) * 128, :]), writes=[k], dma=k)
                    for kk in range(2):
                        S.op("gpsimd", lambda e, tt=tt, b4=b4, kk=kk: e.indirect_dma_start(
                            out=Xs, out_offset=bass.IndirectOffsetOnAxis(ap=DSTi[:, tt, kk:kk + 1], axis=0), in_=xr[b4][:], in_offset=None),
                            reads=[k], writes=["Xs_dram"], dma="sc%d" % b4)
                S.barrier()
                S.emit()

            with ExitStack() as ph:
                wg = [SB(ph, "e_wg%d" % i, [128, 8, DE], BF16) for i in range(2)]
                wu = [SB(ph, "e_wu%d" % i, [128, 8, DE], BF16) for i in range(2)]
                wd = [SB(ph, "e_wd%d" % i, [128, 4, DM], BF16) for i in range(2)]
                xs = [SB(ph, "e_xs%d" % i, [128, 4, DM], BF16) for i in range(2)]
                xsT = [SB(ph, "e_xsT%d" % i, [128, 8, 512], BF16) for i in range(2)]
                sg = [SB(ph, "e_sg%d" % i, [128, 512], F32) for i in range(2)]
                hT = [SB(ph, "e_hT%d" % i, [128, 4, 512], BF16) for i in range(2)]
                ysb = [SB(ph, "e_y%d" % i, [128, 4, DM], BF16) for i in range(2)]
                ps_t = [PS(ph, "e_pst%d" % i, [128, 8, 128], BF16) for i in range(2)]
                ps_g = [PS(ph, "e_psg%d" % i, [128, 512]) for i in range(2)]
                ps_u = [PS(ph, "e_psu%d" % i, [128, 512]) for i in range(2)]
                ps_y = [PS(ph, "e_psy%d" % i, [128, 512]) for i in range(2)]
                stg = [SB(ph, "e_stg%d" % i, [128, 2048], F32) for i in range(6)]
                wgv = ewg.rearrange("l e (p h j) f -> (l e p h) (j f)", h=2, j=4)
                wuv = ewu.rearrange("l e (p h j) f -> (l e p h) (j f)", h=2, j=4)
                wdv = ewd.rearrange("l e (p h j) n -> (l e p h) (j n)", h=2, j=2)
                piece = [0]
                yc_ = [0]
                def load_weights(b, which):
                    wb = b % 2
                    for (wt, wv, nm, jn) in which:
                        for h in range(2):
                            si = piece[0] % 6
                            ceng = "vector"
                            piece[0] += 1
                            S.op("gpsimd", lambda e, wv=wv, si=si, h=h: e.indirect_dma_start(
                                out=stg[si][:], out_offset=None, in_=wv,
                                in_offset=bass.IndirectOffsetOnAxis(ap=GIDX2[:, b, h:h + 1], axis=0),
                                bounds_check=S.breg, oob_is_err=False),
                                writes=["stg%d" % si], dma="stg%d" % si)
                            dst = wt[wb][:, h * jn:(h + 1) * jn, :].rearrange("p j f -> p (j f)")
                            if ceng == "vector":
                                S.op("vector", lambda e, dst=dst, si=si: e.tensor_copy(dst, stg[si][:]), reads=["stg%d" % si], writes=["%s%d" % (nm, wb)])
                            else:
                                S.op("scalar", lambda e, dst=dst, si=si: e.copy(dst, stg[si][:]), reads=["stg%d" % si], writes=["%s%d" % (nm, wb)])

                W_GU = ((wg, wgv, "wg", 4), (wu, wuv, "wu", 4))
                W_D = ((wd, wdv, "wd", 2),)

                def load_xs(b):
                    bb = b % 2
                    S.op("sync", lambda e: e.dma_start(out=xs[bb][:], in_=Xs[b * BLK:(b + 1) * BLK, :].rearrange("(s p) f -> p s f", p=128)),
                         reads=["Xs_dram"], writes=["xs%d" % bb], dma="xs%d" % bb)

                def e_front(b):
                    bb = b % 2
                    wb = bb
                    if b + 1 < NBLK:
                        load_xs(b + 1)
                    for sub in range(4):
                        ti = sub % 2
                        for j in range(8):
                            S.op("tensor", lambda e, ti=ti, j=j, sub=sub: e.transpose(
                                ps_t[ti][:, j, :], xs[bb][:, sub, :].rearrange("t (p j) -> t j p", j=8)[:, j, :], ident_bf[:]),
                                reads=["xs%d" % bb, "ident_bf"], writes=["e_pst%d" % ti])
                        S.op("vector", lambda e, ti=ti, sub=sub: e.tensor_copy(xsT[bb][:, :, sub * 128:(sub + 1) * 128], ps_t[ti][:]),
                             reads=["e_pst%d" % ti], writes=["xsT%d" % bb])
                    for jf in range(4):
                        gi = jf % 2
                        for (pst_, wt, nm) in ((ps_g, wg, "wg"), (ps_u, wu, "wu")):
                            for jd in range(8):
                                S.op("tensor", lambda e, pst_=pst_, wt=wt, gi=gi, jd=jd, jf=jf: e.matmul(
                                    pst_[gi][:], wt[wb][:, jd, :].rearrange("p (pf jf) -> p jf pf", jf=4)[:, jf, :], xsT[bb][:, jd, :],
                                    start=(jd == 0), stop=(jd == 7)), reads=["%s%d" % (nm, wb), "xsT%d" % bb], writes=["e_%s%d" % (nm, gi)])
                        S.op("scalar", lambda e, gi=gi: e.activation(out=sg[gi][:], in_=ps_g[gi][:], func=AF.Silu, bias=zeroc[:], scale=1.0),
                             reads=["e_wg%d" % gi, "zeroc"], writes=["sg%d" % gi])
                        S.op("vector", lambda e, gi=gi, jf=jf: e.tensor_tensor(out=hT[bb][:, jf, :], in0=ps_u[gi][:], in1=sg[gi][:], op=ALU.mult),
                             reads=["e_wu%d" % gi, "sg%d" % gi], writes=["hT%d" % bb])

                def e_back(b):
                    bb = b % 2
                    wb = bb
                    for sub in range(4):
                        for half in range(2):
                            yi = yc_[0] % 2
                            yc_[0] += 1
                            for jf in range(4):
                                S.op("tensor", lambda e, yi=yi, jf=jf, sub=sub, half=half: e.matmul(
                                    ps_y[yi][:], hT[bb][:, jf, sub * 128:(sub + 1) * 128], wd[wb][:, jf, half * 512:(half + 1) * 512],
                                    start=(jf == 0), stop=(jf == 3)), reads=["hT%d" % bb, "wd%d" % wb], writes=["e_psy%d" % yi])
                            S.op("scalar", lambda e, yi=yi, sub=sub, half=half: e.copy(ysb[bb][:, sub, half * 512:(half + 1) * 512], ps_y[yi][:]),
                                 reads=["e_psy%d" % yi], writes=["ysb%d" % bb])
                    S.op("sync", lambda e: e.dma_start(out=Ys[b * BLK:(b + 1) * BLK, :].rearrange("(s p) f -> p s f", p=128), in_=ysb[bb][:]),
                         reads=["ysb%d" % bb], writes=["Ys_dram"], dma="ysb%d" % bb)

                load_weights(0, W_GU)
                load_weights(0, W_D)
                load_xs(0)
                for b in range(NBLK + 1):
                    if b < NBLK:
                        e_front(b)
                        if b + 1 < NBLK:
                            load_weights(b + 1, W_GU)
                    if b >= 1:
                        e_back(b - 1)
                    if b + 1 < NBLK:
                        load_weights(b + 1, W_D)
                S.barrier()
                S.emit(want_breg=True)

            with ExitStack() as ph:
                lng = SB(ph, "f_lng", [128, DM], F32)
                lnb = SB(ph, "f_lnb", [128, DM], F32)
                y0 = [SB(ph, "f_y0%d" % i, [128, DM], BF16) for i in range(4)]
                y1 = [SB(ph, "f_y1%d" % i, [128, DM], BF16) for i in range(4)]
                xa = [SB(ph, "f_xa%d" % i, [128, DM], F32) for i in range(4)]
                accs = [SB(ph, "f_acc%d" % i, [128, DM], F32) for i in range(2)]
                junk = SB(ph, "f_junk", [128, DM], F32)
                st = [SB(ph, "f_st%d" % i, [128, 16], F32) for i in range(2)]
                bst = [SB(ph, "f_bst%d" % i, [128, 2, 6], F32) for i in range(2)]
                x2 = [SB(ph, "f_x2%d" % i, [128, DM], F32) for i in range(2)]
                xb = [SB(ph, "f_xb%d" % i, [128, DM], BF16) for i in range(4)]
                ps_t = [PS(ph, "f_pst%d" % i, [128, 8, 128], BF16) for i in range(2)]
                xTg = SB(ph, "f_xTg", [128, 8, 512], BF16)
                S.op("sync", lambda e: e.dma_start(out=lng[:], in_=ln_gain[l, 1:2, :].partition_broadcast(128)), writes=["lng"], dma="lng")
                S.op("sync", lambda e: e.dma_start(out=lnb[:], in_=ln_bias[l, 1:2, :].partition_broadcast(128)), writes=["lnb"], dma="lnb")
                last = (l == L - 1)

                def f_s0(tt):
                    b4 = tt % 4
                    S.op("sync", lambda e: e.dma_start(out=xa[b4][:], in_=X1[tt * 128:(tt + 1) * 128, :]),
                         reads=["X1_dram"], writes=["xa%d" % b4], dma="xa%d" % b4)
                    for kk, yt in enumerate((y0, y1)):
                        S.op("gpsimd", lambda e, kk=kk, yt=yt: e.indirect_dma_start(
                            out=yt[b4][:], out_offset=None, in_=Ys, in_offset=bass.IndirectOffsetOnAxis(ap=DSTi[:, tt, kk:kk + 1], axis=0)),
                            reads=["Ys_dram"], writes=["y%d_%d" % (kk, b4)], dma="y%d_%d" % (kk, b4))

                def f_s1(tt):
                    b2 = tt % 2
                    b4 = tt % 4
                    stt = st[b2]
                    sk_ = "st_%d" % b2
                    ak = "accs%d" % b2
                    S.op("vector", lambda e: e.scalar_tensor_tensor(out=accs[b2][:], in0=y0[b4][:], scalar=W01[:, tt, 0:1], in1=xa[b4][:],
                                                                    op0=ALU.mult, op1=ALU.add), reads=["y0_%d" % b4, "xa%d" % b4], writes=[ak])
                    S.op("vector", lambda e: e.scalar_tensor_tensor(out=accs[b2][:], in0=y1[b4][:], scalar=W01[:, tt, 1:2], in1=accs[b2][:],
                                                                    op0=ALU.mult, op1=ALU.add), reads=["y1_%d" % b4, ak], writes=[ak])
                    S.op("vector", lambda e: e.bn_stats(bst[b2][:, 0, :], accs[b2][:, 0:512]), reads=[ak], writes=["bst%d" % b2])
                    S.op("vector", lambda e: e.bn_stats(bst[b2][:, 1, :], accs[b2][:, 512:1024]), reads=[ak], writes=["bst%d" % b2])
                    S.op("vector", lambda e: e.bn_aggr(stt[:, 0:2], bst[b2][:]), reads=["bst%d" % b2], writes=[sk_])
                    S.op("scalar", lambda e: e.activation(out=stt[:, 2:3], in_=stt[:, 1:2], func=AF.Sqrt, bias=epsLN[:], scale=1.0), reads=[sk_, "epsLN"], writes=[sk_])
                    S.op("vector", lambda e: e.reciprocal(stt[:, 2:3], stt[:, 2:3]), reads=[sk_], writes=[sk_])

                def f_s2(tt):
                    g, sub = divmod(tt, 4)
                    b2 = tt % 2
                    stt = st[b2]
                    sk_ = "st_%d" % b2
                    ak = "accs%d" % b2
                    x2k = "x2%d" % b2
                    S.op("vector", lambda e: e.tensor_scalar(x2[b2][:], accs[b2][:], stt[:, 0:1], stt[:, 2:3], ALU.subtract, ALU.mult),
                         reads=[ak, sk_], writes=[x2k])
                    S.op("vector", lambda e: e.tensor_tensor(out=x2[b2][:], in0=x2[b2][:], in1=lng[:], op=ALU.mult), reads=[x2k, "lng"], writes=[x2k])
                    S.op("gpsimd", lambda e: e.tensor_tensor(out=x2[b2][:], in0=x2[b2][:], in1=lnb[:], op=ALU.add), reads=[x2k, "lnb"], writes=[x2k])
                    S.op("sync", lambda e: e.dma_start(out=Xout[tt * 128:(tt + 1) * 128, :], in_=x2[b2][:]), reads=[x2k], writes=["X2_dram"], dma="x2o%d" % b2)
                    if not last:
                        S.op("scalar", lambda e: e.copy(xb[sub][:], x2[b2][:]), reads=[x2k], writes=["xb%d" % sub])
                        if sub == 3:
                            emit_xT_group((ps_t, xTg), lambda s_: "xb%d" % s_, lambda s_: xb[s_], g)

                f_s0(0)
                f_s0(1)
                for step in range(NT + 1):
                    if step + 2 < NT:
                        f_s0(step + 2)
                    if step < NT:
                        f_s1(step)
                    if 0 <= step - 1 < NT:
                        f_s2(step - 1)
                S.barrier()
                S.emit()
    return nc


def make_consts():
    c = np.zeros((128, C_END), np.float32)
    c[:, C_ID:C_ID + 128] = np.eye(128, dtype=np.float32)
    m = np.arange(128)
    sw = np.where((m % 64) < 32, m + 32, m - 32)
    perm = np.zeros((128, 128), np.float32)
    perm[sw, m] = 1.0
    c[:, C_PERM:C_PERM + 128] = perm
    j = np.arange(128)[:, None]
    i = np.arange(128)[None, :]
    c[:, C_MASK:C_MASK + 128] = np.where(i >= j, 0.0, NEG)
    c[:, C_MASK + 128:C_MASK + 256] = np.where(j >= i, 0.0, NEG)
    c[:, C_LS:C_LS + 128] = (j < i).astype(np.float32)
    e1 = np.arange(32)[:, None]
    e2 = np.arange(32)[None, :]
    c[0:32, C_UCAT:C_UCAT + 32] = (e1 <= e2).astype(np.float32)
    c[0:32, C_UCAT + 32:C_UCAT + 64] = np.eye(32, dtype=np.float32)
    c[:, C_THR:C_THR + 33] = (np.arange(33) * BLK).astype(np.float32)[None, :]
    c[:, C_BV:C_BV + NBLK] = np.arange(NBLK, dtype=np.float32)[None, :]
    c[:, C_IE:C_IE + 32] = np.arange(32, dtype=np.float32)[None, :]
    c[:, C_IP] = np.arange(128, dtype=np.float32)
    c[64, C_SEL:C_SEL + 64] = 1.0
    c[:, C_INVW:C_INVW + 3] = np.array([1.0 / 512, 1.0 / 256, 1.0 / 256], np.float32)[None, :]
    half = 32
    inv_freq = (np.float32(10000.0) ** (-np.arange(half, dtype=np.float32) / np.float32(half))).astype(np.float32)
    ang = (np.arange(S_LEN, dtype=np.float32)[:, None] * inv_freq[None, :]).astype(np.float32)
    cos = np.cos(ang).astype(np.float32).T
    sin = np.sin(ang).astype(np.float32).T
    rc = np.zeros((128, S_LEN), np.float32)
    rs = np.zeros((128, S_LEN), np.float32)
    for p in range(128):
        cc = p % 64
        rc[p] = cos[cc % 32]
        rs[p] = -sin[cc] if cc < 32 else sin[cc - 32]
    return c, rc, rs


_CACHE = {}


def kernel(**inputs):
    depth = DEPTH
    if "nc" not in _CACHE:
        _CACHE["nc"] = build_program(depth)
        _CACHE["consts"] = make_consts()
    nc = _CACHE["nc"]
    c, rc, rs = _CACHE["consts"]
    x = np.ascontiguousarray(np.asarray(inputs["x"], dtype=np.float32))
    shared = {k: np.ascontiguousarray(np.asarray(v, dtype=np.float32)) for k, v in inputs.items() if k != "x"}
    shared["consts"] = c
    shared["ropecos"] = rc
    shared["ropesin"] = rs
    in_maps = []
    for i in range(NCORES):
        m = dict(shared)
        m["x"] = x[i]
        in_maps.append(m)
    res = run_bass_kernel_spmd(nc, in_maps, core_ids=list(range(NCORES)))
    return np.stack([np.asarray(r["out"], dtype=np.float32) for r in res.results], axis=0)
```

```python
import os
import numpy as np
from contextlib import ExitStack
import concourse.bass as bass
import concourse.mybir as mybir
from concourse.bass_utils import run_bass_kernel_spmd

F32 = mybir.dt.float32
BF16 = mybir.dt.bfloat16
I32 = mybir.dt.int32
AF = mybir.ActivationFunctionType
ALU = mybir.AluOpType
AX = mybir.AxisListType

NCORES = 8
S_LEN = 8192
DM = 1024
DEPTH = 4
PW = 2816
NE = 32
DE = 512
NT = S_LEN // 128
NG = S_LEN // 512
BLK = 512
NBLK = 63
NSLOT = NBLK * BLK
ALPHA = (2.0 * DEPTH) ** 0.25
EPS = 1e-5
BRANCH_D = (1, 4, 16)
NEG = -30000.0

C_ID, C_PERM, C_MASK, C_LS, C_UCAT, C_THR, C_BV, C_IE, C_IP, C_SEL, C_INVW, C_END = (
    0, 128, 256, 512, 640, 704, 737, 801, 833, 834, 898, 904)

ENGS = ["tensor", "vector", "scalar", "gpsimd", "sync"]


class Sched:
    def __init__(self, nc, stack):
        self.nc = nc
        self.stack = stack
        self.q = {e: [] for e in ENGS}
        self.sems = {}
        self.cnt = {}
        self.seen = {e: {} for e in ENGS}
        self.last_w = {}
        self.readers = {}
        self.epoch = 0
        for e in ENGS:
            self._sem("E_%s_0" % e)
        self.n_ops = 0
        self.breg = None
        self.breg_val = None

    def new_epoch(self):
        self.epoch += 1
        self._sem("E_tensor_%d" % self.epoch)

    def _sem(self, name):
        if name not in self.sems:
            self.sems[name] = self.stack.enter_context(self.nc.semaphore(name))
            self.cnt[name] = 0
        return self.sems[name]

    def op(self, eng, fn, reads=(), writes=(), dma=None):
        pr = [k for k in reads if k.startswith(("ps", "e_ps", "e_wg", "e_wu"))]
        if pr:
            writes = list(writes) + pr
        deps = {}

        def add(ev):
            if ev is not None and deps.get(ev[0], 0) < ev[1]:
                deps[ev[0]] = ev[1]

        for k in reads:
            add(self.last_w.get(k))
        for k in writes:
            add(self.last_w.get(k))
            for ev in self.readers.get(k, ()):
                add(ev)
        waits = []
        seen = self.seen[eng]
        for s, v in deps.items():
            if eng == "tensor" and s.startswith("E_tensor"):
                continue
            if seen.get(s, 0) < v:
                seen[s] = v
                waits.append((self.sems[s], v))
        if dma is None:
            sname, inc = "E_%s_%d" % (eng, self.epoch if eng == "tensor" else 0), 1
        else:
            sname, inc = "D_" + dma, 16
            self._sem(sname)
        self.cnt[sname] += inc
        ev = (sname, self.cnt[sname])
        self.q[eng].append((waits, fn, self.sems[sname], inc))
        for k in reads:
            self.readers.setdefault(k, []).append(ev)
        for k in writes:
            self.last_w[k] = ev
            self.readers[k] = []
        self.n_ops += 1
        return ev

    def barrier(self):
        for e in ENGS:
            waits = []
            for s, c in self.cnt.items():
                if c > 0 and self.seen[e].get(s, 0) < c:
                    self.seen[e][s] = c
                    waits.append((self.sems[s], c))
            if waits:
                self.q[e].append((waits, None, None, 0))
        self.last_w = {}
        self.readers = {}

    def emit(self, want_breg=False):
        with self.nc.Block() as block:
            for e in ENGS:
                items = self.q[e]

                def body(engine, items=items, e=e):
                    if e == "gpsimd" and want_breg:
                        self.breg = engine.to_reg(self.breg_val)
                    for waits, fn, sem, inc in items:
                        for s, v in waits:
                            engine.wait_ge(s, v)
                        if fn is not None:
                            fn(engine).then_inc(sem, inc)

                getattr(block, e)(body)
        self.q = {e: [] for e in ENGS}


def build_program(depth=DEPTH, dbg=None):
    nc = bass.Bass("TRN2", target_bir_lowering=False)
    dt_in = lambda name, shape, dt=F32: nc.dram_tensor(name, shape, dt, kind="ExternalInput").ap()
    dt_sc = lambda name, shape, dt: nc.dram_tensor(name, shape, dt, kind=("ExternalOutput" if dbg else "Internal")).ap()
    L = depth
    x_in = dt_in("x", [S_LEN, DM])
    w_in = dt_in("w_in", [L, DM, PW])
    w_out = dt_in("w_out", [L, DM, DM])
    branch_gain = dt_in("branch_gain", [L, DM])
    sgu_gain = dt_in("sgu_gain", [L, 256])
    sgu_w = dt_in("sgu_w", [L, 4, 128, 128])
    sgu_b = dt_in("sgu_b", [L, 4, 128])
    conv_w = dt_in("conv_w", [L, 3, 256])
    ln_gain = dt_in("ln_gain", [L, 2, DM])
    ln_bias = dt_in("ln_bias", [L, 2, DM])
    rgw = dt_in("router_group_w", [L, DM, 4])
    rgb = dt_in("router_group_b", [L, 4])
    rew = dt_in("router_expert_w", [L, DM, NE])
    reb = dt_in("router_expert_b", [L, NE])
    ewg = dt_in("expert_w_gate", [L, NE, DM, DE])
    ewu = dt_in("expert_w_up", [L, NE, DM, DE])
    ewd = dt_in("expert_w_down", [L, NE, DE, DM])
    consts = dt_in("consts", [128, C_END])
    ropecos = dt_in("ropecos", [128, S_LEN])
    ropesin = dt_in("ropesin", [128, S_LEN])
    out = nc.dram_tensor("out", [S_LEN, DM], F32, kind="ExternalOutput").ap()

    xT = dt_sc("xT", [8, 128, S_LEN], BF16)
    QT = dt_sc("QT", [4, 128, S_LEN], BF16)
    KT = dt_sc("KT", [4, 128, S_LEN], BF16)
    Vd = dt_sc("Vd", [S_LEN, 512], BF16)
    mixT = dt_sc("mixT", [8, 128, S_LEN], BF16)
    X1 = dt_sc("X1", [S_LEN, DM], F32)
    X1b = dt_sc("X1b", [S_LEN, DM], BF16)
    X2 = dt_sc("X2", [S_LEN, DM], F32)
    Xs = dt_sc("Xs", [NSLOT, DM], BF16)
    Ys = dt_sc("Ys", [NSLOT, DM], BF16)

    with ExitStack() as top:
        S = Sched(nc, top)
        S.breg_val = L * NE * 128 * 2 - 1

        uniq = [0]

        def SB(st, name, shape, dt):
            uniq[0] += 1
            return st.enter_context(nc.sbuf_tensor("%s_%d" % (name, uniq[0]), shape, dt))

        def PS(st, name, shape, dt=F32):
            uniq[0] += 1
            return st.enter_context(nc.psum_tensor("%s_%d" % (name, uniq[0]), shape, dt))

        cst = SB(top, "cst", [128, C_END], F32)
        ident_bf = SB(top, "ident_bf", [128, 128], BF16)
        perm_bf = SB(top, "perm_bf", [128, 128], BF16)
        mask_bf = SB(top, "mask_bf", [128, 256], BF16)
        ls_bf = SB(top, "ls_bf", [128, 128], BF16)
        ones_bf = SB(top, "ones_bf", [128, 128], BF16)
        zeroc = SB(top, "zeroc", [128, 1], F32)
        epsc = SB(top, "epsc", [128, 1], F32)
        epsA2 = SB(top, "epsA2", [128, 1], F32)
        epsLN = SB(top, "epsLN", [128, 1], F32)
        M1all = SB(top, "M1all", [128, NT, NE], F32)
        M2all = SB(top, "M2all", [128, NT, NE], F32)
        RK = SB(top, "RK", [128, NT, 2], F32)
        W01 = SB(top, "W01", [128, NT, 2], F32)
        DSTi = SB(top, "DSTi", [128, NT, 2], I32)
        GIDX = SB(top, "GIDX", [128, NBLK], I32)
        GIDX2 = SB(top, "GIDX2", [128, NBLK, 2], I32)

        S.op("sync", lambda e: e.dma_start(out=cst[:], in_=consts), writes=["cst"], dma="cst")
        S.op("vector", lambda e: e.tensor_copy(ident_bf[:], cst[:, C_ID:C_ID + 128]), reads=["cst"], writes=["ident_bf"])
        S.op("vector", lambda e: e.tensor_copy(perm_bf[:], cst[:, C_PERM:C_PERM + 128]), reads=["cst"], writes=["perm_bf"])
        S.op("vector", lambda e: e.tensor_copy(mask_bf[:], cst[:, C_MASK:C_MASK + 256]), reads=["cst"], writes=["mask_bf"])
        S.op("vector", lambda e: e.tensor_copy(ls_bf[:], cst[:, C_LS:C_LS + 128]), reads=["cst"], writes=["ls_bf"])
        S.op("vector", lambda e: e.memset(ones_bf[:], 1.0), writes=["ones_bf"])
        S.op("vector", lambda e: e.memset(zeroc[:], 0.0), writes=["zeroc"])
        S.op("vector", lambda e: e.memset(epsc[:], EPS), writes=["epsc"])
        S.op("vector", lambda e: e.memset(epsA2[:], EPS * ALPHA * ALPHA), writes=["epsA2"])
        S.op("vector", lambda e: e.memset(epsLN[:], EPS / (ALPHA * ALPHA)), writes=["epsLN"])
        S.barrier()
        S.emit()

        def emit_xT_group(st_tiles, src_key_fn, src_ap_fn, g):
            ps_t, xTg = st_tiles
            for sub in range(4):
                pk = "ps_xt%d" % (sub % 2)
                for j in range(8):
                    S.op("tensor", lambda e, sub=sub, j=j: e.transpose(
                        ps_t[sub % 2][:, j, :], src_ap_fn(sub)[:, j * 128:(j + 1) * 128], ident_bf[:]),
                        reads=[src_key_fn(sub), "ident_bf"], writes=[pk])
                S.op("scalar", lambda e, sub=sub: e.copy(xTg[:, :, sub * 128:(sub + 1) * 128], ps_t[sub % 2][:]),
                     reads=[pk], writes=["xTg"])
            S.op("sync", lambda e: e.dma_start(
                out=xT.rearrange("c p t -> p c t")[:, :, g * 512:(g + 1) * 512], in_=xTg[:]),
                reads=["xTg"], writes=["xT_dram"], dma="xTg")

        with ExitStack() as ph:
            xin = [SB(ph, "p_xin%d" % i, [128, DM], F32) for i in range(2)]
            xb = [SB(ph, "p_xb%d" % i, [128, DM], BF16) for i in range(4)]
            ps_t = [PS(ph, "p_pst%d" % i, [128, 8, 128], BF16) for i in range(2)]
            xTg = SB(ph, "p_xTg", [128, 8, 512], BF16)
            for g in range(NG):
                for sub in range(4):
                    tt = g * 4 + sub
                    k = "xin%d" % (tt % 2)
                    S.op("sync", lambda e, tt=tt: e.dma_start(out=xin[tt % 2][:], in_=x_in[tt * 128:(tt + 1) * 128, :]),
                         writes=[k], dma=k)
                    S.op("vector", lambda e, tt=tt, sub=sub: e.tensor_copy(xb[sub][:], xin[tt % 2][:]),
                         reads=[k], writes=["xb%d" % sub])
                emit_xT_group((ps_t, xTg), lambda sub: "xb%d" % sub, lambda sub: xb[sub], g)
            S.barrier()
            S.emit()

        for l in range(L):
            if l > 0:
                S.new_epoch()
            Xin = x_in if l == 0 else X2
            Xout = out if l == L - 1 else X2
            with ExitStack() as ph:
                win = SB(ph, "a_win", [128, 8, PW], BF16)
                xt = [SB(ph, "a_xt%d" % i, [128, 8, 512], BF16) for i in range(2)]
                cs = [SB(ph, "a_cos%d" % i, [128, 512], F32) for i in range(2)]
                sn = [SB(ph, "a_sin%d" % i, [128, 512], F32) for i in range(2)]
                qbf = [SB(ph, "a_qbf%d" % i, [128, 512], BF16) for i in range(2)]
                t1 = [SB(ph, "a_t1%d" % i, [128, 512], F32) for i in range(2)]
                t2 = [SB(ph, "a_t2%d" % i, [128, 512], F32) for i in range(2)]
                qr = [SB(ph, "a_qr%d" % i, [128, 512], BF16) for i in range(2)]
                vsb = [SB(ph, "a_v%d" % i, [128, 4, 512], BF16) for i in range(2)]
                ga = [SB(ph, "a_ga%d" % i, [128, 512], F32) for i in range(2)]
                gt = [SB(ph, "a_gt%d" % i, [128, 512], F32) for i in range(2)]
                ug = [SB(ph, "a_ug%d" % i, [64, 512], F32) for i in range(4)]
                zg = [SB(ph, "a_zg%d" % i, [128, 256], F32) for i in range(2)]
                zsq = SB(ph, "a_zsq", [128, 256], F32)
                zst = SB(ph, "a_zst", [128, 16], F32)
                zn = [SB(ph, "a_zn%d" % i, [128, 4, 64], BF16) for i in range(4)]
                spv = [SB(ph, "a_spv%d" % i, [64, 512], F32) for i in range(2)]
                yb = [SB(ph, "a_yb%d" % i, [64, 512], BF16) for i in range(2)]
                wsf = SB(ph, "a_wsf", [128, 4, 128], F32)
                wsT = SB(ph, "a_wsT", [128, 4, 128], BF16)
                bsb = SB(ph, "a_bsb", [64, 4, 128], F32)
                sgain = SB(ph, "a_sgain", [64, 4], F32)
                cw = SB(ph, "a_cw", [128, 2, 3], F32)
                hs = [SB(ph, "a_hs%d" % i, [128, 512], F32) for i in range(2)]
                gbs = [SB(ph, "a_gbs%d" % i, [128, 512], F32) for i in range(2)]
                zce = SB(ph, "a_zce", [128, 2, 514], F32)
                yc = [SB(ph, "a_yc%d" % i, [128, 512], F32) for i in range(2)]
                ycb = [SB(ph, "a_ycb%d" % i, [128, 512], BF16) for i in range(2)]
                psA = [PS(ph, "a_ps%d" % i, [128, 512]) for i in range(7)]
                psW = psA[6][:].rearrange("p (g t) -> p g t", g=4)

                for dc in range(8):
                    S.op("gpsimd", lambda e, dc=dc: e.dma_start(out=win[:, dc, :], in_=w_in[l, dc * 128:(dc + 1) * 128, :]),
                         writes=["win"], dma="win")
                S.op("sync", lambda e: e.dma_start(out=wsf[:], in_=sgu_w[l].rearrange("g t s -> t g s")), writes=["wsf"], dma="wsf")
                for g in range(4):
                    S.op("gpsimd", lambda e, g=g: e.affine_select(out=wsf[:, g, :], in_=wsf[:, g, :], pattern=[[-1, 128]],
                                                                  compare_op=ALU.is_ge, fill=0.0, base=0, channel_multiplier=1),
                         reads=["wsf"], writes=["wsf"])
                for g in range(4):
                    S.op("tensor", lambda e, g=g: e.transpose(psW[:, g, :], wsf[:, g, :], cst[:, C_ID:C_ID + 128]),
                         reads=["wsf", "cst"], writes=["psA6"])
                S.op("vector", lambda e: e.tensor_copy(wsT[:], psW), reads=["psA6"], writes=["wsT"])
                for g in range(4):
                    S.op("sync", lambda e, g=g: e.dma_start(out=bsb[:, g, :], in_=sgu_b[l, g:g + 1, :].partition_broadcast(64)),
                         writes=["bsb"], dma="bsb")
                S.op("sync", lambda e: e.dma_start(out=sgain[:], in_=sgu_gain[l].rearrange("(g c) -> c g", g=4),
                                                   allow_slow_non_contiguous=True), writes=["sgain"], dma="sgain")
                for cc in range(2):
                    for k in range(3):
                        S.op("sync", lambda e, cc=cc, k=k: e.dma_start(out=cw[:, cc, k:k + 1], in_=conv_w[l, k:k + 1, cc * 128:(cc + 1) * 128].rearrange("k p -> p k"),
                                                                       allow_slow_non_contiguous=True), writes=["cw"], dma="cw")
                S.op("vector", lambda e: e.memset(zce[:], 0.0), writes=["zce"])

                psn = [0]

                def next_ps():
                    i = psn[0] % 7
                    psn[0] += 1
                    return i

                def gelu(src_ap, src_key, out_ap, out_key, P, N, bi):
                    a = ga[bi][0:P, 0:N]
                    t = gt[bi][0:P, 0:N]
                    ak, tk = "ga%d" % bi, "gt%d" % bi
                    S.op("scalar", lambda e: e.copy(a, src_ap), reads=[src_key], writes=[ak])
                    S.op("gpsimd", lambda e: e.tensor_tensor(out=t, in0=a, in1=a, op=ALU.mult), reads=[ak], writes=[tk])
                    S.op("vector", lambda e: e.tensor_scalar(t, t, 0.044715, 1.0, ALU.mult, ALU.add), reads=[tk], writes=[tk])
                    S.op("vector", lambda e: e.tensor_tensor(out=t, in0=t, in1=a, op=ALU.mult), reads=[tk, ak], writes=[tk])
                    S.op("scalar", lambda e: e.activation(out=t, in_=t, func=AF.Sigmoid, bias=zeroc[0:P, :], scale=1.5957691216),
                         reads=[tk, "zeroc"], writes=[tk])
                    S.op("vector", lambda e: e.tensor_tensor(out=out_ap, in0=a, in1=t, op=ALU.mult), reads=[ak, tk], writes=[out_key])

                gcount = [0]
                def a_load(i):
                    S.op("sync", lambda e: e.dma_start(out=xt[i % 2][:], in_=xT.rearrange("c p t -> p c t")[:, :, i * 512:(i + 1) * 512]),
                         reads=["xT_dram"], writes=["xt%d" % (i % 2)], dma="xt%d" % (i % 2))
                    S.op("sync", lambda e: e.dma_start(out=cs[i % 2][:], in_=ropecos[:, i * 512:(i + 1) * 512]),
                         writes=["cs%d" % (i % 2)], dma="cs%d" % (i % 2))
                    S.op("sync", lambda e: e.dma_start(out=sn[i % 2][:], in_=ropesin[:, i * 512:(i + 1) * 512]),
                         writes=["sn%d" % (i % 2)], dma="sn%d" % (i % 2))

                a_load(0)
                for i in range(NG):
                    tsl = slice(i * 512, (i + 1) * 512)
                    xk = "xt%d" % (i % 2)
                    xti = xt[i % 2]
                    csk, snk = "cs%d" % (i % 2), "sn%d" % (i % 2)
                    def qk_main(qi, xti=xti):
                        c = qi % 4
                        isq = qi < 4
                        col0 = (0 if isq else 512) + c * 128
                        scale = 0.125 if isq else 1.0
                        p = next_ps()
                        pk = "psA%d" % p
                        for dc in range(8):
                            S.op("tensor", lambda e, dc=dc: e.matmul(
                                psA[p][:], win[:, dc, col0:col0 + 128], xti[:, dc, :], start=(dc == 0), stop=(dc == 7)),
                                reads=["win", xk], writes=[pk])
                        b2 = qi % 2
                        S.op("scalar", lambda e: e.activation(
                            out=qbf[b2][:], in_=psA[p][:], func=AF.Identity, bias=zeroc[:], scale=scale),
                            reads=[pk, "zeroc"], writes=["qbf%d" % b2])
                        return p

                    def qk_rope(qi, p, i=i, xti=xti):
                        c = qi % 4
                        isq = qi < 4
                        scale = 0.125 if isq else 1.0
                        pk = "psA%d" % p
                        b2 = qi % 2
                        S.op("vector", lambda e: e.scalar_tensor_tensor(
                            out=t2[b2][:], in0=psA[p][:], scalar=scale, in1=cs[i % 2][:], op0=ALU.mult, op1=ALU.mult),
                            reads=[pk, csk], writes=["t2%d" % b2])
                        p2 = next_ps()
                        if p2 == p:
                            p2 = next_ps()
                        pk2 = "psA%d" % p2
                        S.op("tensor", lambda e: e.matmul(psA[p2][:], perm_bf[:], qbf[b2][:], start=True, stop=True),
                             reads=["perm_bf", "qbf%d" % b2], writes=[pk2])
                        S.op("vector", lambda e: e.tensor_tensor(
                            out=t1[b2][:], in0=psA[p2][:], in1=sn[i % 2][:], op=ALU.mult),
                            reads=[pk2, snk], writes=["t1%d" % b2])
                        S.op("gpsimd", lambda e: e.tensor_tensor(out=qr[b2][:], in0=t1[b2][:], in1=t2[b2][:], op=ALU.add),
                             reads=["t1%d" % b2, "t2%d" % b2], writes=["qr%d" % b2])
                        dst = QT if isq else KT
                        S.op("sync", lambda e: e.dma_start(
                            out=dst[c, :, i * 512:(i + 1) * 512], in_=qr[b2][:]),
                            reads=["qr%d" % b2], writes=["QK_dram"], dma="qr%d" % b2)

                    pend = None
                    for qi in range(8):
                        p_ = qk_main(qi)
                        if pend is not None:
                            qk_rope(*pend)
                        pend = (qi, p_)
                    qk_rope(*pend)
                    for cc in range(2):
                        pgb, pgc, ph_ = next_ps(), next_ps(), next_ps()
                        for (p, col0) in ((pgb, 2048), (pgc, 2304), (ph_, 2560)):
                            for dc in range(8):
                                S.op("tensor", lambda e, p=p, dc=dc, col0=col0, cc=cc, xti=xti: e.matmul(
                                    psA[p][:], win[:, dc, col0 + cc * 128:col0 + (cc + 1) * 128], xti[:, dc, :], start=(dc == 0), stop=(dc == 7)),
                                    reads=["win", xk], writes=["psA%d" % p])
                        S.op("scalar", lambda e, cc=cc, ph_=ph_: e.copy(hs[cc][:], psA[ph_][:]), reads=["psA%d" % ph_], writes=["hs%d" % cc])
                        S.op("scalar", lambda e, cc=cc, pgb=pgb: e.copy(gbs[cc][:], psA[pgb][:]), reads=["psA%d" % pgb], writes=["gbs%d" % cc])
                        zk = "zce%d" % cc
                        if i > 0:
                            S.op("gpsimd", lambda e, cc=cc: e.tensor_copy(zce[:, cc, 0:2], zce[:, cc, 512:514]), reads=[zk, "zce"], writes=[zk])
                        S.op("vector", lambda e, cc=cc, pgc=pgc: e.tensor_tensor(out=zce[:, cc, 2:514], in0=psA[pgc][:], in1=hs[cc][:], op=ALU.mult),
                             reads=["psA%d" % pgc, "hs%d" % cc, "zce"], writes=[zk])
                        S.op("scalar", lambda e, cc=cc: e.activation(out=yc[cc][:], in_=zce[:, cc, 2:514], func=AF.Identity, bias=zeroc[:], scale=cw[:, cc, 2:3]),
                             reads=[zk, "cw", "zeroc"], writes=["yc%d" % cc])
                        S.op("vector", lambda e, cc=cc: e.scalar_tensor_tensor(out=yc[cc][:], in0=zce[:, cc, 1:513], scalar=cw[:, cc, 1:2], in1=yc[cc][:],
                                                                               op0=ALU.mult, op1=ALU.add), reads=[zk, "cw", "yc%d" % cc], writes=["yc%d" % cc])
                        S.op("vector", lambda e, cc=cc: e.scalar_tensor_tensor(out=yc[cc][:], in0=zce[:, cc, 0:512], scalar=cw[:, cc, 0:1], in1=yc[cc][:],
                                                                               op0=ALU.mult, op1=ALU.add), reads=[zk, "cw", "yc%d" % cc], writes=["yc%d" % cc])
                        S.op("gpsimd", lambda e, cc=cc: e.tensor_tensor(out=ycb[cc][:], in0=gbs[cc][:], in1=yc[cc][:], op=ALU.mult),
                             reads=["gbs%d" % cc, "yc%d" % cc], writes=["ycb%d" % cc])
                        S.op("sync", lambda e, cc=cc, i=i: e.dma_start(out=mixT[6 + cc, :, i * 512:(i + 1) * 512], in_=ycb[cc][:]),
                             reads=["ycb%d" % cc], writes=["mix_dram"], dma="ycb%d" % cc)
                    if i + 1 < NG:
                        a_load(i + 1)
                    vb = i % 2
                    for sub in range(4):
                        p = next_ps()
                        pk = "psA%d" % p
                        for dc in range(8):
                            S.op("tensor", lambda e, p=p, dc=dc, sub=sub, xti=xti: e.matmul(
                                psA[p][:], xti[:, dc, sub * 128:(sub + 1) * 128], win[:, dc, 1024:1536], start=(dc == 0), stop=(dc == 7)),
                                reads=["win", xk], writes=[pk])
                        S.op("scalar", lambda e, p=p, sub=sub, vb=vb: e.copy(vsb[vb][:, sub, :], psA[p][:]),
                             reads=[pk], writes=["vsb%d" % vb])
                    S.op("sync", lambda e, vb=vb, i=i: e.dma_start(
                        out=Vd[i * 512:(i + 1) * 512, :].rearrange("(s p) f -> p s f", p=128), in_=vsb[vb][:]),
                        reads=["vsb%d" % vb], writes=["V_dram"], dma="vsb%d" % vb)
                    for sub in range(4):
                        p = next_ps()
                        pk = "psA%d" % p
                        for dc in range(8):
                            S.op("tensor", lambda e, p=p, dc=dc, sub=sub, xti=xti: e.matmul(
                                psA[p][:, 0:256], xti[:, dc, sub * 128:(sub + 1) * 128], win[:, dc, 1792:2048], start=(dc == 0), stop=(dc == 7)),
                                reads=["win", xk], writes=[pk])
                        zb = sub % 2
                        zk = "zg%d" % zb
                        gelu(psA[p][:, 0:256], pk, zg[zb][:], zk, 128, 256, gcount[0] % 2)
                        gcount[0] += 1
                        z3 = zg[zb][:].rearrange("p (g c) -> p g c", g=4)
                        S.op("vector", lambda e, z3=z3: e.tensor_reduce(zst[:, 0:4], z3, axis=AX.X, op=ALU.add), reads=[zk], writes=["zst"])
                        S.op("gpsimd", lambda e, zb=zb: e.tensor_tensor(out=zsq[:], in0=zg[zb][:], in1=zg[zb][:], op=ALU.mult), reads=[zk], writes=["zsq"])
                        S.op("vector", lambda e: e.tensor_reduce(zst[:, 4:8], zsq[:].rearrange("p (g c) -> p g c", g=4), axis=AX.X, op=ALU.add),
                             reads=["zsq"], writes=["zst"])
                        S.op("vector", lambda e: e.tensor_scalar(zst[:, 0:4], zst[:, 0:4], 1.0 / 64, None, ALU.mult), reads=["zst"], writes=["zst"])
                        S.op("vector", lambda e: e.tensor_tensor(out=zst[:, 8:12], in0=zst[:, 0:4], in1=zst[:, 0:4], op=ALU.mult), reads=["zst"], writes=["zst"])
                        S.op("vector", lambda e: e.scalar_tensor_tensor(out=zst[:, 4:8], in0=zst[:, 4:8], scalar=1.0 / 64, in1=zst[:, 8:12],
                                                                        op0=ALU.mult, op1=ALU.subtract), reads=["zst"], writes=["zst"])
                        S.op("scalar", lambda e: e.activation(out=zst[:, 4:8], in_=zst[:, 4:8], func=AF.Sqrt, bias=epsc[:], scale=1.0),
                             reads=["zst", "epsc"], writes=["zst"])
                        S.op("vector", lambda e: e.reciprocal(zst[:, 4:8], zst[:, 4:8]), reads=["zst"], writes=["zst"])
                        S.op("vector", lambda e, z3=z3: e.tensor_tensor(out=z3, in0=z3, in1=zst[:, 0:4].unsqueeze(2).to_broadcast([128, 4, 64]), op=ALU.subtract),
                             reads=[zk, "zst"], writes=[zk])
                        S.op("vector", lambda e, z3=z3, sub=sub: e.tensor_tensor(out=zn[sub][:], in0=z3, in1=zst[:, 4:8].unsqueeze(2).to_broadcast([128, 4, 64]), op=ALU.mult),
                             reads=[zk, "zst"], writes=["zn%d" % sub])
                    for g in range(4):
                        p = next_ps()
                        pk = "psA%d" % p
                        for dc in range(8):
                            S.op("tensor", lambda e, p=p, dc=dc, g=g, xti=xti: e.matmul(
                                psA[p][0:64, :], win[:, dc, 1536 + 64 * g:1536 + 64 * (g + 1)], xti[:, dc, :], start=(dc == 0), stop=(dc == 7)),
                                reads=["win", xk], writes=[pk])
                        gelu(psA[p][0:64, :], pk, ug[g][:], "ug%d" % g, 64, 512, gcount[0] % 2)
                        gcount[0] += 1
                    for g in range(4):
                        p = next_ps()
                        pk = "psA%d" % p
                        for sub in range(4):
                            S.op("tensor", lambda e, p=p, g=g, sub=sub: e.matmul(
                                psA[p][0:64, sub * 128:(sub + 1) * 128], zn[sub][:, g, :], wsT[:, g, :], start=True, stop=True),
                                reads=["zn%d" % sub, "wsT"], writes=[pk])
                        sb2 = g % 2
                        S.op("vector", lambda e, p=p, g=g, sb2=sb2: e.scalar_tensor_tensor(
                            out=spv[sb2][:].rearrange("p (s t) -> p s t", s=4), in0=psA[p][0:64, :].rearrange("p (s t) -> p s t", s=4),
                            scalar=sgain[:, g:g + 1], in1=bsb[:, g:g + 1, :].to_broadcast([64, 4, 128]), op0=ALU.mult, op1=ALU.add),
                            reads=[pk, "sgain", "bsb"], writes=["spv%d" % sb2])
                        S.op("gpsimd", lambda e, g=g, sb2=sb2: e.tensor_tensor(out=yb[sb2][:], in0=spv[sb2][:], in1=ug[g][:], op=ALU.mult),
                             reads=["spv%d" % sb2, "ug%d" % g], writes=["yb%d" % sb2])
                        S.op("sync", lambda e, g=g, sb2=sb2, i=i: e.dma_start(
                            out=mixT[4 + g // 2, (g % 2) * 64:(g % 2) * 64 + 64, i * 512:(i + 1) * 512], in_=yb[sb2][:]),
                            reads=["yb%d" % sb2], writes=["mix_dram"], dma="yb%d" % sb2)
                S.barrier()
                S.emit()

            with ExitStack() as ph:
                qn = SB(ph, "b_qn", [128, S_LEN], BF16)
                kn = SB(ph, "b_kn", [128, S_LEN], BF16)
                qd = SB(ph, "b_qd", [128, S_LEN], BF16)
                kd = SB(ph, "b_kd", [128, S_LEN], BF16)
                vas = [SB(ph, "b_va%d" % i, [128, NT, 2, 65], BF16) for i in range(2)]
                acc = SB(ph, "b_acc", [65, 2, S_LEN], F32)
                pt = [SB(ph, "b_pt%d" % i, [128, 2, 256], BF16) for i in range(4)]
                m01 = SB(ph, "b_m01", [128, 2, 256], BF16)
                rec = [SB(ph, "b_rec%d" % i, [64, 512], F32) for i in range(2)]
                yaT = [SB(ph, "b_yaT%d" % i, [64, 2048], BF16) for i in range(2)]
                sel_f = cst[0:65, C_SEL:C_SEL + 64]
                ps_s = [PS(ph, "b_pss%d" % i, [128, 2, 256]) for i in range(3)]
                ps_o = [PS(ph, "b_pso%d" % i, [65, 512]) for i in range(3)]
                ps_d = [PS(ph, "b_psd%d" % i, [64, 512]) for i in range(2)]
                for s_ in range(2):
                    S.op("vector", lambda e, s_=s_: e.tensor_scalar(m01[:, s_, :], cst[:, C_MASK:C_MASK + 256], 0.0, None, ALU.is_equal),
                         reads=["cst"], writes=["m01"])
                S.op("vector", lambda e: e.memset(vas[0][:], 1.0), writes=["va0"])
                S.op("gpsimd", lambda e: e.memset(vas[1][:], 1.0), writes=["va1"])
                pcount = [0]

                def load_v(job):
                    c_, bi_ = divmod(job, 3)
                    d_ = BRANCH_D[bi_]
                    nb_ = NT // d_
                    vi = job % 2
                    vsrc = Vd.rearrange("(n j r) f -> j r n f", j=128, r=d_)
                    for r in range(d_):
                        for h2 in range(2):
                            S.op("sync", lambda e, r=r, h2=h2: e.dma_start(
                                out=vas[vi][:, r * nb_:(r + 1) * nb_, h2, 0:64],
                                in_=vsrc[:, r, :, c_ * 128 + h2 * 64:c_ * 128 + (h2 + 1) * 64]),
                                reads=["V_dram"], writes=["va%d" % vi], dma="va%d" % vi)

                load_v(0)

                def load_qk(c_):
                    S.op("sync", lambda e: e.dma_start(out=qn[:], in_=QT[c_]), reads=["QK_dram"], writes=["qn"], dma="qn")
                    S.op("sync", lambda e: e.dma_start(out=kn[:], in_=KT[c_]), reads=["QK_dram"], writes=["kn"], dma="kn")

                def deint(d_):
                    S.op("vector", lambda e: e.tensor_copy(qd[:].rearrange("p (r m) -> p r m", r=d_),
                                                           qn[:].rearrange("p (m r) -> p r m", r=d_)), reads=["qn"], writes=["qd"])
                    S.op("gpsimd", lambda e: e.tensor_copy(kd[:].rearrange("p (r m) -> p r m", r=d_),
                                                           kn[:].rearrange("p (m r) -> p r m", r=d_)), reads=["kn"], writes=["kd"])

                load_qk(0)
                for c in range(4):
                    for bi, d in enumerate(BRANCH_D):
                        nb = NT // d
                        if bi == 0:
                            deint(4)
                            qs, ks, qsk, ksk = qn, kn, "qn", "kn"
                        elif bi == 1:
                            qs, ks, qsk, ksk = qd, kd, "qd", "kd"
                        else:
                            qs, ks, qsk, ksk = qd, kd, "qd", "kd"
                            deint(16)
                            if c + 1 < 4:
                                load_qk(c + 1)
                        job = c * 3 + bi
                        if job + 1 < 12:
                            load_v(job + 1)
                        va = vas[job % 2]
                        vak = "va%d" % (job % 2)
                        for hh in range(2):
                            P0 = hh * 64
                            started = {}

                            def emit_scores(kp, P0=P0, qs=qs, ks=ks, qsk=qsk, ksk=ksk, nb=nb):
                                sb_i = kp % 3
                                sk = "pss%d" % sb_i
                                for T in (2 * kp, 2 * kp + 1):
                                    n = T % nb
                                    N = 256 if n < nb - 1 else 128
                                    slot = T % 2
                                    S.op("tensor", lambda e, sb_i=sb_i, slot=slot, N=N, T=T: e.matmul(
                                        ps_s[sb_i][:, slot, 0:N], ks[P0:P0 + 64, T * 128:(T + 1) * 128], qs[P0:P0 + 64, T * 128:T * 128 + N],
                                        start=True, stop=True, skip_group_check=True), reads=[ksk, qsk], writes=[sk])
                                pi = pcount[0] % 4
                                pcount[0] += 1
                                S.op("scalar", lambda e, sb_i=sb_i, pi=pi: e.activation(out=pt[pi][:], in_=ps_s[sb_i][:], func=AF.Exp,
                                                                                        bias=zeroc[:], scale=1.0),
                                     reads=[sk, "zeroc"], writes=["pt%d" % pi])
                                meng = "vector" if (kp % 3 == 0) else "gpsimd"
                                S.op(meng, lambda e, pi=pi: e.tensor_tensor(out=pt[pi][:], in0=pt[pi][:], in1=m01[:], op=ALU.mult),
                                     reads=["pt%d" % pi, "m01"], writes=["pt%d" % pi])
                                return pi

                            def emit_pv(kp, pi, hh=hh, nb=nb, d=d, bi=bi, started=started, va=va, vak=vak):
                                ptk = "pt%d" % pi
                                for T2 in (2 * kp, 2 * kp + 1):
                                    n2 = T2 % nb
                                    N2 = 256 if n2 < nb - 1 else 128
                                    sl2 = T2 % 2
                                    if N2 == 256 and (T2 % 4) != 3:
                                        pieces = [(T2, 0, 256)]
                                    else:
                                        pieces = [(T2, 0, 128)]
                                        if N2 == 256:
                                            pieces.append((T2 + 1, 128, 128))
                                    for (qb, off, wdt) in pieces:
                                        B = qb // 4
                                        ob = B % 3
                                        first = B not in started
                                        started[B] = True
                                        S.op("tensor", lambda e, ob=ob, qb=qb, off=off, wdt=wdt, T2=T2, sl2=sl2, first=first: e.matmul(
                                            ps_o[ob][:, (qb % 4) * 128:(qb % 4) * 128 + wdt], va[:, T2, hh, :], pt[pi][:, sl2, off:off + wdt],
                                            start=first, stop=False, skip_group_check=True),
                                            reads=[vak, ptk], writes=["pso%d" % ob])
                                    if T2 % 4 == 3:
                                        B = T2 // 4
                                        ob = B % 3
                                        pos0 = B * 512
                                        r = pos0 // (S_LEN // d)
                                        m0 = pos0 % (S_LEN // d)
                                        dstv = acc[:, hh, :].rearrange("p (m r) -> p r m", r=d)[:, r, m0:m0 + 512]
                                        if bi == 0:
                                            S.op("vector", lambda e, ob=ob, dstv=dstv: e.tensor_copy(dstv, ps_o[ob][:]),
                                                 reads=["pso%d" % ob], writes=["acc%d" % hh])
                                        else:
                                            S.op("vector", lambda e, ob=ob, dstv=dstv: e.tensor_tensor(out=dstv, in0=ps_o[ob][:], in1=dstv, op=ALU.add),
                                                 reads=["pso%d" % ob, "acc%d" % hh], writes=["acc%d" % hh])

                            hist = []
                            for kp in range(NT // 2):
                                pi = emit_scores(kp)
                                hist.append((kp, pi))
                                if len(hist) > 2:
                                    emit_pv(*hist[-3])
                            emit_pv(*hist[-2])
                            emit_pv(*hist[-1])
                    for hh in range(2):
                        for B in range(NG):
                            di = B % 2
                            S.op("tensor", lambda e, di=di, hh=hh, B=B: e.matmul(ps_d[di][:], sel_f, acc[:, hh, B * 512:(B + 1) * 512], start=True, stop=True),
                                 reads=["cst", "acc%d" % hh], writes=["psd%d" % di])
                            S.op("vector", lambda e, di=di: e.reciprocal(rec[di][:], ps_d[di][:]), reads=["psd%d" % di], writes=["rec%d" % di])
                            yi_ = (B // 4) % 2
                            S.op("gpsimd", lambda e, di=di, hh=hh, B=B, yi_=yi_: e.tensor_tensor(out=yaT[yi_][:, (B % 4) * 512:(B % 4 + 1) * 512], in0=acc[0:64, hh, B * 512:(B + 1) * 512],
                                                                                                  in1=rec[di][:], op=ALU.mult),
                                 reads=["rec%d" % di, "acc%d" % hh], writes=["yaT%d" % yi_])
                            if B % 4 == 3:
                                S.op("sync", lambda e, c=c, hh=hh, B=B, yi_=yi_: e.dma_start(
                                    out=mixT[c, hh * 64:(hh + 1) * 64, (B // 4) * 2048:(B // 4 + 1) * 2048], in_=yaT[yi_][:]),
                                    reads=["yaT%d" % yi_], writes=["mix_dram"], dma="yaT%d" % yi_)
                S.barrier()
                S.emit()

            with ExitStack() as ph:
                wo_f = SB(ph, "c_wof", [128, DM], F32)
                wo = SB(ph, "c_wo", [128, 8, DM], BF16)
                bg = SB(ph, "c_bg", [128, 8], F32)
                lng = SB(ph, "c_lng", [128, DM], F32)
                lnb = SB(ph, "c_lnb", [128, DM], F32)
                wr_f = SB(ph, "c_wrf", [128, 8, 36], F32)
                wr = SB(ph, "c_wr", [128, 8, 36], BF16)
                rb = SB(ph, "c_rb", [128, 36], F32)
                mx = [SB(ph, "c_mx%d" % i, [128, 8, 512], BF16) for i in range(2)]
                sq = SB(ph, "c_sq", [128, 8, 512], BF16)
                xa = [SB(ph, "c_xa%d" % i, [128, DM], F32) for i in range(4)]
                accs = [SB(ph, "c_acc%d" % i, [128, DM], F32) for i in range(2)]
                junk = SB(ph, "c_junk", [128, DM], F32)
                st = [SB(ph, "c_st%d" % i, [128, 16], F32) for i in range(2)]
                rs3 = [SB(ph, "c_rs3%d" % i, [128, 3], F32) for i in range(2)]
                bst = [SB(ph, "c_bst%d" % i, [128, 2, 6], F32) for i in range(2)]
                x1 = [SB(ph, "c_x1%d" % i, [128, DM], F32) for i in range(2)]
                x1b = [SB(ph, "c_x1b%d" % i, [128, DM], BF16) for i in range(2)]
                x1T = SB(ph, "c_x1T", [128, 8, 128], BF16)
                lg = [SB(ph, "c_lg%d" % i, [128, 36], F32) for i in range(2)]
                rt = SB(ph, "c_rt", [128, 48], F32)
                elm = SB(ph, "c_elm", [128, NE], F32)
                top8 = SB(ph, "c_top8", [128, 8], F32)
                Abf = SB(ph, "c_Abf", [128, NE], BF16)
                rk = SB(ph, "c_rk", [128, NE], F32)
                tmp32 = SB(ph, "c_tmp32", [128, NE], F32)
                base = SB(ph, "c_base", [128, NE], F32)
                ps_z = [PS(ph, "c_psz%d" % i, [128, DM]) for i in range(2)]
                ps_ss = PS(ph, "c_psss", [128, 512])
                ps_tr = PS(ph, "c_pstr", [128, 8, 128], BF16)
                ps_lg = PS(ph, "c_pslg", [128, 512])
                ps_cnt = PS(ph, "c_pscnt", [128, 512])

                S.op("sync", lambda e: e.dma_start(out=bg[:], in_=branch_gain[l].rearrange("(c p) -> p c", p=128), allow_slow_non_contiguous=True),
                     writes=["bg"], dma="bg")
                for ch in range(8):
                    S.op("sync", lambda e, ch=ch: e.dma_start(out=wo_f[:], in_=w_out[l, ch * 128:(ch + 1) * 128, :]), writes=["wo_f"], dma="wo_f")
                    S.op("vector", lambda e, ch=ch: e.tensor_scalar(wo[:, ch, :], wo_f[:], bg[:, ch:ch + 1], None, ALU.mult),
                         reads=["wo_f", "bg"], writes=["wo"])
                S.op("sync", lambda e: e.dma_start(out=lng[:], in_=ln_gain[l, 0:1, :].partition_broadcast(128)), writes=["lng"], dma="lng")
                S.op("sync", lambda e: e.dma_start(out=lnb[:], in_=ln_bias[l, 0:1, :].partition_broadcast(128)), writes=["lnb"], dma="lnb")
                S.op("sync", lambda e: e.dma_start(out=wr_f[:, :, 0:4], in_=rgw[l].rearrange("(c p) g -> p c g", p=128), allow_slow_non_contiguous=True),
                     writes=["wr_f"], dma="wr_f")
                S.op("sync", lambda e: e.dma_start(out=wr_f[:, :, 4:36], in_=rew[l].rearrange("(c p) g -> p c g", p=128), allow_slow_non_contiguous=True),
                     writes=["wr_f"], dma="wr_f")
                S.op("vector", lambda e: e.tensor_copy(wr[:], wr_f[:]), reads=["wr_f"], writes=["wr"])
                S.op("sync", lambda e: e.dma_start(out=rb[:, 0:4], in_=rgb[l:l + 1, :].partition_broadcast(128)), writes=["rb"], dma="rb")
                S.op("sync", lambda e: e.dma_start(out=rb[:, 4:36], in_=reb[l:l + 1, :].partition_broadcast(128)), writes=["rb"], dma="rb")
                S.op("vector", lambda e: e.memset(base[:], 0.0), writes=["base"])

                parts = ((0, 4), (4, 6), (6, 8))

                def c_s0(tt):
                    g, sub = divmod(tt, 4)
                    if sub == 0:
                        mk = "mx%d" % (g % 2)
                        S.op("sync", lambda e, g=g: e.dma_start(out=mx[g % 2][:], in_=mixT.rearrange("c p t -> p c t")[:, :, g * 512:(g + 1) * 512]),
                             reads=["mix_dram"], writes=[mk], dma=mk)
                    b4 = tt % 4
                    S.op("sync", lambda e, tt=tt, b4=b4: e.dma_start(out=xa[b4][:], in_=Xin[tt * 128:(tt + 1) * 128, :]),
                         writes=["xa%d" % b4], dma="xa%d" % b4)

                def c_s0b(tt):
                    g, sub = divmod(tt, 4)
                    mk = "mx%d" % (g % 2)
                    mxg = mx[g % 2]
                    b2 = tt % 2
                    tok = slice(sub * 128, (sub + 1) * 128)
                    rk_ = "rs3_%d" % b2
                    if sub == 0:
                        S.op("scalar", lambda e: e.activation(out=sq[:], in_=mxg[:], func=AF.Square, bias=zeroc[:], scale=1.0),
                             reads=[mk, "zeroc"], writes=["sq"])
                    for pi_, (c0, c1) in enumerate(parts):
                        for ch in range(c0, c1):
                            S.op("tensor", lambda e, pi_=pi_, ch=ch, c0=c0, c1=c1: e.matmul(
                                ps_ss[:, pi_:pi_ + 1], sq[:, ch, tok], ones_bf[:, 0:1], start=(ch == c0), stop=(ch == c1 - 1), skip_group_check=True),
                                reads=["sq", "ones_bf"], writes=["ps_ss"])
                    S.op("vector", lambda e: e.tensor_tensor(out=rs3[b2][:], in0=ps_ss[:, 0:3], in1=cst[:, C_INVW:C_INVW + 3], op=ALU.mult),
                         reads=["ps_ss", "cst"], writes=[rk_])
                    S.op("scalar", lambda e: e.activation(out=rs3[b2][:], in_=rs3[b2][:], func=AF.Sqrt, bias=epsA2[:], scale=ALPHA * ALPHA),
                         reads=[rk_, "epsA2"], writes=[rk_])
                    S.op("vector", lambda e: e.reciprocal(rs3[b2][:], rs3[b2][:]), reads=[rk_], writes=[rk_])

                def c_s1(tt):
                    g, sub = divmod(tt, 4)
                    mk = "mx%d" % (g % 2)
                    mxg = mx[g % 2]
                    b2 = tt % 2
                    b4 = tt % 4
                    stt = st[b2]
                    sk_ = "st_%d" % b2
                    rk_ = "rs3_%d" % b2
                    tok = slice(sub * 128, (sub + 1) * 128)
                    ak = "accs%d" % b2
                    for pi_, (c0, c1) in enumerate(parts):
                        zi = (tt * 3 + pi_) % 2
                        zk = "psz%d" % zi
                        for half in range(2):
                            for ch in range(c0, c1):
                                S.op("tensor", lambda e, zi=zi, half=half, ch=ch, c0=c0, c1=c1: e.matmul(
                                    ps_z[zi][:, half * 512:(half + 1) * 512], mxg[:, ch, tok], wo[:, ch, half * 512:(half + 1) * 512],
                                    start=(ch == c0), stop=(ch == c1 - 1)), reads=[mk, "wo"], writes=[zk])
                        if pi_ == 0:
                            S.op("vector", lambda e, zi=zi, pi_=pi_: e.scalar_tensor_tensor(
                                out=accs[b2][:], in0=ps_z[zi][:], scalar=rs3[b2][:, pi_:pi_ + 1], in1=xa[b4][:], op0=ALU.mult, op1=ALU.add),
                                reads=[zk, rk_, "xa%d" % b4], writes=[ak])
                        else:
                            S.op("vector", lambda e, zi=zi, pi_=pi_: e.scalar_tensor_tensor(
                                out=accs[b2][:], in0=ps_z[zi][:], scalar=rs3[b2][:, pi_:pi_ + 1], in1=accs[b2][:], op0=ALU.mult, op1=ALU.add),
                                reads=[zk, rk_, ak], writes=[ak])
                    S.op("vector", lambda e: e.bn_stats(bst[b2][:, 0, :], accs[b2][:, 0:512]), reads=[ak], writes=["bst%d" % b2])
                    S.op("vector", lambda e: e.bn_stats(bst[b2][:, 1, :], accs[b2][:, 512:1024]), reads=[ak], writes=["bst%d" % b2])
                    S.op("vector", lambda e: e.bn_aggr(stt[:, 0:2], bst[b2][:]), reads=["bst%d" % b2], writes=[sk_])
                    S.op("scalar", lambda e: e.activation(out=stt[:, 2:3], in_=stt[:, 1:2], func=AF.Sqrt, bias=epsLN[:], scale=1.0), reads=[sk_, "epsLN"], writes=[sk_])
                    S.op("vector", lambda e: e.reciprocal(stt[:, 2:3], stt[:, 2:3]), reads=[sk_], writes=[sk_])

                def c_s2(tt):
                    b2 = tt % 2
                    stt = st[b2]
                    sk_ = "st_%d" % b2
                    ak = "accs%d" % b2
                    x1k = "x1%d" % b2
                    S.op("vector", lambda e: e.tensor_scalar(x1[b2][:], accs[b2][:], stt[:, 0:1], stt[:, 2:3], ALU.subtract, ALU.mult),
                         reads=[ak, sk_], writes=[x1k])
                    S.op("vector", lambda e: e.tensor_tensor(out=x1[b2][:], in0=x1[b2][:], in1=lng[:], op=ALU.mult), reads=[x1k, "lng"], writes=[x1k])
                    S.op("gpsimd", lambda e: e.tensor_tensor(out=x1[b2][:], in0=x1[b2][:], in1=lnb[:], op=ALU.add), reads=[x1k, "lnb"], writes=[x1k])
                    S.op("sync", lambda e: e.dma_start(out=X1[tt * 128:(tt + 1) * 128, :], in_=x1[b2][:]), reads=[x1k], writes=["X1_dram"], dma="x1o%d" % b2)
                    xbk = "x1b%d" % b2
                    S.op("gpsimd", lambda e: e.tensor_copy(x1b[b2][:], x1[b2][:]), reads=[x1k], writes=[xbk])
                    S.op("sync", lambda e: e.dma_start(out=X1b[tt * 128:(tt + 1) * 128, :], in_=x1b[b2][:]), reads=[xbk], writes=["X1b_dram"], dma="x1bo%d" % b2)

                def c_s2b(tt):
                    b2 = tt % 2
                    xbk = "x1b%d" % b2
                    for j in range(8):
                        S.op("tensor", lambda e, j=j: e.transpose(ps_tr[:, j, :], x1b[b2][:, j * 128:(j + 1) * 128], ident_bf[:]),
                             reads=[xbk, "ident_bf"], writes=["ps_tr"])
                    S.op("scalar", lambda e: e.copy(x1T[:], ps_tr[:]), reads=["ps_tr"], writes=["x1T"])
                    for j in range(8):
                        S.op("tensor", lambda e, j=j: e.matmul(ps_lg[:, 0:36], x1T[:, j, :], wr[:, j, :], start=(j == 0), stop=(j == 7), skip_group_check=True),
                             reads=["x1T", "wr"], writes=["ps_lg"])
                    S.op("vector", lambda e: e.tensor_tensor(out=lg[b2][:], in0=ps_lg[:, 0:36], in1=rb[:], op=ALU.add), reads=["ps_lg", "rb"], writes=["lg%d" % b2])

                def c_s3(tt):
                    b2 = tt % 2
                    lgt = lg[b2]
                    lk = "lg%d" % b2
                    S.op("vector", lambda e: e.reduce_max(rt[:, 0:1], lgt[:, 0:4], axis=AX.X), reads=[lk], writes=["rt"])
                    S.op("vector", lambda e: e.tensor_scalar(rt[:, 1:2], rt[:, 0:1], -1.0, None, ALU.mult), reads=["rt"], writes=["rt"])
                    S.op("vector", lambda e: e.memset(rt[:, 2:3], 0.0), reads=["rt"], writes=["rt"])
                    S.op("scalar", lambda e: e.activation(out=rt[:, 4:8], in_=lgt[:, 0:4], func=AF.Exp, bias=rt[:, 1:2], scale=1.0, accum_out=rt[:, 2:3]),
                         reads=[lk, "rt"], writes=["rt"])
                    S.op("vector", lambda e: e.reciprocal(rt[:, 3:4], rt[:, 2:3]), reads=["rt"], writes=["rt"])
                    S.op("vector", lambda e: e.tensor_scalar(rt[:, 8:12], lgt[:, 0:4], rt[:, 0:1], None, ALU.is_equal), reads=[lk, "rt"], writes=["rt"])
                    S.op("vector", lambda e: e.tensor_scalar(rt[:, 12:16], rt[:, 8:12], 1e30, -1e30, ALU.mult, ALU.add), reads=["rt"], writes=["rt"])
                    S.op("vector", lambda e: e.tensor_tensor(out=elm[:].rearrange("p (g j) -> p g j", g=4), in0=lgt[:, 4:36].rearrange("p (g j) -> p g j", g=4),
                                                             in1=rt[:, 12:16].unsqueeze(2).to_broadcast([128, 4, 8]), op=ALU.add),
                         reads=[lk, "rt"], writes=["elm"])
                    S.op("vector", lambda e: e.max(out=top8[:], in_=elm[:]), reads=["elm"], writes=["top8"])
                    S.op("vector", lambda e: e.tensor_scalar(M1all[:, tt, :], elm[:], top8[:, 0:1], None, ALU.is_equal), reads=["elm", "top8"], writes=["M1"])
                    S.op("vector", lambda e: e.tensor_scalar(M2all[:, tt, :], elm[:], top8[:, 1:2], None, ALU.is_equal), reads=["elm", "top8"], writes=["M2"])
                    S.op("vector", lambda e: e.tensor_tensor(out=rt[:, 16:17], in0=top8[:, 0:1], in1=top8[:, 1:2], op=ALU.subtract), reads=["top8"], writes=["rt"])
                    S.op("vector", lambda e: e.tensor_tensor(out=rt[:, 17:18], in0=top8[:, 1:2], in1=top8[:, 0:1], op=ALU.subtract), reads=["top8"], writes=["rt"])
                    S.op("scalar", lambda e: e.activation(out=rt[:, 18:20], in_=rt[:, 16:18], func=AF.Sigmoid, bias=zeroc[:], scale=1.0), reads=["rt", "zeroc"], writes=["rt"])
                    S.op("vector", lambda e: e.tensor_scalar(W01[:, tt, :], rt[:, 18:20], rt[:, 3:4], 1.0 / ALPHA, ALU.mult, ALU.mult), reads=["rt"], writes=["W01"])
                    S.op("vector", lambda e: e.tensor_tensor(out=Abf[:], in0=M1all[:, tt, :], in1=M2all[:, tt, :], op=ALU.add), reads=["M1", "M2"], writes=["Abf"])
                    S.op("tensor", lambda e: e.matmul(ps_lg[:, 64:96], ls_bf[:], Abf[:], start=True, stop=True, skip_group_check=True),
                         reads=["ls_bf", "Abf"], writes=["ps_lg"])
                    S.op("tensor", lambda e: e.matmul(ps_lg[:, 96:128], ones_bf[:], Abf[:], start=False, stop=True, skip_group_check=True),
                         reads=["ones_bf", "Abf"], writes=["ps_lg"])
                    S.op("tensor", lambda e: e.matmul(ps_cnt[0:32, 0:1], Abf[:], ones_bf[:, 0:1], start=(tt == 0), stop=(tt == NT - 1), skip_group_check=True),
                         reads=["Abf", "ones_bf"], writes=["ps_cnt"])
                    S.op("vector", lambda e: e.tensor_tensor(out=rk[:], in0=ps_lg[:, 64:96], in1=base[:], op=ALU.add), reads=["ps_lg", "base"], writes=["rk"])
                    S.op("vector", lambda e: e.tensor_tensor(out=base[:], in0=ps_lg[:, 96:128], in1=base[:], op=ALU.add), reads=["ps_lg", "base", "rk"], writes=["base"])
                    S.op("vector", lambda e: e.tensor_tensor(out=tmp32[:], in0=rk[:], in1=M1all[:, tt, :], op=ALU.mult), reads=["rk", "M1"], writes=["tmp32"])
                    S.op("vector", lambda e: e.reduce_sum(RK[:, tt, 0:1], tmp32[:], axis=AX.X), reads=["tmp32"], writes=["RK"])
                    S.op("vector", lambda e: e.tensor_tensor(out=tmp32[:], in0=rk[:], in1=M2all[:, tt, :], op=ALU.mult), reads=["rk", "M2"], writes=["tmp32"])
                    S.op("vector", lambda e: e.reduce_sum(RK[:, tt, 1:2], tmp32[:], axis=AX.X), reads=["tmp32"], writes=["RK"])

                c_s0(0)
                c_s0(1)
                c_s0b(0)
                for step in range(NT + 3):
                    if step + 2 < NT:
                        c_s0(step + 2)
                    if step + 1 < NT:
                        c_s0b(step + 1)
                    if step < NT:
                        c_s1(step)
                    if 0 <= step - 1 < NT:
                        c_s2(step - 1)
                    if 0 <= step - 2 < NT:
                        c_s2b(step - 2)
                    if 0 <= step - 3 < NT:
                        c_s3(step - 3)

                cntT = SB(ph, "c_cntT", [32, 1], F32)
                cmp_ = SB(ph, "c_cmp", [32, 33], F32)
                nblkT = SB(ph, "c_nblkT", [32, 1], F32)
                nbl_b = SB(ph, "c_nblb", [32, 128], F32)
                pe_ps = SB(ph, "c_peps", [128, 64], F32)
                cmp2 = SB(ph, "c_cmp2", [128, NBLK, NE], F32)
                ebf = SB(ph, "c_ebf", [128, NBLK], F32)
                ebf2 = SB(ph, "c_ebf2", [128, NBLK, 2], F32)
                trail = SB(ph, "c_trail", [128, NBLK], F32)
                pstart = SB(ph, "c_pstart", [128, NE], F32)
                big = SB(ph, "c_big", [128, NT, NE], F32)
                dstf = SB(ph, "c_dstf", [128, NT, 2], F32)
                S.op("vector", lambda e: e.tensor_copy(cntT[:], ps_cnt[0:32, 0:1]), reads=["ps_cnt"], writes=["cntT"])
                S.op("vector", lambda e: e.tensor_scalar(cmp_[:], cst[0:32, C_THR:C_THR + 33], cntT[:, 0:1], None, ALU.is_lt), reads=["cst", "cntT"], writes=["cmp_"])
                S.op("vector", lambda e: e.reduce_sum(nblkT[:], cmp_[:], axis=AX.X), reads=["cmp_"], writes=["nblkT"])
                S.op("vector", lambda e: e.tensor_copy(nbl_b[:], nblkT[:, 0:1].to_broadcast([32, 128])), reads=["nblkT"], writes=["nbl_b"])
                S.op("tensor", lambda e: e.matmul(ps_ss[:, 0:64], nbl_b[:], cst[0:32, C_UCAT:C_UCAT + 64], start=True, stop=True, skip_group_check=True),
                     reads=["nbl_b", "cst"], writes=["ps_ss"])
                S.op("vector", lambda e: e.tensor_copy(pe_ps[:], ps_ss[:, 0:64]), reads=["ps_ss"], writes=["pe_ps"])
                S.op("vector", lambda e: e.tensor_tensor(out=pstart[:], in0=pe_ps[:, 0:32], in1=pe_ps[:, 32:64], op=ALU.subtract), reads=["pe_ps"], writes=["pstart"])
                S.op("vector", lambda e: e.tensor_tensor(out=cmp2[:], in0=pe_ps[:, 0:32].unsqueeze(1).to_broadcast([128, NBLK, NE]),
                                                         in1=cst[:, C_BV:C_BV + NBLK].unsqueeze(2).to_broadcast([128, NBLK, NE]), op=ALU.is_le),
                     reads=["pe_ps", "cst"], writes=["cmp2"])
                S.op("vector", lambda e: e.tensor_reduce(ebf[:], cmp2[:], axis=AX.X, op=ALU.add), reads=["cmp2"], writes=["ebf"])
                S.op("vector", lambda e: e.tensor_scalar(trail[:], ebf[:], 31.5, 4.0e6, ALU.is_ge, ALU.mult), reads=["ebf"], writes=["trail"])
                S.op("vector", lambda e: e.tensor_scalar(ebf[:], ebf[:], 31.0, 128.0, ALU.min, ALU.mult), reads=["ebf"], writes=["ebf"])
                S.op("vector", lambda e: e.tensor_scalar(ebf[:], ebf[:], cst[:, C_IP:C_IP + 1], float(l * NE * 128), ALU.add, ALU.add), reads=["ebf", "cst"], writes=["ebf"])
                S.op("vector", lambda e: e.tensor_copy(GIDX[:], ebf[:]), reads=["ebf"], writes=["GIDX"])
                for h in range(2):
                    S.op("vector", lambda e, h=h: e.tensor_scalar(ebf2[:, :, h], ebf[:], 2.0, float(h), ALU.mult, ALU.add), reads=["ebf"], writes=["ebf2"])
                    S.op("vector", lambda e, h=h: e.tensor_tensor(out=ebf2[:, :, h], in0=ebf2[:, :, h], in1=trail[:], op=ALU.add), reads=["ebf2", "trail"], writes=["ebf2"])
                S.op("vector", lambda e: e.tensor_copy(GIDX2[:], ebf2[:]), reads=["ebf2"], writes=["GIDX2"])
                for k, Mk in enumerate((M1all, M2all)):
                    S.op("vector", lambda e, Mk=Mk: e.tensor_tensor(out=big[:], in0=Mk[:], in1=pstart[:].unsqueeze(1).to_broadcast([128, NT, NE]), op=ALU.mult),
                         reads=["M1", "M2", "pstart"], writes=["big"])
                    S.op("vector", lambda e, k=k: e.tensor_reduce(dstf[:, :, k], big[:], axis=AX.X, op=ALU.add), reads=["big"], writes=["dstf"])
                S.op("vector", lambda e: e.scalar_tensor_tensor(out=dstf[:], in0=dstf[:], scalar=float(BLK), in1=RK[:], op0=ALU.mult, op1=ALU.add),
                     reads=["dstf", "RK"], writes=["dstf"])
                S.op("vector", lambda e: e.tensor_copy(DSTi[:], dstf[:]), reads=["dstf"], writes=["DSTi"])
                S.barrier()
                S.emit()

            with ExitStack() as ph:
                xr = [SB(ph, "d_xr%d" % i, [128, DM], BF16) for i in range(8)]
                for tt in range(NT):
                    b4 = tt % 8
                    k = "xr%d" % b4
                    S.op("sync", lambda e, tt=tt, b4=b4: e.dma_start(out=xr[b4][:], in_=X1b[tt * 128:(tt + 1) * 128, :]), writes=[k], dma=k)
                    for kk in range(2):
                        S.op("gpsimd", lambda e, tt=tt, b4=b4, kk=kk: e.indirect_dma_start(
                            out=Xs, out_offset=bass.IndirectOffsetOnAxis(ap=DSTi[:, tt, kk:kk + 1], axis=0), in_=xr[b4][:], in_offset=None),
                            reads=[k], writes=["Xs_dram"], dma="sc%d" % b4)
                S.barrier()
                S.emit()

            with ExitStack() as ph:
                wg = [SB(ph, "e_wg%d" % i, [128, 8, DE], BF16) for i in range(2)]
                wu = [SB(ph, "e_wu%d" % i, [128, 8, DE], BF16) for i in range(2)]
                wd = [SB(ph, "e_wd%d" % i, [128, 4, DM], BF16) for i in range(2)]
                xs = [SB(ph, "e_xs%d" % i, [128, 4, DM], BF16) for i in range(2)]
                xsT = [SB(ph, "e_xsT%d" % i, [128, 8, 512], BF16) for i in range(2)]
                sg = [SB(ph, "e_sg%d" % i, [128, 512], F32) for i in range(2)]
                hT = [SB(ph, "e_hT%d" % i, [128, 4, 512], BF16) for i in range(2)]
                ysb = [SB(ph, "e_y%d" % i, [128, 4, DM], BF16) for i in range(2)]
                ps_t = [PS(ph, "e_pst%d" % i, [128, 8, 128], BF16) for i in range(2)]
                ps_g = [PS(ph, "e_psg%d" % i, [128, 512]) for i in range(2)]
                ps_u = [PS(ph, "e_psu%d" % i, [128, 512]) for i in range(2)]
                ps_y = [PS(ph, "e_psy%d" % i, [128, 512]) for i in range(2)]
                stg = [SB(ph, "e_stg%d" % i, [128, 2048], F32) for i in range(6)]
                wgv = ewg.rearrange("l e (p h j) f -> (l e p h) (j f)", h=2, j=4)
                wuv = ewu.rearrange("l e (p h j) f -> (l e p h) (j f)", h=2, j=4)
                wdv = ewd.rearrange("l e (p h j) n -> (l e p h) (j n)", h=2, j=2)
                piece = [0]
                yc_ = [0]
                def load_weights(b, which):
                    wb = b % 2
                    for (wt, wv, nm, jn) in which:
                        for h in range(2):
                            si = piece[0] % 6
                            ceng = "scalar" if nm == "wd" else "vector"
                            piece[0] += 1
                            S.op("gpsimd", lambda e, wv=wv, si=si, h=h: e.indirect_dma_start(
                                out=stg[si][:], out_offset=None, in_=wv,
                                in_offset=bass.IndirectOffsetOnAxis(ap=GIDX2[:, b, h:h + 1], axis=0),
                                bounds_check=S.breg, oob_is_err=False),
                                writes=["stg%d" % si], dma="stg%d" % si)
                            dst = wt[wb][:, h * jn:(h + 1) * jn, :].rearrange("p j f -> p (j f)")
                            if ceng == "vector":
                                S.op("vector", lambda e, dst=dst, si=si: e.tensor_copy(dst, stg[si][:]), reads=["stg%d" % si], writes=["%s%d" % (nm, wb)])
                            else:
                                S.op("scalar", lambda e, dst=dst, si=si: e.copy(dst, stg[si][:]), reads=["stg%d" % si], writes=["%s%d" % (nm, wb)])

                W_GU = ((wg, wgv, "wg", 4), (wu, wuv, "wu", 4))
                W_D = ((wd, wdv, "wd", 2),)

                def load_xs(b):
                    bb = b % 2
                    S.op("sync", lambda e: e.dma_start(out=xs[bb][:], in_=Xs[b * BLK:(b + 1) * BLK, :].rearrange("(s p) f -> p s f", p=128)),
                         reads=["Xs_dram"], writes=["xs%d" % bb], dma="xs%d" % bb)

                def e_front(b):
                    bb = b % 2
                    wb = bb
                    if b + 1 < NBLK:
                        load_xs(b + 1)
                    for sub in range(4):
                        ti = sub % 2
                        for j in range(8):
                            S.op("tensor", lambda e, ti=ti, j=j, sub=sub: e.transpose(
                                ps_t[ti][:, j, :], xs[bb][:, sub, :].rearrange("t (p j) -> t j p", j=8)[:, j, :], ident_bf[:]),
                                reads=["xs%d" % bb, "ident_bf"], writes=["e_pst%d" % ti])
                        S.op("vector", lambda e, ti=ti, sub=sub: e.tensor_copy(xsT[bb][:, :, sub * 128:(sub + 1) * 128], ps_t[ti][:]),
                             reads=["e_pst%d" % ti], writes=["xsT%d" % bb])
                    for jf in range(4):
                        gi = jf % 2
                        for (pst_, wt, nm) in ((ps_g, wg, "wg"), (ps_u, wu, "wu")):
                            for jd in range(8):
                                S.op("tensor", lambda e, pst_=pst_, wt=wt, gi=gi, jd=jd, jf=jf: e.matmul(
                                    pst_[gi][:], wt[wb][:, jd, :].rearrange("p (pf jf) -> p jf pf", jf=4)[:, jf, :], xsT[bb][:, jd, :],
                                    start=(jd == 0), stop=(jd == 7)), reads=["%s%d" % (nm, wb), "xsT%d" % bb], writes=["e_%s%d" % (nm, gi)])
                        S.op("scalar", lambda e, gi=gi: e.activation(out=sg[gi][:], in_=ps_g[gi][:], func=AF.Silu, bias=zeroc[:], scale=1.0),
                             reads=["e_wg%d" % gi, "zeroc"], writes=["sg%d" % gi])
                        S.op("vector", lambda e, gi=gi, jf=jf: e.tensor_tensor(out=hT[bb][:, jf, :], in0=ps_u[gi][:], in1=sg[gi][:], op=ALU.mult),
                             reads=["e_wu%d" % gi, "sg%d" % gi], writes=["hT%d" % bb])

                def e_back(b):
                    bb = b % 2
                    wb = bb
                    for sub in range(4):
                        for half in range(2):
                            yi = yc_[0] % 2
                            yc_[0] += 1
                            for jf in range(4):
                                S.op("tensor", lambda e, yi=yi, jf=jf, sub=sub, half=half: e.matmul(
                                    ps_y[yi][:], hT[bb][:, jf, sub * 128:(sub + 1) * 128], wd[wb][:, jf, half * 512:(half + 1) * 512],
                                    start=(jf == 0), stop=(jf == 3)), reads=["hT%d" % bb, "wd%d" % wb], writes=["e_psy%d" % yi])
                            S.op("scalar", lambda e, yi=yi, sub=sub, half=half: e.copy(ysb[bb][:, sub, half * 512:(half + 1) * 512], ps_y[yi][:]),
                                 reads=["e_psy%d" % yi], writes=["ysb%d" % bb])
                    S.op("sync", lambda e: e.dma_start(out=Ys[b * BLK:(b + 1) * BLK, :].rearrange("(s p) f -> p s f", p=128), in_=ysb[bb][:]),
                         reads=["ysb%d" % bb], writes=["Ys_dram"], dma="ysb%d" % bb)

                load_weights(0, W_GU)
                load_weights(0, W_D)
                load_xs(0)
                for b in range(NBLK + 1):
                    if b < NBLK:
                        e_front(b)
                        if b + 1 < NBLK:
                            load_weights(b + 1, W_GU)
                    if b >= 1:
                        e_back(b - 1)
                    if b + 1 < NBLK:
                        load_weights(b + 1, W_D)
                S.barrier()
                S.emit(want_breg=True)

            with ExitStack() as ph:
                lng = SB(ph, "f_lng", [128, DM], F32)
                lnb = SB(ph, "f_lnb", [128, DM], F32)
                y0 = [SB(ph, "f_y0%d" % i, [128, DM], BF16) for i in range(4)]
                y1 = [SB(ph, "f_y1%d" % i, [128, DM], BF16) for i in range(4)]
                xa = [SB(ph, "f_xa%d" % i, [128, DM], F32) for i in range(4)]
                accs = [SB(ph, "f_acc%d" % i, [128, DM], F32) for i in range(2)]
                junk = SB(ph, "f_junk", [128, DM], F32)
                st = [SB(ph, "f_st%d" % i, [128, 16], F32) for i in range(2)]
                bst = [SB(ph, "f_bst%d" % i, [128, 2, 6], F32) for i in range(2)]
                x2 = [SB(ph, "f_x2%d" % i, [128, DM], F32) for i in range(2)]
                xb = [SB(ph, "f_xb%d" % i, [128, DM], BF16) for i in range(4)]
                ps_t = [PS(ph, "f_pst%d" % i, [128, 8, 128], BF16) for i in range(2)]
                xTg = SB(ph, "f_xTg", [128, 8, 512], BF16)
                S.op("sync", lambda e: e.dma_start(out=lng[:], in_=ln_gain[l, 1:2, :].partition_broadcast(128)), writes=["lng"], dma="lng")
                S.op("sync", lambda e: e.dma_start(out=lnb[:], in_=ln_bias[l, 1:2, :].partition_broadcast(128)), writes=["lnb"], dma="lnb")
                last = (l == L - 1)

                def f_s0(tt):
                    b4 = tt % 4
                    S.op("sync", lambda e: e.dma_start(out=xa[b4][:], in_=X1[tt * 128:(tt + 1) * 128, :]),
                         reads=["X1_dram"], writes=["xa%d" % b4], dma="xa%d" % b4)
                    for kk, yt in enumerate((y0, y1)):
                        S.op("gpsimd", lambda e, kk=kk, yt=yt: e.indirect_dma_start(
                            out=yt[b4][:], out_offset=None, in_=Ys, in_offset=bass.IndirectOffsetOnAxis(ap=DSTi[:, tt, kk:kk + 1], axis=0)),
                            reads=["Ys_dram"], writes=["y%d_%d" % (kk, b4)], dma="y%d_%d" % (kk, b4))

                def f_s1(tt):
                    b2 = tt % 2
                    b4 = tt % 4
                    stt = st[b2]
                    sk_ = "st_%d" % b2
                    ak = "accs%d" % b2
                    S.op("vector", lambda e: e.scalar_tensor_tensor(out=accs[b2][:], in0=y0[b4][:], scalar=W01[:, tt, 0:1], in1=xa[b4][:],
                                                                    op0=ALU.mult, op1=ALU.add), reads=["y0_%d" % b4, "xa%d" % b4], writes=[ak])
                    S.op("vector", lambda e: e.scalar_tensor_tensor(out=accs[b2][:], in0=y1[b4][:], scalar=W01[:, tt, 1:2], in1=accs[b2][:],
                                                                    op0=ALU.mult, op1=ALU.add), reads=["y1_%d" % b4, ak], writes=[ak])
                    S.op("vector", lambda e: e.bn_stats(bst[b2][:, 0, :], accs[b2][:, 0:512]), reads=[ak], writes=["bst%d" % b2])
                    S.op("vector", lambda e: e.bn_stats(bst[b2][:, 1, :], accs[b2][:, 512:1024]), reads=[ak], writes=["bst%d" % b2])
                    S.op("vector", lambda e: e.bn_aggr(stt[:, 0:2], bst[b2][:]), reads=["bst%d" % b2], writes=[sk_])
                    S.op("scalar", lambda e: e.activation(out=stt[:, 2:3], in_=stt[:, 1:2], func=AF.Sqrt, bias=epsLN[:], scale=1.0), reads=[sk_, "epsLN"], writes=[sk_])
                    S.op("vector", lambda e: e.reciprocal(stt[:, 2:3], stt[:, 2:3]), reads=[sk_], writes=[sk_])

                def f_s2(tt):
                    g, sub = divmod(tt, 4)
                    b2 = tt % 2
                    stt = st[b2]
                    sk_ = "st_%d" % b2
                    ak = "accs%d" % b2
                    x2k = "x2%d" % b2
                    S.op("vector", lambda e: e.tensor_scalar(x2[b2][:], accs[b2][:], stt[:, 0:1], stt[:, 2:3], ALU.subtract, ALU.mult),
                         reads=[ak, sk_], writes=[x2k])
                    S.op("vector", lambda e: e.tensor_tensor(out=x2[b2][:], in0=x2[b2][:], in1=lng[:], op=ALU.mult), reads=[x2k, "lng"], writes=[x2k])
                    S.op("gpsimd", lambda e: e.tensor_tensor(out=x2[b2][:], in0=x2[b2][:], in1=lnb[:], op=ALU.add), reads=[x2k, "lnb"], writes=[x2k])
                    S.op("sync", lambda e: e.dma_start(out=Xout[tt * 128:(tt + 1) * 128, :], in_=x2[b2][:]), reads=[x2k], writes=["X2_dram"], dma="x2o%d" % b2)
                    if not last:
                        S.op("scalar", lambda e: e.copy(xb[sub][:], x2[b2][:]), reads=[x2k], writes=["xb%d" % sub])
                        if sub == 3:
                            emit_xT_group((ps_t, xTg), lambda s_: "xb%d" % s_, lambda s_: xb[s_], g)

                f_s0(0)
                f_s0(1)
                for step in range(NT + 1):
                    if step + 2 < NT:
                        f_s0(step + 2)
                    if step < NT:
                        f_s1(step)
                    if 0 <= step - 1 < NT:
                        f_s2(step - 1)
                S.barrier()
                S.emit()
    return nc


def make_consts():
    c = np.zeros((128, C_END), np.float32)
    c[:, C_ID:C_ID + 128] = np.eye(128, dtype=np.float32)
    m = np.arange(128)
    sw = np.where((m % 64) < 32, m + 32, m - 32)
    perm = np.zeros((128, 128), np.float32)
    perm[sw, m] = 1.0
    c[:, C_PERM:C_PERM + 128] = perm
    j = np.arange(128)[:, None]
    i = np.arange(128)[None, :]
    c[:, C_MASK:C_MASK + 128] = np.where(i >= j, 0.0, NEG)
    c[:, C_MASK + 128:C_MASK + 256] = np.where(j >= i, 0.0, NEG)
    c[:, C_LS:C_LS + 128] = (j < i).astype(np.float32)
    e1 = np.arange(32)[:, None]
    e2 = np.arange(32)[None, :]
    c[0:32, C_UCAT:C_UCAT + 32] = (e1 <= e2).astype(np.float32)
    c[0:32, C_UCAT + 32:C_UCAT + 64] = np.eye(32, dtype=np.float32)
    c[:, C_THR:C_THR + 33] = (np.arange(33) * BLK).astype(np.float32)[None, :]
    c[:, C_BV:C_BV + NBLK] = np.arange(NBLK, dtype=np.float32)[None, :]
    c[:, C_IE:C_IE + 32] = np.arange(32, dtype=np.float32)[None, :]
    c[:, C_IP] = np.arange(128, dtype=np.float32)
    c[64, C_SEL:C_SEL + 64] = 1.0
    c[:, C_INVW:C_INVW + 3] = np.array([1.0 / 512, 1.0 / 256, 1.0 / 256], np.float32)[None, :]
    half = 32
    inv_freq = (np.float32(10000.0) ** (-np.arange(half, dtype=np.float32) / np.float32(half))).astype(np.float32)
    ang = (np.arange(S_LEN, dtype=np.float32)[:, None] * inv_freq[None, :]).astype(np.float32)
    cos = np.cos(ang).astype(np.float32).T
    sin = np.sin(ang).astype(np.float32).T
    rc = np.zeros((128, S_LEN), np.float32)
    rs = np.zeros((128, S_LEN), np.float32)
    for p in range(128):
        cc = p % 64
        rc[p] = cos[cc % 32]
        rs[p] = -sin[cc] if cc < 32 else sin[cc - 32]
    return c, rc, rs


_CACHE = {}


def kernel(**inputs):
    depth = DEPTH
    if "nc" not in _CACHE:
        _CACHE["nc"] = build_program(depth)
        _CACHE["consts"] = make_consts()
    nc = _CACHE["nc"]
    c, rc, rs = _CACHE["consts"]
    x = np.ascontiguousarray(np.asarray(inputs["x"], dtype=np.float32))
    shared = {k: np.ascontiguousarray(np.asarray(v, dtype=np.float32)) for k, v in inputs.items() if k != "x"}
    shared["consts"] = c
    shared["ropecos"] = rc
    shared["ropesin"] = rs
    in_maps = []
    for i in range(NCORES):
        m = dict(shared)
        m["x"] = x[i]
        in_maps.append(m)
    res = run_bass_kernel_spmd(nc, in_maps, core_ids=list(range(NCORES)))
    return np.stack([np.asarray(r["out"], dtype=np.float32) for r in res.results], axis=0)
```
